# Optimizing a Trainium2 kernel written in Bass

```python
import jax, jax.numpy as jnp
from jax import lax
import numpy as np

D_MODEL = 1024
BATCH = 4
SEQ = 4096
DEPTH = 4

GRID_W = 64
CTX_LEN = 256
N_MIXERS = 3
N_MOD = 6
NORM_EPS = 1e-6

D_RNN = D_MODEL
RG_BLOCKS = 8
RG_BW = D_RNN // RG_BLOCKS
CONV_W = 4
RG_C = 8.0

HEAD_DIM = 128
N_Q_HEADS = D_MODEL // HEAD_DIM
N_KV_HEADS = 2
GQA_GROUP = N_Q_HEADS // N_KV_HEADS
Q_BLOCK = 128
ROPE_THETA = 10000.0
AXIS_DIM = HEAD_DIM // 2
N_FREQ = AXIS_DIM // 2

CHUNK = 128
D_CM = 2 * D_MODEL
CM_GROUPS = 8
CM_GW = D_CM // CM_GROUPS

N_GROUPS = 4
EXPERTS_PER_GROUP = 8
N_EXPERTS = N_GROUPS * EXPERTS_PER_GROUP
TOP_K = 2
D_EXPERT = 512
MOE_BLOCK = 128

kernel_name = "hybrid_rglru_gqa_gmlp_hmoe_prefix_dit"


def rms_norm(x, g):
    xf = x.astype(jnp.float32)
    y = xf * lax.rsqrt(jnp.mean(xf * xf, axis=-1, keepdims=True) + NORM_EPS)
    return (y * g.astype(jnp.float32)).astype(x.dtype)


def layer_norm(x, g, b):
    xf = x.astype(jnp.float32)
    mu = jnp.mean(xf, axis=-1, keepdims=True)
    var = jnp.mean(jnp.square(xf - mu), axis=-1, keepdims=True)
    y = (xf - mu) * lax.rsqrt(var + NORM_EPS) * g.astype(jnp.float32) + b.astype(jnp.float32)
    return y.astype(x.dtype)


def modulate(h, shift, scale):
    return h * (1 + scale) + shift


def centred_dwconv(x, w, b):
    left = CONV_W // 2
    y = lax.conv_general_dilated(x, w[:, None, :].astype(x.dtype), (1,), [(left, CONV_W - 1 - left)],
                                 dimension_numbers=('NWC', 'WIO', 'NWC'),
                                 feature_group_count=x.shape[-1])
    return y + b


def _linear_combine(left, right):
    a_l, b_l = left
    a_r, b_r = right
    return a_l * a_r, a_r * b_l + b_r


def rglru_scan(xs, wa, ba, wi, bi, lam, h0):
    B, L, _ = xs.shape
    xb = xs.reshape(B, L, RG_BLOCKS, RG_BW)
    r = jax.nn.sigmoid((jnp.einsum('blnc,ncd->blnd', xb, wa).reshape(B, L, D_RNN) + ba).astype(jnp.float32))
    i = jax.nn.sigmoid((jnp.einsum('blnc,ncd->blnd', xb, wi).reshape(B, L, D_RNN) + bi).astype(jnp.float32))
    log_a = -RG_C * r * jax.nn.softplus(-lam.astype(jnp.float32))
    a = jnp.exp(log_a)
    b = jnp.sqrt(-jnp.expm1(2.0 * log_a)) * i * xs.astype(jnp.float32)
    a_cum, h = lax.associative_scan(_linear_combine, (a, b), axis=1)
    if h0 is not None:
        h = h + a_cum * h0[:, None, :]
    return h, h[:, -1]


def rglru_mixer(h_ctx, h_lat, w_in, conv_w, conv_b, wa, ba, wi, bi, lam, w_out, with_ctx):
    z_lat = h_lat @ w_in
    gate_lat, x_lat = z_lat[..., :D_RNN], z_lat[..., D_RNN:]
    if with_ctx:
        z_ctx = h_ctx @ w_in
        gate_ctx, x_ctx = z_ctx[..., :D_RNN], z_ctx[..., D_RNN:]
    else:
        x_ctx = h_ctx @ w_in[:, D_RNN:]
    x_lat = centred_dwconv(x_lat, conv_w, conv_b)
    x_ctx = centred_dwconv(x_ctx, conv_w, conv_b)
    hl_dirs, hc_dirs = [], []
    for d in range(2):
        flip = (lambda t: t[:, ::-1]) if d == 1 else (lambda t: t)
        hc, hc_last = rglru_scan(flip(x_ctx), wa[d], ba[d], wi[d], bi[d], lam[d], None)
        hl, _ = rglru_scan(flip(x_lat), wa[d], ba[d], wi[d], bi[d], lam[d], hc_last)
        hl_dirs.append(flip(hl))
        if with_ctx:
            hc_dirs.append(flip(hc))
    y_lat = (jax.nn.gelu(gate_lat) * (hl_dirs[0] + hl_dirs[1]).astype(h_lat.dtype)) @ w_out
    y_ctx = None
    if with_ctx:
        y_ctx = (jax.nn.gelu(gate_ctx) * (hc_dirs[0] + hc_dirs[1]).astype(h_ctx.dtype)) @ w_out
    return y_ctx, y_lat


def axial_rope_tables(rows):
    row = jnp.broadcast_to(jnp.arange(rows, dtype=jnp.float32)[:, None], (rows, GRID_W)).reshape(-1)
    col = jnp.broadcast_to(jnp.arange(GRID_W, dtype=jnp.float32)[None, :], (rows, GRID_W)).reshape(-1)
    inv = ROPE_THETA ** (-jnp.arange(N_FREQ, dtype=jnp.float32) * 2.0 / AXIS_DIM)
    ang = jnp.stack([row[:, None] * inv, col[:, None] * inv], axis=1)
    return jnp.cos(ang), jnp.sin(ang)


def apply_axial_rope(x, cos, sin):
    xf = x.astype(jnp.float32).reshape(*x.shape[:-1], 2, 2, N_FREQ)
    x1, x2 = xf[..., 0, :], xf[..., 1, :]
    out = jnp.stack([x1 * cos - x2 * sin, x2 * cos + x1 * sin], axis=-2)
    return out.reshape(x.shape).astype(x.dtype)


def _project_heads(h, w_qkv, q_g, k_g, need_q):
    B, L, _ = h.shape
    nq = N_Q_HEADS * HEAD_DIM
    nkv = N_KV_HEADS * HEAD_DIM
    kv = h @ w_qkv[:, nq:]
    k = kv[..., :nkv].reshape(B, L, N_KV_HEADS, HEAD_DIM).transpose(0, 2, 1, 3)
    v = kv[..., nkv:].reshape(B, L, N_KV_HEADS, HEAD_DIM).transpose(0, 2, 1, 3)
    q = None
    if need_q:
        q = (h @ w_qkv[:, :nq]).reshape(B, L, N_KV_HEADS, GQA_GROUP, HEAD_DIM).transpose(0, 2, 3, 1, 4)
        q = rms_norm(q, q_g)
    return q, rms_norm(k, k_g), v


def _attend(q, k, v):
    s = jnp.einsum('bkgqd,bknd->bkgqn', q, k, preferred_element_type=jnp.float32) * (HEAD_DIM ** -0.5)
    p = jax.nn.softmax(s, axis=-1)
    return jnp.einsum('bkgqn,bknd->bkgqd', p.astype(v.dtype), v)


def attention_mixer(h_ctx, h_lat, w_qkv, q_g, k_g, w_o, cos, sin, with_ctx):
    B, L, _ = h_lat.shape
    q_c, k_c, v_c = _project_heads(h_ctx, w_qkv, q_g, k_g, with_ctx)
    q_l, k_l, v_l = _project_heads(h_lat, w_qkv, q_g, k_g, True)
    q_l = apply_axial_rope(q_l, cos, sin)
    k_l = apply_axial_rope(k_l, cos, sin)
    k_all = jnp.concatenate([k_c, k_l], axis=2)
    v_all = jnp.concatenate([v_c, v_l], axis=2)
    nblk = L // Q_BLOCK
    q_blocks = q_l.reshape(B, N_KV_HEADS, GQA_GROUP, nblk, Q_BLOCK, HEAD_DIM).transpose(3, 0, 1, 2, 4, 5)
    o = lax.map(lambda qb: _attend(qb, k_all, v_all), q_blocks)
    o = o.transpose(1, 0, 4, 2, 3, 5).reshape(B, L, N_Q_HEADS * HEAD_DIM)
    y_lat = o @ w_o
    y_ctx = None
    if with_ctx:
        oc = _attend(q_c, k_c, v_c)
        oc = oc.transpose(0, 3, 1, 2, 4).reshape(B, h_ctx.shape[1], N_Q_HEADS * HEAD_DIM)
        y_ctx = oc @ w_o
    return y_ctx, y_lat


def chunk_mlp(h, w_in, ln_g, ln_b, w_s, b_s, w_out):
    B, L, _ = h.shape
    z = jax.nn.gelu(h @ w_in)
    u, v = z[..., :D_CM], z[..., D_CM:]
    v = layer_norm(v, ln_g, ln_b).reshape(B, L // CHUNK, CHUNK, CM_GROUPS, CM_GW)
    v = jnp.einsum('gpq,bnqgc->bnpgc', w_s, v) + b_s.T[:, :, None]
    return (u * v.reshape(B, L, D_CM)) @ w_out


def hier_moe(xt, w_group, b_group, w_router, b_router, w_gate, w_up, w_down):
    T, D = xt.shape
    gl = (xt @ w_group + b_group).astype(jnp.float32)
    gp = jax.nn.softmax(gl, axis=-1)
    _, g_sel = lax.top_k(gl, 1)
    gate_g = jnp.take_along_axis(gp, g_sel, axis=1)
    el = (jnp.einsum('td,dge->tge', xt, w_router) + b_router).astype(jnp.float32)
    el = jnp.take_along_axis(el, g_sel[:, :, None], axis=1)[:, 0]
    top_v, top_i = lax.top_k(el, TOP_K)
    weights = jax.nn.softmax(top_v, axis=-1) * gate_g
    expert = g_sel * EXPERTS_PER_GROUP + top_i
    A = T * TOP_K
    flat_e = expert.reshape(-1)
    flat_w = weights.reshape(-1)
    flat_tok = jnp.repeat(jnp.arange(T, dtype=jnp.int32), TOP_K)
    order = jnp.argsort(flat_e)
    se = flat_e[order]
    counts = jnp.bincount(flat_e, length=N_EXPERTS)
    padded = (counts + MOE_BLOCK - 1) // MOE_BLOCK * MOE_BLOCK
    pad_end = jnp.cumsum(padded)
    pad_start = pad_end - padded
    start = jnp.cumsum(counts) - counts
    dest = pad_start[se] + jnp.arange(A, dtype=jnp.int32) - start[se]
    R = -(-A // MOE_BLOCK) * MOE_BLOCK + N_EXPERTS * MOE_BLOCK
    n_blocks = R // MOE_BLOCK
    row_tok = jnp.full((R,), T, jnp.int32).at[dest].set(flat_tok[order])
    row_w = jnp.zeros((R,), jnp.float32).at[dest].set(flat_w[order])
    blk_e = jnp.minimum(jnp.searchsorted(pad_end, jnp.arange(n_blocks, dtype=jnp.int32) * MOE_BLOCK,
                                         side='right'), N_EXPERTS - 1)
    x_rows = jnp.concatenate([xt, jnp.zeros((1, D), xt.dtype)], axis=0)[row_tok]
    x_rows = x_rows.reshape(n_blocks, MOE_BLOCK, D)

    def expert_block(args):
        xb, e = args
        return (jax.nn.silu(xb @ w_gate[e]) * (xb @ w_up[e])) @ w_down[e]

    y_rows = lax.map(expert_block, (x_rows, blk_e)).reshape(R, D)
    y = jax.ops.segment_sum(y_rows * row_w[:, None].astype(y_rows.dtype), row_tok, num_segments=T + 1)
    return y[:T]


def setup_inputs(seed: int = 0) -> dict:
    key = jax.random.key(seed)
    keys = iter(jax.random.split(key, 48))
    f32 = jnp.float32

    def normal(shape, scale):
        return jax.random.normal(next(keys), shape, f32) * scale

    n_a = len(range(0, DEPTH, N_MIXERS))
    n_b = len(range(1, DEPTH, N_MIXERS))
    n_c = len(range(2, DEPTH, N_MIXERS))
    a0 = jax.random.uniform(next(keys), (n_a, 2, D_RNN), f32, 0.9, 0.999) ** (1.0 / RG_C)
    hd_all = N_Q_HEADS * HEAD_DIM
    return {
        "x": normal((BATCH, SEQ, D_MODEL), 1.0),
        "c": normal((BATCH, D_MODEL), 1.0),
        "ctx": normal((BATCH, CTX_LEN, D_MODEL), 1.0),
        "c_ctx": normal((D_MODEL,), 1.0),
        "ada_w": normal((DEPTH, D_MODEL, N_MOD * D_MODEL), 0.5 * D_MODEL ** -0.5),
        "ada_b": normal((DEPTH, N_MOD * D_MODEL), 0.02),
        "norm_mix_g": 1.0 + normal((DEPTH, D_MODEL), 0.02),
        "norm_ffn_g": 1.0 + normal((DEPTH, D_MODEL), 0.02),
        "rg_w_in": normal((n_a, D_MODEL, 2 * D_RNN), D_MODEL ** -0.5),
        "rg_conv_w": normal((n_a, CONV_W, D_RNN), CONV_W ** -0.5),
        "rg_conv_b": normal((n_a, D_RNN), 0.02),
        "rg_wa": normal((n_a, 2, RG_BLOCKS, RG_BW, RG_BW), RG_BW ** -0.5),
        "rg_ba": normal((n_a, 2, D_RNN), 0.02),
        "rg_wi": normal((n_a, 2, RG_BLOCKS, RG_BW, RG_BW), RG_BW ** -0.5),
        "rg_bi": normal((n_a, 2, D_RNN), 0.02),
        "rg_lambda": jnp.log(a0) - jnp.log1p(-a0),
        "rg_w_out": normal((n_a, D_RNN, D_MODEL), D_RNN ** -0.5),
        "at_w_qkv": normal((n_b, D_MODEL, (N_Q_HEADS + 2 * N_KV_HEADS) * HEAD_DIM), D_MODEL ** -0.5),
        "at_q_g": 1.0 + normal((n_b, HEAD_DIM), 0.02),
        "at_k_g": 1.0 + normal((n_b, HEAD_DIM), 0.02),
        "at_w_o": normal((n_b, hd_all, D_MODEL), hd_all ** -0.5),
        "cm_w_in": normal((n_c, D_MODEL, 2 * D_CM), D_MODEL ** -0.5),
        "cm_ln_g": 1.0 + normal((n_c, D_CM), 0.02),
        "cm_ln_b": normal((n_c, D_CM), 0.02),
        "cm_w_s": normal((n_c, CM_GROUPS, CHUNK, CHUNK), CHUNK ** -0.5),
        "cm_b_s": 1.0 + normal((n_c, CM_GROUPS, CHUNK), 0.02),
        "cm_w_out": normal((n_c, D_CM, D_MODEL), D_CM ** -0.5),
        "moe_w_group": normal((DEPTH, D_MODEL, N_GROUPS), D_MODEL ** -0.5),
        "moe_b_group": normal((DEPTH, N_GROUPS), 0.01),
        "moe_w_router": normal((DEPTH, D_MODEL, N_GROUPS, EXPERTS_PER_GROUP), D_MODEL ** -0.5),
        "moe_b_router": normal((DEPTH, N_GROUPS, EXPERTS_PER_GROUP), 0.01),
        "moe_w_gate": normal((DEPTH, N_EXPERTS, D_MODEL, D_EXPERT), D_MODEL ** -0.5),
        "moe_w_up": normal((DEPTH, N_EXPERTS, D_MODEL, D_EXPERT), D_MODEL ** -0.5),
        "moe_w_down": normal((DEPTH, N_EXPERTS, D_EXPERT, D_MODEL), D_EXPERT ** -0.5),
    }


def reference(x, c, ctx, c_ctx, ada_w, ada_b, norm_mix_g, norm_ffn_g,
              rg_w_in, rg_conv_w, rg_conv_b, rg_wa, rg_ba, rg_wi, rg_bi, rg_lambda, rg_w_out,
              at_w_qkv, at_q_g, at_k_g, at_w_o,
              cm_w_in, cm_ln_g, cm_ln_b, cm_w_s, cm_b_s, cm_w_out,
              moe_w_group, moe_b_group, moe_w_router, moe_b_router, moe_w_gate, moe_w_up, moe_w_down):
    B, S, D = x.shape
    rows = S // GRID_W
    cos, sin = axial_rope_tables(rows)
    c_act = jax.nn.silu(c)
    cc_act = jax.nn.silu(c_ctx)
    for l in range(DEPTH):
        kind = l % N_MIXERS
        j = l // N_MIXERS
        last = l == DEPTH - 1
        with_ctx = not last
        mod_l = (c_act @ ada_w[l] + ada_b[l]).reshape(B, N_MOD, 1, D)
        mod_c = (cc_act @ ada_w[l] + ada_b[l]).reshape(N_MOD, D)
        h_lat = modulate(rms_norm(x, norm_mix_g[l]), mod_l[:, 0], mod_l[:, 1])
        need_ctx_in = not (last and kind == 2)
        h_ctx = modulate(rms_norm(ctx, norm_mix_g[l]), mod_c[0], mod_c[1]) if need_ctx_in else None
        if kind == 0:
            y_ctx, y_lat = rglru_mixer(h_ctx, h_lat, rg_w_in[j], rg_conv_w[j], rg_conv_b[j], rg_wa[j], rg_ba[j],
                                       rg_wi[j], rg_bi[j], rg_lambda[j], rg_w_out[j], with_ctx)
        elif kind == 1:
            y_ctx, y_lat = attention_mixer(h_ctx, h_lat, at_w_qkv[j], at_q_g[j], at_k_g[j], at_w_o[j],
                                           cos, sin, with_ctx)
        else:
            y_lat = chunk_mlp(h_lat, cm_w_in[j], cm_ln_g[j], cm_ln_b[j], cm_w_s[j], cm_b_s[j], cm_w_out[j])
            y_ctx = None
            if with_ctx:
                y_ctx = chunk_mlp(h_ctx, cm_w_in[j], cm_ln_g[j], cm_ln_b[j], cm_w_s[j], cm_b_s[j], cm_w_out[j])
        x = x + mod_l[:, 2] * y_lat
        hf_l = modulate(rms_norm(x, norm_ffn_g[l]), mod_l[:, 3], mod_l[:, 4])
        if last:
            y = hier_moe(hf_l.reshape(-1, D), moe_w_group[l], moe_b_group[l], moe_w_router[l],
                         moe_b_router[l], moe_w_gate[l], moe_w_up[l], moe_w_down[l])
            x = x + mod_l[:, 5] * y.reshape(B, S, D)
        else:
            ctx = ctx + mod_c[2] * y_ctx
            hf_c = modulate(rms_norm(ctx, norm_ffn_g[l]), mod_c[3], mod_c[4])
            n_ctx_tok = hf_c.shape[0] * hf_c.shape[1]
            tokens = jnp.concatenate([hf_c.reshape(-1, D), hf_l.reshape(-1, D)], axis=0)
            y = hier_moe(tokens, moe_w_group[l], moe_b_group[l], moe_w_router[l], moe_b_router[l],
                         moe_w_gate[l], moe_w_up[l], moe_w_down[l])
            ctx = ctx + mod_c[5] * y[:n_ctx_tok].reshape(ctx.shape)
            x = x + mod_l[:, 5] * y[n_ctx_tok:].reshape(B, S, D)
    return x
```

```python
import numpy as np
from contextlib import ExitStack
import concourse.bass as bass
import concourse.mybir as mybir
from concourse.bass_utils import run_bass_kernel_spmd

F32 = mybir.dt.float32
BF16 = mybir.dt.bfloat16
AF = mybir.ActivationFunctionType
ALU = mybir.AluOpType
AX = mybir.AxisListType

SAME_ENGINE_SYNC = True
SPARSE = True
T = 4352
NCTX = 256
EPS = 1e-6
NBLK = 49
I32 = mybir.dt.int32
TILES = [(0, 256, 1)] + [(256 + 512 * i, 512, 0) for i in range(8)]


class Prog:
    ENG = ("pe", "act", "dve", "pool", "sp")

    def __init__(self, nc, stack):
        self.nc = nc
        self.stack = stack
        self.q = {e: [] for e in self.ENG}
        self.cnt = {e: 0 for e in self.ENG}
        self.esem = {e: stack.enter_context(nc.semaphore("s_" + e)) for e in self.ENG}
        self.known = {e: {} for e in self.ENG}
        self.state = {}
        self.dsem = {}
        self.dcnt = {}
        self.semobj = {}
        self.n_ins = 0

    def sb(self, st, name, shape, dt):
        self.n_sb = getattr(self, "n_sb", 0) + 1
        return st.enter_context(self.nc.sbuf_tensor("s%d_%s" % (self.n_sb, name), list(shape), dt))

    def _st(self, k):
        s = self.state.get(k)
        if s is None:
            s = self.state[k] = [None, []]
        return s

    def _need(self, eng, ev, skip_sem=None):
        if ev is None:
            return
        sem, val, src = ev
        if skip_sem is not None and sem is skip_sem:
            return
        if src == eng and (eng == "pe" or not SAME_ENGINE_SYNC):
            return
        kn = self.known[eng]
        if kn.get(id(sem), 0) >= val:
            return
        kn[id(sem)] = val
        self.q[eng].append(("w", sem, val))

    def _deps(self, eng, reads, writes, skip_sem=None):
        for k in reads:
            self._need(eng, self._st(k)[0])
        for k in writes:
            s = self._st(k)
            self._need(eng, s[0], skip_sem)
            for ev in s[1]:
                self._need(eng, ev)

    def _commit(self, ev, reads, writes):
        for k in reads:
            s = self._st(k)
            s[1].append(ev)
            if len(s[1]) > 64:
                s[1] = s[1][-64:]
        for k in writes:
            s = self._st(k)
            s[0] = ev
            s[1] = []

    def op(self, eng, fn, r=(), w=()):
        self._deps(eng, r, w)
        self.cnt[eng] += 1
        ev = (self.esem[eng], self.cnt[eng], eng)
        self.q[eng].append(("o", fn, self.esem[eng], 1))
        self._commit(ev, r, w)

    def pe(self, fn, r=(), w=()):
        self.op("pe", fn, r, w)

    def act(self, fn, r=(), w=()):
        self.op("act", fn, r, w)

    def dve(self, fn, r=(), w=()):
        self.op("dve", fn, r, w)

    def pool(self, fn, r=(), w=()):
        self.op("pool", fn, r, w)

    def dma(self, eng, out, in_, r, w, sbkey, **kw):
        sem = self.dsem.get(sbkey)
        if sem is None:
            sem = self.dsem[sbkey] = self.stack.enter_context(
                self.nc.semaphore("d%d" % len(self.dsem)))
            self.dcnt[sbkey] = 0
        self._deps(eng, r, w, skip_sem=sem)
        self.dcnt[sbkey] += 16
        ev = (sem, self.dcnt[sbkey], "dma")
        self.q[eng].append(("o", lambda e: e.dma_start(out=out, in_=in_, **kw), sem, 16))
        self._commit(ev, r, w)

    def idma(self, out, out_idx, in_, in_idx, r, w, sbkey):
        eng = "pool"
        sem = self.dsem.get(sbkey)
        if sem is None:
            sem = self.dsem[sbkey] = self.stack.enter_context(
                self.nc.semaphore("d%d" % len(self.dsem)))
            self.dcnt[sbkey] = 0
        self._deps(eng, r, w, skip_sem=sem)
        self.dcnt[sbkey] += 16
        ev = (sem, self.dcnt[sbkey], "dma")
        oo = None if out_idx is None else bass.IndirectOffsetOnAxis(out_idx, 0)
        io = None if in_idx is None else bass.IndirectOffsetOnAxis(in_idx, 0)
        self.q[eng].append(("o", lambda e: e.indirect_dma_start(out=out, out_offset=oo, in_=in_, in_offset=io),
                            sem, 16))
        self._commit(ev, r, w)

    def barrier(self):
        for e in self.ENG:
            for e2 in self.ENG:
                if e2 != e and self.cnt[e2] > 0:
                    self._need(e, (self.esem[e2], self.cnt[e2], e2))
            for k, sem in self.dsem.items():
                if self.dcnt[k] > 0:
                    self._need(e, (sem, self.dcnt[k], "dma"))

    def emit(self):
        nc = self.nc
        q = self.q

        def run(e, lst):
            for it in lst:
                if it[0] == "w":
                    e.wait_ge(it[1], it[2])
                else:
                    it[1](e).then_inc(it[2], it[3])
        self.n_ins += sum(len(v) for v in q.values())
        with nc.Block() as block:
            @block.tensor
            def _(e):
                run(e, q["pe"])

            @block.scalar
            def _(e):
                run(e, q["act"])

            @block.vector
            def _(e):
                run(e, q["dve"])

            @block.gpsimd
            def _(e):
                run(e, q["pool"])

            @block.sync
            def _(e):
                run(e, q["sp"])
        self.q = {e: [] for e in self.ENG}


def _vec_layout():
    off = {}
    n = 0

    def add(name, k):
        nonlocal n
        off[name] = n
        n += k
    for l in range(4):
        add(("gmix", l), 8)
        add(("gffn", l), 8)
        add(("adab", l), 48)
    for j in range(2):
        add(("convw", j), 32)
        add(("convb", j), 8)
        add(("ba", j), 16)
        add(("bi", j), 16)
        add(("lam", j), 16)
    add("qg", 1)
    add("kg", 1)
    return off, n


VOFF, NV = _vec_layout()


def build(nlayers=4, stop_after=None, debug=False):
    nc = bass.Bass("TRN2", target_bir_lowering=False)

    def din(name, shape, dt=F32):
        return nc.dram_tensor(name, list(shape), dt, kind="ExternalInput").ap()

    xT_d = din("xT", [1024, T])
    cT_d = din("cT", [128, 8, 2])
    vecs_d = din("vecs", [128, NV])
    ada_w_d = din("ada_w", [4, 1024, 6144])
    rg_w_in_d = din("rg_w_in", [2, 1024, 2048])
    rg_wa_d = din("rg_wa", [2, 2, 8, 128, 128])
    rg_wi_d = din("rg_wi", [2, 2, 8, 128, 128])
    rg_w_out_d = din("rg_w_out", [2, 1024, 1024])
    at_w_qkv_d = din("at_w_qkv", [1024, 1536])
    at_w_o_d = din("at_w_o", [1024, 1024])
    cm_w_in_d = din("cm_w_in", [1024, 4096])
    cm_w_sT_d = din("cm_w_sT", [128, 8, 128])
    cm_w_out_d = din("cm_w_out", [2048, 1024])
    cm_lng_d = din("cm_lng", [128, 2048])
    cm_lnb_d = din("cm_lnb", [128, 2048])
    cm_bs_d = din("cm_bs", [128, 16, 128])
    wr_d = din("wr", [4, 128, 8, 36])
    br_d = din("br", [4, 128, 36])
    moe_wg_d = din("moe_w_gate", [4 * 32 * 128 * 2, 2048])
    moe_wu_d = din("moe_w_up", [4 * 32 * 128 * 2, 2048])
    moe_wd_d = din("moe_w_down", [4 * 32 * 128 * 2, 2048])
    hconst_d = din("hconst", [128, 193])
    cos_d = din("cosT", [128, 4096])
    sin_d = din("sinT", [128, 4096])
    rm_d = din("rotm", [128, 128])
    ident_d = din("ident", [128, 128])
    outT_d = nc.dram_tensor("outT", [1024, 4096], F32, kind="ExternalOutput").ap()
    skind = "ExternalOutput" if debug else "Internal"
    xs_d = nc.dram_tensor("xs", [1024, T], F32, kind=skind).ap()
    uT_d = nc.dram_tensor("uT", [2048, T], BF16, kind=skind).ap()
    hfT_d = nc.dram_tensor("hfT", [1024, T], BF16, kind=skind).ap()
    hftok_d = nc.dram_tensor("hftok", [T, 1024], BF16, kind=skind).ap()
    xrows_d = nc.dram_tensor("xrows", [NBLK * 512, 1024], BF16, kind="Internal").ap()
    yrows_d = nc.dram_tensor("yrows", [NBLK * 512, 1024], F32, kind="Internal").ap()
    wcT_d = nc.dram_tensor("wcT", [32, T], F32, kind=skind).ap()
    ydbg_d = nc.dram_tensor("ydbg", [1024, 2176], F32, kind="ExternalOutput").ap() if debug else None
    modT_d = nc.dram_tensor("modT_dbg", [128, 4 * 48 * 2], F32, kind="ExternalOutput").ap() if debug else None
    dbg_d = nc.dram_tensor("dbg", [1024, T], F32, kind="ExternalOutput").ap() if debug else None

    def fm(ap2d):
        return ap2d.rearrange("(c p) t -> p c t", p=128)

    with ExitStack() as gst:
        P = Prog(nc, gst)
        vecs = P.sb(gst, "vecs", [128, NV], F32)
        modT = P.sb(gst, "modT", [128, 4, 48, 2], F32)
        gsA = P.sb(gst, "gsA", [128, 4, 8, 2], F32)
        gsF = P.sb(gst, "gsF", [128, 4, 8, 2], F32)
        ones_bf = P.sb(gst, "ones_bf", [128, 128], BF16)
        ident = P.sb(gst, "ident", [128, 128], F32)
        epsc = P.sb(gst, "epsc", [128, 1], F32)
        sdec = P.sb(gst, "sdec", [128, 2, 16], F32)
        sdec2 = P.sb(gst, "sdec2", [128, 2, 16], F32)
        pb = [gst.enter_context(nc.psum_tensor("pb%d" % i, [128, 512], F32)) for i in range(8)]
        hconst = P.sb(gst, "hconst", [128, 193], F32)
        ustrict = P.sb(gst, "ustrict", [128, 128], BF16)
        identb = P.sb(gst, "identb", [128, 128], BF16)
        iota_e = hconst[:, 128:160]
        iota2p = hconst[:, 160:161]
        ones32 = hconst[:, 161:193]
        pb4b = pb[4][:, :].bitcast(BF16)
        pb5b = pb[5][:, :].bitcast(BF16)
        base = P.sb(gst, "base", [128, 32], F32)
        rinfo = P.sb(gst, "rinfo", [128, 6, 34], F32)
        desti = P.sb(gst, "desti", [128, 2, 34], I32)
        widx = P.sb(gst, "widx", [128, NBLK, 2], I32)

        def V(name, i=0, n=1):
            o = VOFF[name] + i
            return vecs[:, o:o + n]

        P.dma("sp", vecs[:], vecs_d, [], ["vecs"], "vecs")
        P.dma("sp", ident[:], ident_d, [], ["ident"], "ident")
        P.dma("sp", hconst[:], hconst_d, [], ["hconst"], "hconst")
        P.dma("pool", ustrict[:], hconst_d[:, 0:128], [], ["ustrict"], "ustrict")
        P.dma("pool", identb[:], ident_d, [], ["identb"], "identb")
        P.pool(lambda e: e.memset(ones_bf[:], 1.0), [], ["ones"])
        P.pool(lambda e: e.memset(modT[:], 0.0), [], ["modT"])
        P.pool(lambda e: e.memset(base[:], 0.0), [], ["base"])
        P.pool(lambda e: e.memset(rinfo[:], 0.0), [], ["rinfo"])
        P.pool(lambda e: e.memset(epsc[:], EPS), [], ["epsc"])

        with ExitStack() as st:
            cT = P.sb(st, "cT", [128, 8, 2], F32)
            cact = P.sb(st, "cact", [128, 8, 2], F32)
            wblk = [P.sb(st, "adaw%d" % i, [128, 8, 768], F32) for i in range(2)]
            P.dma("sp", cT[:], cT_d, [], ["cT"], "cT")
            P.act(lambda e: e.activation(cact[:], cT[:], AF.Silu), ["cT"], ["cact"])
            it = 0
            for l in range(nlayers):
                wv = ada_w_d[l].rearrange("(kc p) n -> p kc n", p=128)
                for nb in range(8):
                    wt = wblk[it % 2]
                    wk = ("adaw", it % 2)
                    P.dma("sp", wt[:], wv[:, :, nb * 768:(nb + 1) * 768], [], [wk], wk)
                    for o in range(6):
                        oc = nb * 6 + o
                        pk = ("pb", oc % 2)
                        pt = pb[oc % 2]
                        for kc in range(8):
                            P.pe(lambda e, pt=pt, wt=wt, o=o, kc=kc: e.matmul(
                                pt[:, 0:2], wt[:, kc, o * 128:(o + 1) * 128], cact[:, kc, :],
                                start=(kc == 0), stop=(kc == 7)), [wk, "cact"], [pk])
                        P.dve(lambda e, pt=pt, l=l, oc=oc: e.tensor_scalar(
                            modT[:, l, oc, :], pt[:, 0:2], V(("adab", l), oc), None, ALU.add),
                            [pk, "vecs"], ["modT"])
                    it += 1
                for j in range(2):
                    P.dve(lambda e, l=l, j=j: e.scalar_tensor_tensor(
                        gsA[:, l, :, j], modT[:, l, 8:16, j], 1.0, V(("gmix", l), 0, 8), ALU.add, ALU.mult),
                        ["modT", "vecs"], ["gs"])
                    P.dve(lambda e, l=l, j=j: e.scalar_tensor_tensor(
                        gsF[:, l, :, j], modT[:, l, 32:40, j], 1.0, V(("gffn", l), 0, 8), ALU.add, ALU.mult),
                        ["modT", "vecs"], ["gs"])
            for j in range(2):
                P.act(lambda e, j=j: e.activation(sdec[:, j, :], V(("lam", j), 0, 16), AF.Exp, scale=-1.0),
                      ["vecs"], ["sdec"])
                P.act(lambda e, j=j: e.activation(sdec2[:, j, :], sdec[:, j, :], AF.Ln, bias=1.0),
                      ["sdec"], ["sdec2"])
                P.dve(lambda e, j=j: e.tensor_scalar(sdec[:, j, :], sdec2[:, j, :], -8.0, None, ALU.mult),
                      ["sdec2"], ["sdec"])
                P.dve(lambda e, j=j: e.tensor_scalar(sdec2[:, j, :], sdec[:, j, :], 2.0, None, ALU.mult),
                      ["sdec"], ["sdec2"])
            P.barrier()
            P.emit()

        def load_x(xt, xk, xsrc, t0, n, rk):
            P.dma("sp", xt[:, :, 0:n], fm(xsrc)[:, :, t0:t0 + n], rk, [xk], xk)

        def norm_mod(wk_, xt, xk, n, l, j, gs, shift_m, hb, hk, h32=None, h32k=None):
            sq, sqk, rt, rtk, tmp, tmpk = wk_
            ends = lambda k, c: [(k, c)] + ([k] if c in (0, 7) else [])
            P.act(lambda e: e.activation(sq[:, :, 0:n], xt[:, :, 0:n], AF.Square), [xk], [sqk])
            for c in range(8):
                P.pe(lambda e, c=c: e.matmul(pb[7][:, 0:n], ones_bf[:], sq[:, c, 0:n],
                                             start=(c == 0), stop=(c == 7)), [sqk, "ones"], [("pb", 7)])
            P.act(lambda e: e.activation(rt[:, 0:n], pb[7][:, 0:n], AF.Sqrt, bias=epsc[:], scale=1.0 / 1024),
                  [("pb", 7), "epsc"], [rtk])
            P.dve(lambda e: e.reciprocal(rt[:, 0:n], rt[:, 0:n]), [rtk], [rtk])
            for c in range(8):
                P.dve(lambda e, c=c: e.scalar_tensor_tensor(
                    tmp[:, c, 0:n], xt[:, c, 0:n], gs[:, l, c, j:j + 1], rt[:, 0:n], ALU.mult, ALU.mult),
                    [xk, rtk, "gs"], [(tmpk, c)])
            for c in range(8):
                sh = modT[:, l, shift_m * 8 + c, j:j + 1]
                if h32 is not None:
                    P.pool(lambda e, c=c, sh=sh: e.tensor_scalar(
                        h32[:, c, 0:n], tmp[:, c, 0:n], sh, None, ALU.add), [(tmpk, c), "modT"], ends(h32k, c))
                    P.act(lambda e, c=c, sh=sh: e.activation(hb[:, c, 0:n], tmp[:, c, 0:n], AF.Identity, bias=sh),
                          [(tmpk, c), "modT"], ends(hk, c))
                else:
                    P.act(lambda e, c=c, sh=sh: e.activation(
                        hb[:, c, 0:n], tmp[:, c, 0:n], AF.Identity, bias=sh), [(tmpk, c), "modT"], ends(hk, c))

        def routing(rws, l, h32, h32k, t0, n):
            wrt, brt = rws[-2], rws[-1]
            S = n // 128
            for s in range(S):
                for kc in range(8):
                    P.pe(lambda e, s=s, kc=kc: e.matmul(
                        pb[6][:, s * 36:(s + 1) * 36], h32[:, kc, s * 128:(s + 1) * 128], wrt[:, kc, :],
                        start=(kc == 0), stop=(kc == 7)), [h32k, "wrt"], [("pb", 6)])
            steps = []
            D = lambda f, r=(), w=(): steps.append(("dve", f, r, w))
            A = lambda f, r=(), w=(): steps.append(("act", f, r, w))
            PEs = lambda f, r=(), w=(): steps.append(("pe", f, r, w))
            D(lambda e, X: e.tensor_tensor(X["L"][:], pb[6][:, X["s"] * 36:(X["s"] + 1) * 36], brt[:], ALU.add),
              [("pb", 6), "wrt"])
            D(lambda e, X: e.tensor_reduce(X["gm"][:], X["L"][:, 0:4], AX.X, ALU.max))
            D(lambda e, X: e.tensor_scalar(X["gsel"][:], X["L"][:, 0:4], X["gm"][:], None, ALU.is_equal))
            D(lambda e, X: e.tensor_scalar(X["pen"][:], X["gsel"][:], 1e30, -1e30, ALU.mult, ALU.add))
            D(lambda e, X: e.tensor_scalar(X["gm"][:], X["gm"][:], -1.0, None, ALU.mult))
            A(lambda e, X: e.activation(X["ge"][:], X["L"][:, 0:4], AF.Exp, bias=X["gm"][:], accum_out=X["gsum"][:]))
            D(lambda e, X: e.reciprocal(X["gsum"][:], X["gsum"][:]))
            for gg in range(4):
                D(lambda e, X, gg=gg: e.tensor_scalar(
                    X["ml"][:, gg * 8:(gg + 1) * 8], X["L"][:, 4 + gg * 8:12 + gg * 8], X["pen"][:, gg:gg + 1],
                    None, ALU.add))
            D(lambda e, X: e.max(out=X["m8"][:], in_=X["ml"][:]))
            D(lambda e, X: e.tensor_scalar(X["nv1"][:], X["m8"][:, 0:1], -1.0, None, ALU.mult))
            A(lambda e, X: e.activation(X["dx"][:], X["m8"][:, 1:2], AF.Exp, bias=X["nv1"][:]))
            D(lambda e, X: e.tensor_scalar(X["sel2"][:], X["ml"][:], X["m8"][:, 1:2], None, ALU.is_ge))
            D(lambda e, X: e.tensor_scalar(X["m1"][:], X["ml"][:], X["m8"][:, 0:1], None, ALU.is_equal))
            D(lambda e, X: e.tensor_scalar(X["m2"][:], X["ml"][:], X["m8"][:, 1:2], None, ALU.is_equal))
            D(lambda e, X: e.tensor_scalar(X["d2"][:], X["dx"][:], 1.0, None, ALU.add))
            D(lambda e, X: e.reciprocal(X["d2"][:], X["d2"][:]))
            D(lambda e, X: e.tensor_tensor(rinfo[:, 4, X["g"]:X["g"] + 1], X["d2"][:], X["gsum"][:], ALU.mult),
              [], ["RI"])
            D(lambda e, X: e.tensor_tensor(rinfo[:, 5, X["g"]:X["g"] + 1], rinfo[:, 4, X["g"]:X["g"] + 1],
                                           X["dx"][:], ALU.mult), [], ["RI"])
            D(lambda e, X: e.tensor_copy(X["ohb"][:], X["sel2"][:]))
            PEs(lambda e, X: e.matmul(pb[5][:, X["s"] * 64:X["s"] * 64 + 32], ustrict[:], X["ohb"][:],
                                      start=True, stop=True), ["ustrict"], [("pb", 5)])
            PEs(lambda e, X: e.matmul(pb[5][:, X["s"] * 64 + 32:X["s"] * 64 + 64], ones_bf[:], X["ohb"][:],
                                      start=True, stop=True), ["ones"], [("pb", 5)])
            Xs = []
            for s in range(S):
                names = ["L", "gm", "ge", "gsum", "gsel", "pen", "ml", "m8", "nv1", "ex", "sel2", "dx", "d2", "coef",
                         "m1", "m2", "rk", "j32", "ohb"]
                X = dict(zip(names, rws[s]))
                X["s"] = s
                X["g"] = t0 // 128 + s
                Xs.append(X)
            for (eng, f, r, w) in steps:
                for X in Xs:
                    ck = ("rt", X["s"])
                    rr = [ck] + list(r)
                    ww = [ck] + [(("rinfo", X["g"]) if k == "RI" else k) for k in w]
                    P.op(eng, (lambda e, f=f, X=X: f(e, X)), rr, ww)
            for X in Xs:
                s_ = X["s"]
                ck = ("rt", s_)
                P.dve(lambda e, X=X, s_=s_: e.tensor_tensor(X["rk"][:], pb[5][:, s_ * 64:s_ * 64 + 32], base[:], ALU.add),
                      [("pb", 5), "base", ck], [ck])
                P.dve(lambda e, s_=s_: e.tensor_tensor(base[:], pb[5][:, s_ * 64 + 32:s_ * 64 + 64], base[:], ALU.add),
                      [("pb", 5), "base"], ["base"])
            for q_ in range(4):
                for X in Xs:
                    mm = X["m1"] if q_ % 2 == 0 else X["m2"]
                    srcap = X["rk"][:] if q_ < 2 else iota_e
                    ck = ("rt", X["s"])
                    P.dve(lambda e, mm=mm, srcap=srcap, q_=q_, X=X: e.scalar_tensor_tensor(
                        X["j32"][:], mm[:], 1.0, srcap, ALU.mult, ALU.mult,
                        accum_out=rinfo[:, q_, X["g"]:X["g"] + 1]),
                        [ck, "hconst"], [ck, ("rinfo", X["g"])])

        def post_route(st, l, subtiles):
            kk = P.sb(st, "pr_kk", [128, 32], F32)
            pend = P.sb(st, "pr_pend", [128, 32], F32)
            pstart = P.sb(st, "pr_pstart", [128, 32], F32)
            j32 = P.sb(st, "pr_j32", [128, 32], F32)
            dcol = P.sb(st, "pr_dcol", [128, 2, 34], F32)
            bke = P.sb(st, "pr_bke", [128, NBLK], F32)
            wf = P.sb(st, "pr_wf", [128, NBLK, 2], F32)
            K = "postroute"
            D = lambda fn, r=(), w=(): P.dve(fn, [K, "base", "hconst"] + [("rinfo", g_) for g_ in range(34)] + list(r),
                                             [K] + list(w))
            D(lambda e: e.tensor_scalar(kk[:], base[:], 0.0, None, ALU.is_gt))
            for m in range(1, 9):
                D(lambda e, m=m: e.scalar_tensor_tensor(kk[:], base[:], 512.0 * m, kk[:], ALU.is_gt, ALU.add))
            D(lambda e: e.tensor_scalar(kk[:], kk[:], 512.0, None, ALU.mult))
            D(lambda e: e.tensor_tensor_scan(pend[:], ones32, kk[:], 0.0, ALU.mult, ALU.add))
            D(lambda e: e.tensor_tensor(pstart[:], pend[:], kk[:], ALU.subtract))
            D(lambda e: e.memset(dcol[:], 0.0))
            for g in subtiles:
                for sl in range(2):
                    D(lambda e, g=g, sl=sl: e.scalar_tensor_tensor(
                        j32[:], iota_e, rinfo[:, 2 + sl, g:g + 1], pstart[:], ALU.is_equal, ALU.mult,
                        accum_out=dcol[:, sl, g:g + 1]))
            D(lambda e: e.tensor_tensor(dcol[:], dcol[:], rinfo[:, 0:2, :], ALU.add))
            D(lambda e: e.tensor_copy(desti[:], dcol[:]), [], ["desti"])
            for bi in range(NBLK):
                D(lambda e, bi=bi: e.tensor_scalar(j32[:], pend[:], 512.0 * bi, None, ALU.is_le, ALU.add,
                                                   accum_out=bke[:, bi:bi + 1]))
            D(lambda e: e.tensor_scalar(bke[:], bke[:], 31.0, None, ALU.min))
            for h in range(2):
                D(lambda e, h=h: e.tensor_scalar(wf[:, :, h], bke[:], 256.0, iota2p, ALU.mult, ALU.add))
                D(lambda e, h=h: e.tensor_scalar(wf[:, :, h], wf[:, :, h], float(l * 8192 + h), None, ALU.add))
            D(lambda e: e.tensor_copy(widx[:], wf[:]), [], ["widx"])
            D(lambda e: e.memset(base[:], 0.0), [], ["base"])

        def alloc_route(st, l):
            names = [("L", 36), ("gm", 1), ("ge", 4), ("gsum", 1), ("gsel", 4), ("pen", 4), ("ml", 32), ("m8", 8),
                     ("nv1", 1), ("ex", 32), ("sel2", 32), ("dx", 1), ("d2", 1), ("coef", 1), ("m1", 32), ("m2", 32),
                     ("rk", 32), ("j32", 32)]
            rws = []
            for s_ in range(4):
                rw = [P.sb(st, "r%d_%s" % (s_, nm), [128, k], F32) for nm, k in names]
                rw.append(P.sb(st, "r%d_ohb" % s_, [128, 32], BF16))
                rws.append(rw)
            wrt = P.sb(st, "wrt", [128, 8, 36], F32)
            brt = P.sb(st, "brt", [128, 36], F32)
            P.dma("sp", wrt[:], wr_d[l], [], ["wrt"], "wrt")
            P.dma("sp", brt[:], br_d[l], [], ["wrt"], "wrt")
            return rws + [wrt, brt]

        def alloc_common(st, nxt=2):
            d = {}
            d["xt"] = [P.sb(st, "xt%d" % i, [128, 8, 512], F32) for i in range(nxt)] * (3 - nxt)
            d["sq"] = P.sb(st, "sq", [128, 8, 512], BF16)
            d["rt"] = P.sb(st, "rt", [128, 512], F32)
            d["tmp"] = P.sb(st, "tmp", [128, 8, 512], F32)
            return d

        def post_mixer(l, j_unused, wo_dram, KC, last):
            with ExitStack() as st:
                cm = alloc_common(st)
                xms = [P.sb(st, "xm%d" % i, [128, 8, 512], F32) for i in range(2)]
                hfbs = [P.sb(st, "hfb%d" % i, [128, 8, 512], BF16) for i in range(2)]
                h32 = P.sb(st, "h32", [128, 8, 512], F32)
                hft = P.sb(st, "hft", [128, 1024], BF16)
                rw = alloc_route(st, l)
                wo = P.sb(st, "wo", [128, KC, 1024], BF16)
                U = [P.sb(st, "U%d" % i, [128, KC, 512], BF16) for i in range(2)]
                P.dma("pool", wo[:], wo_dram.rearrange("(kc p) n -> p kc n", p=128), [], ["wo"], "wo")
                xsrc = xT_d if l == 0 else xs_d
                tiles = TILES[1:] if last else TILES

                def WO(i):
                    t0, n, j = tiles[i]
                    xt, xk = cm["xt"][i % 2], ("xt", i % 2)
                    load_x(xt, xk, xsrc, t0, n, [("xs", t0)])
                    Ut, uk = U[i % 2], ("U", i % 2)
                    P.dma("sp", Ut[:, :, 0:n], fm(uT_d)[:, 0:KC, t0:t0 + n], ["uT_d"], [uk], uk)
                    xm, xmk = xms[i % 2], "xm%d" % (i % 2)
                    for co in range(8):
                        pk = ("pb", co % 2)
                        pt = pb[co % 2]
                        for kc in range(KC):
                            P.pe(lambda e, pt=pt, co=co, kc=kc, Ut=Ut, n=n: e.matmul(
                                pt[:, 0:n], wo[:, kc, co * 128:(co + 1) * 128], Ut[:, kc, 0:n],
                                start=(kc == 0), stop=(kc == KC - 1)), ["wo", uk], [pk])
                        P.dve(lambda e, pt=pt, co=co, xm=xm, xt=xt, n=n, j=j: e.scalar_tensor_tensor(
                            xm[:, co, 0:n], pt[:, 0:n], modT[:, l, 16 + co, j:j + 1], xt[:, co, 0:n], ALU.mult, ALU.add),
                            [pk, xk, "modT"], [(xmk, co)] + ([xmk] if co in (0, 7) else []))
                    P.dma("pool", fm(xs_d)[:, :, t0:t0 + n], xm[:, :, 0:n], [xmk], [("xs", t0)], xmk)

                def NR(i):
                    t0, n, j = tiles[i]
                    xm, xmk = xms[i % 2], "xm%d" % (i % 2)
                    hfb, hfk = hfbs[i % 2], "hfb%d" % (i % 2)
                    norm_mod((cm["sq"], "sq", cm["rt"], "rt", cm["tmp"], "tmp"), xm, xmk, n, l, j, gsF, 3,
                             hfb, hfk, h32, "h32")

                def TRR(i):
                    t0, n, j = tiles[i]
                    hfb, hfk = hfbs[i % 2], "hfb%d" % (i % 2)
                    for s_ in range(n // 128):
                        pk4 = ("pb", 4)
                        for c in range(8):
                            P.pe(lambda e, s_=s_, c=c, hfb=hfb: e.transpose(
                                pb4b[:, c * 128:(c + 1) * 128], hfb[:, c, s_ * 128:(s_ + 1) * 128], identb[:]),
                                [hfk, "identb"], [pk4])
                        P.act(lambda e: e.activation(hft[:], pb4b[:, :], AF.Identity), [pk4], ["hft"])
                        P.dma("sp", hftok_d[t0 + s_ * 128:t0 + (s_ + 1) * 128, :], hft[:], ["hft"], ["hftok_d"], "hft")
                    routing(rw, l, h32, "h32", t0, n)

                WO(0)
                for i in range(len(tiles)):
                    if i + 1 < len(tiles):
                        WO(i + 1)
                    NR(i)
                    TRR(i)
                post_route(st, l, [t0 // 128 + s_ for (t0, n, j) in tiles for s_ in range(n // 128)])
                P.barrier()
                P.emit()

        def rglru(l, jj, last):
            xsrc = xT_d if l == 0 else xs_d
            with ExitStack() as st:
                hT = P.sb(st, "hT", [128, 8, T], BF16)
                with ExitStack() as st1:
                    cm = alloc_common(st1)
                    for i, (t0, n, j) in enumerate(TILES):
                        xt, xk = cm["xt"][i % 2], ("xt", i % 2)
                        load_x(xt, xk, xsrc, t0, n, [("xs", t0)])
                        norm_mod((cm["sq"], "sq", cm["rt"], "rt", cm["tmp"], "tmp"), xt, xk, n, l, j, gsA, 0,
                                 hT[:, :, t0:t0 + n], "hT")
                    P.barrier()
                    P.emit()
                xrs = [P.sb(st, "xr%d" % i, [128, T], F32) for i in range(2)]
                xc = P.sb(st, "xc", [128, T], F32)
                bb = P.sb(st, "bb", [128, T], F32)
                hs = P.sb(st, "hs", [128, T], F32)
                xcb = P.sb(st, "xcb", [128, T], BF16)
                gls = [P.sb(st, "gl%d" % i, [128, T], BF16) for i in range(2)]
                wx = [P.sb(st, "wx%d" % i, [128, 8, 128], BF16) for i in range(2)]
                wg = [P.sb(st, "wgt%d" % i, [128, 8, 128], BF16) for i in range(2)]
                wai = [P.sb(st, "wai%d" % i, [128, 4, 128], BF16) for i in range(2)]
                rtl = P.sb(st, "rtl", [128, 512], F32)
                itl = P.sb(st, "itl", [128, 512], F32)
                e2 = P.sb(st, "e2", [128, 512], F32)
                tk = lambda k, i: [(k, i)] + ([k] if i in (0, 8) else [])
                win = rg_w_in_d[jj].rearrange("(kc p) n -> p kc n", p=128)

                def LOADW(c):
                    wxt, wgt, wat = wx[c % 2], wg[c % 2], wai[c % 2]
                    wk = ("rgw", c % 2)
                    P.dma("pool", wxt[:], win[:, :, 1024 + c * 128:1024 + (c + 1) * 128], [], [wk], wk)
                    P.dma("pool", wgt[:], win[:, :, c * 128:(c + 1) * 128], [], [wk], wk)
                    for d in range(2):
                        P.dma("pool", wat[:, 2 * d, :], rg_wa_d[jj, d, c], [], [wk], wk)
                        P.dma("pool", wat[:, 2 * d + 1, :], rg_wi_d[jj, d, c], [], [wk], wk)

                def PROJ(c, i0, i1):
                    wxt, wgt = wx[c % 2], wg[c % 2]
                    wk = ("rgw", c % 2)
                    xr, xrn = xrs[c % 2], "xr%d" % (c % 2)
                    gl, gln = gls[c % 2], "gl%d" % (c % 2)
                    for i, (t0, n, j) in enumerate(TILES):
                        if not (i0 <= i < i1):
                            continue
                        pa, pka = pb[i % 2], ("pb", i % 2)
                        pg, pkg = pb[2 + i % 2], ("pb", 2 + i % 2)
                        for kc in range(8):
                            P.pe(lambda e, pa=pa, kc=kc, t0=t0, n=n, wxt=wxt: e.matmul(
                                pa[:, 0:n], wxt[:, kc, :], hT[:, kc, t0:t0 + n], start=(kc == 0), stop=(kc == 7)),
                                [wk, "hT"], [pka])
                        P.act(lambda e, pa=pa, t0=t0, n=n, xr=xr: e.activation(xr[:, t0:t0 + n], pa[:, 0:n], AF.Identity),
                              [pka], tk(xrn, i))
                        for kc in range(8):
                            P.pe(lambda e, pg=pg, kc=kc, t0=t0, n=n, wgt=wgt: e.matmul(
                                pg[:, 0:n], wgt[:, kc, :], hT[:, kc, t0:t0 + n], start=(kc == 0), stop=(kc == 7)),
                                [wk, "hT"], [pkg])
                        P.act(lambda e, pg=pg, t0=t0, n=n, gl=gl: e.activation(
                            gl[:, t0:t0 + n], pg[:, 0:n], AF.Gelu_apprx_tanh), [pkg], tk(gln, i))

                def CONV(c):
                    xr, xrn = xrs[c % 2], "xr%d" % (c % 2)
                    cw = lambda k, c=c: V(("convw", jj), k * 8 + c)
                    for (s0, e0) in ((0, NCTX), (NCTX, T)):
                        P.dve(lambda e, s0=s0, e0=e0, c=c, cw=cw, xr=xr: e.tensor_scalar(
                            xc[:, s0:e0], xr[:, s0:e0], cw(2), V(("convb", jj), c), ALU.mult, ALU.add),
                            [xrn, "vecs"], ["xc"])
                        for k, off in ((0, -2), (1, -1), (3, 1)):
                            lo = max(s0, s0 - off)
                            hi = min(e0, e0 - off)
                            P.dve(lambda e, lo=lo, hi=hi, off=off, k=k, cw=cw, xr=xr: e.scalar_tensor_tensor(
                                xc[:, lo:hi], xr[:, lo + off:hi + off], cw(k), xc[:, lo:hi], ALU.mult, ALU.add),
                                [xrn, "xc", "vecs"], ["xc"])
                    P.act(lambda e: e.activation(xcb[:], xc[:], AF.Identity), ["xc"], ["xcb"])

                def GATES(c, d):
                    wat = wai[c % 2]
                    wk = ("rgw", c % 2)
                    aa, xrn = xrs[c % 2], "xr%d" % (c % 2)
                    for i, (t0, n, j) in enumerate(TILES):
                        pr, pkr = pb[4 + i % 2], ("pb", 4 + i % 2)
                        pi, pki = pb[6 + i % 2], ("pb", 6 + i % 2)
                        P.pe(lambda e, pr=pr, t0=t0, n=n, d=d, wat=wat: e.matmul(
                            pr[:, 0:n], wat[:, 2 * d, :], xcb[:, t0:t0 + n], start=True, stop=True),
                            [wk, "xcb"], [pkr])
                        P.pe(lambda e, pi=pi, t0=t0, n=n, d=d, wat=wat: e.matmul(
                            pi[:, 0:n], wat[:, 2 * d + 1, :], xcb[:, t0:t0 + n], start=True, stop=True),
                            [wk, "xcb"], [pki])
                        P.act(lambda e, pr=pr, n=n, d=d, c=c: e.activation(
                            rtl[:, 0:n], pr[:, 0:n], AF.Sigmoid, bias=V(("ba", jj), d * 8 + c)),
                            [pkr, "vecs"], ["rtl"])
                        P.act(lambda e, pi=pi, n=n, d=d, c=c: e.activation(
                            itl[:, 0:n], pi[:, 0:n], AF.Sigmoid, bias=V(("bi", jj), d * 8 + c)),
                            [pki, "vecs"], ["itl"])
                        P.act(lambda e, t0=t0, n=n, d=d, c=c, aa=aa: e.activation(
                            aa[:, t0:t0 + n], rtl[:, 0:n], AF.Exp, scale=sdec[:, jj, d * 8 + c:d * 8 + c + 1]),
                            ["rtl", "sdec"], tk(xrn, i))
                        P.act(lambda e, n=n, d=d, c=c: e.activation(
                            e2[:, 0:n], rtl[:, 0:n], AF.Exp, scale=sdec2[:, jj, d * 8 + c:d * 8 + c + 1]),
                            ["rtl", "sdec2"], ["e2"])
                        P.act(lambda e, n=n: e.activation(e2[:, 0:n], e2[:, 0:n], AF.Sqrt, bias=1.0, scale=-1.0),
                              ["e2"], ["e2"])
                        P.pool(lambda e, t0=t0, n=n: e.tensor_tensor(
                            itl[:, 0:n], itl[:, 0:n], xc[:, t0:t0 + n], ALU.mult), ["itl", "xc"], ["itl"])
                        P.dve(lambda e, t0=t0, n=n: e.tensor_tensor(
                            bb[:, t0:t0 + n], itl[:, 0:n], e2[:, 0:n], ALU.mult), ["itl", "e2"], tk("bb", i))

                def SCAN(c, d):
                    aa, xrn = xrs[c % 2], "xr%d" % (c % 2)
                    if d == 0:
                        P.dve(lambda e: e.tensor_tensor_scan(hs[:], aa[:], bb[:], 0.0, ALU.mult, ALU.add),
                              [xrn, "bb"], ["hs"])
                    else:
                        P.dve(lambda e: e.tensor_tensor_scan(
                            bb[:, NCTX - 1::-1], aa[:, NCTX - 1::-1], bb[:, NCTX - 1::-1], 0.0, ALU.mult, ALU.add),
                            [xrn, "bb"], ["bb"])
                        P.dve(lambda e: e.tensor_tensor_scan(
                            bb[:, T - 1:NCTX - 1:-1], aa[:, T - 1:NCTX - 1:-1], bb[:, T - 1:NCTX - 1:-1],
                            bb[:, 0:1], ALU.mult, ALU.add), [xrn, "bb"], ["bb"])
                        P.pool(lambda e: e.tensor_tensor(hs[:], hs[:], bb[:], ALU.add), ["hs", "bb"], ["hs"])

                def OUT(c):
                    gl, gln = gls[c % 2], "gl%d" % (c % 2)
                    P.pool(lambda e, gl=gl: e.tensor_tensor(xcb[:], hs[:], gl[:], ALU.mult), ["hs", gln], ["xcb"])
                    P.dma("sp", uT_d[c * 128:(c + 1) * 128, :], xcb[:], ["xcb"], ["uT_d"], "ub")

                LOADW(0)
                PROJ(0, 0, 9)
                for c in range(8):
                    if c + 1 < 8:
                        LOADW(c + 1)
                    CONV(c)
                    GATES(c, 0)
                    if c + 1 < 8:
                        PROJ(c + 1, 0, 5)
                    SCAN(c, 0)
                    GATES(c, 1)
                    if c + 1 < 8:
                        PROJ(c + 1, 5, 9)
                    SCAN(c, 1)
                    OUT(c)
                P.barrier()
                P.emit()
            if stop_after == ("r2", l):
                return
            post_mixer(l, 0, rg_w_out_d[jj], 8, last)

        def attention(l):
            xsrc = xs_d
            with ExitStack() as st:
                QT = P.sb(st, "QT", [128, 8, T], BF16)
                KT = P.sb(st, "KT", [128, 2, T], BF16)
                Vt = P.sb(st, "Vt", [128, 34, 256], BF16)
                with ExitStack() as st1:
                    cm = alloc_common(st1, 1)
                    hb = P.sb(st1, "hb", [128, 8, 512], BF16)
                    wq = P.sb(st1, "wq", [128, 8, 1536], BF16)
                    cosT = P.sb(st1, "cosT", [128, 512], F32)
                    sinT = P.sb(st1, "sinT", [128, 512], F32)
                    rotm = P.sb(st1, "rotm", [128, 128], BF16)
                    sqhs = [P.sb(st1, "sqh%d" % i, [128, 512], BF16) for i in range(2)]
                    rths = [P.sb(st1, "rth%d" % i, [128, 512], F32) for i in range(2)]
                    qns = [P.sb(st1, "qn%d" % i, [128, 512], BF16) for i in range(2)]
                    t1s = [P.sb(st1, "t1%d" % i, [128, 512], F32) for i in range(2)]
                    t2s = [P.sb(st1, "t2%d" % i, [128, 512], F32) for i in range(2)]
                    P.dma("pool", wq[:], at_w_qkv_d.rearrange("(kc p) n -> p kc n", p=128), [], ["wq"], "wq")
                    P.dma("pool", rotm[:], rm_d, [], ["rotm"], "rotm")
                    for i, (t0, n, j) in enumerate(TILES):
                        xt, xk = cm["xt"][0], ("xt", 0)
                        load_x(xt, xk, xsrc, t0, n, [("xs", t0)])
                        if j == 0:
                            P.dma("sp", cosT[:], cos_d[:, t0 - NCTX:t0 - NCTX + 512], [], ["cs"], "cosT")
                            P.dma("sp", sinT[:], sin_d[:, t0 - NCTX:t0 - NCTX + 512], [], ["cs"], "sinT")
                        norm_mod((cm["sq"], "sq", cm["rt"], "rt", cm["tmp"], "tmp"), xt, xk, n, l, j, gsA, 0,
                                 hb, "hb")
                        for hh in range(10):
                            pq, pkq = pb[hh % 2], ("pb", hh % 2)
                            hp = hh % 2
                            sqh, rth, qn, t1, t2 = sqhs[hp], rths[hp], qns[hp], t1s[hp], t2s[hp]
                            sqk, rthk, qnk, t1k, t2k = ("sqh", hp), ("rth", hp), ("qn", hp), ("t1", hp), ("t2", hp)
                            pss, pks = pb[2 + hp], ("pb", 2 + hp)
                            prr, pkr = pb[4 + hp], ("pb", 4 + hp)
                            for kc in range(8):
                                P.pe(lambda e, pq=pq, kc=kc, hh=hh, n=n: e.matmul(
                                    pq[:, 0:n], wq[:, kc, hh * 128:(hh + 1) * 128], hb[:, kc, 0:n],
                                    start=(kc == 0), stop=(kc == 7)), ["wq", "hb"], [pkq])
                            P.act(lambda e, pq=pq, n=n, sqh=sqh: e.activation(sqh[:, 0:n], pq[:, 0:n], AF.Square),
                                  [pkq], [sqk])
                            P.pe(lambda e, n=n, sqh=sqh, pss=pss: e.matmul(pss[:, 0:n], ones_bf[:], sqh[:, 0:n],
                                                                         start=True, stop=True),
                                 [sqk, "ones"], [pks])
                            P.act(lambda e, n=n, rth=rth, pss=pss: e.activation(
                                rth[:, 0:n], pss[:, 0:n], AF.Sqrt, bias=epsc[:], scale=1.0 / 128),
                                [pks, "epsc"], [rthk])
                            P.dve(lambda e, n=n, rth=rth: e.reciprocal(rth[:, 0:n], rth[:, 0:n]), [rthk], [rthk])
                            gvec = V("qg") if hh < 8 else V("kg")
                            dst = QT[:, hh, t0:t0 + n] if hh < 8 else KT[:, hh - 8, t0:t0 + n]
                            dk = ("QT", hh) if hh < 8 else ("KT", hh - 8)
                            dkw = [dk, "QT" if hh < 8 else "KT"]
                            if j == 1:
                                P.dve(lambda e, pq=pq, n=n, gvec=gvec, dst=dst, rth=rth: e.scalar_tensor_tensor(
                                    dst, pq[:, 0:n], gvec, rth[:, 0:n], ALU.mult, ALU.mult),
                                    [pkq, rthk, "vecs"], dkw)
                            else:
                                P.dve(lambda e, pq=pq, n=n, gvec=gvec, rth=rth, qn=qn: e.scalar_tensor_tensor(
                                    qn[:, 0:n], pq[:, 0:n], gvec, rth[:, 0:n], ALU.mult, ALU.mult),
                                    [pkq, rthk, "vecs"], [qnk])
                                P.pe(lambda e, n=n, qn=qn, prr=prr: e.matmul(prr[:, 0:n], rotm[:], qn[:, 0:n],
                                                                           start=True, stop=True),
                                     [qnk, "rotm"], [pkr])
                                P.pool(lambda e, n=n, qn=qn, t1=t1: e.tensor_tensor(
                                    t1[:, 0:n], qn[:, 0:n], cosT[:, 0:n], ALU.mult), [qnk, "cs"], [t1k])
                                P.dve(lambda e, n=n, t2=t2, prr=prr: e.tensor_tensor(
                                    t2[:, 0:n], prr[:, 0:n], sinT[:, 0:n], ALU.mult), [pkr, "cs"], [t2k])
                                P.pool(lambda e, n=n, dst=dst, t1=t1, t2=t2: e.tensor_tensor(
                                    dst, t1[:, 0:n], t2[:, 0:n], ALU.add), [t1k, t2k], dkw)
                        for s in range(n // 128):
                            kt = t0 // 128 + s
                            pv, pkv = pb[6], ("pb", 6)
                            for kc in range(8):
                                P.pe(lambda e, pv=pv, kc=kc, s=s: e.matmul(
                                    pv[:, 0:256], hb[:, kc, s * 128:(s + 1) * 128], wq[:, kc, 1280:1536],
                                    start=(kc == 0), stop=(kc == 7)), ["wq", "hb"], [pkv])
                            P.act(lambda e, pv=pv, kt=kt: e.activation(Vt[:, kt, :], pv[:, 0:256], AF.Identity),
                                  [pkv], ["Vt"])
                    P.barrier()
                    P.emit()
                pT = [P.sb(st, "pT%d" % i, [128, 512], BF16) for i in range(4)]
                oT = [P.sb(st, "oT%d" % i, [128, 8, 512], BF16) for i in range(2)]
                rd = P.sb(st, "rd", [128, 512], F32)
                accs = [P.sb(st, "acc%d" % i, [128, 512], F32) for i in range(2)]
                accb = P.sb(st, "accb", [128, 512], F32)
                ones_f = P.sb(st, "ones_f", [128, 128], F32)
                P.pool(lambda e: e.memset(ones_f[:], 1.0), [], ["ones_f"])
                SC = 128.0 ** -0.5
                DEPTH = 3
                jobs = []
                for i, (t0, n, j) in enumerate(TILES):
                    for hq in range(8):
                        jobs.append(dict(i=i, t0=t0, n=n, j=j, hq=hq, kv=hq // 4, nkt=(2 if j == 1 else 34),
                                         ot=oT[i % 2], ok=("oT", i % 2),
                                         pO=pb[4 + hq % 2], pkO=("pb", 4 + hq % 2),
                                         pD=pb[6 + hq % 2], pkD=("pb", 6 + hq % 2)))

                def SE(J, kt):
                    pS, pkS = pb[kt % 4], ("pb", kt % 4)
                    ptt, ptk = pT[kt % 4], ("pT", kt % 4)
                    n, t0, kv, hq = J["n"], J["t0"], J["kv"], J["hq"]
                    P.pe(lambda e: e.matmul(
                        pS[:, 0:n], KT[:, kv, kt * 128:(kt + 1) * 128], QT[:, hq, t0:t0 + n],
                        start=True, stop=True), ["QT", "KT"], [pkS])
                    P.act(lambda e: e.activation(ptt[:, 0:n], pS[:, 0:n], AF.Exp, scale=SC), [pkS], [ptk])

                def VV(J, kt):
                    ptt, ptk = pT[kt % 4], ("pT", kt % 4)
                    n, kv, nkt, pO, pkO = J["n"], J["kv"], J["nkt"], J["pO"], J["pkO"]
                    P.pe(lambda e: e.matmul(
                        pO[:, 0:n], Vt[:, kt, kv * 128:(kv + 1) * 128], ptt[:, 0:n],
                        start=(kt == 0), stop=(kt == nkt - 1)), ["Vt", ptk], [pkO])
                    acc, acck = accs[kt % 2], ("acc", kt % 2)
                    if kt < 2:
                        P.dve(lambda e: e.tensor_copy(acc[:, 0:n], ptt[:, 0:n]), [ptk], [acck])
                    else:
                        P.dve(lambda e: e.tensor_tensor(acc[:, 0:n], acc[:, 0:n], ptt[:, 0:n], ALU.add),
                              [ptk, acck], [acck])

                def PRO(J):
                    for kt in range(min(DEPTH, J["nkt"])):
                        SE(J, kt)

                def BODY(J):
                    for kt in range(J["nkt"]):
                        if kt + DEPTH < J["nkt"]:
                            SE(J, kt + DEPTH)
                        VV(J, kt)
                    n = J["n"]
                    P.dve(lambda e: e.tensor_tensor(accb[:, 0:n], accs[0][:, 0:n], accs[1][:, 0:n], ALU.add),
                          [("acc", 0), ("acc", 1)], ["accb"])

                def EPI(J):
                    n, pD, pkD, pO, pkO, hq, ot, ok = (J["n"], J["pD"], J["pkD"], J["pO"], J["pkO"], J["hq"],
                                                       J["ot"], J["ok"])
                    P.pe(lambda e: e.matmul(pD[:, 0:n], ones_f[:], accb[:, 0:n], start=True, stop=True),
                         ["ones_f", "accb"], [pkD])
                    P.dve(lambda e: e.reciprocal(rd[:, 0:n], pD[:, 0:n]), [pkD], ["rd"])
                    P.dve(lambda e: e.tensor_tensor(ot[:, hq, 0:n], pO[:, 0:n], rd[:, 0:n], ALU.mult),
                          [pkO, "rd"], [ok])
                    if hq == 7:
                        t0 = J["t0"]
                        P.dma("sp", fm(uT_d)[:, 0:8, t0:t0 + n], ot[:, :, 0:n], [ok], ["uT_d"], ok)

                PRO(jobs[0])
                for k_, J in enumerate(jobs):
                    BODY(J)
                    if k_ + 1 < len(jobs):
                        PRO(jobs[k_ + 1])
                    EPI(J)
                P.barrier()
                P.emit()
            post_mixer(l, 0, at_w_o_d, 8, False)

        def gmlp(l):
            xsrc = xs_d
            with ExitStack() as st:
                cm = alloc_common(st, 1)
                hb = P.sb(st, "hb", [128, 8, 512], BF16)
                win = P.sb(st, "cwin", [128, 8, 4096], BF16)
                wsT = P.sb(st, "wsT", [128, 8, 128], BF16)
                lng = P.sb(st, "lng", [128, 2048], BF16)
                lnb = P.sb(st, "lnb", [128, 2048], BF16)
                bsr = P.sb(st, "bsr", [128, 16, 128], F32)
                uTs = [P.sb(st, "uTt%d" % i, [128, 16, 512], BF16) for i in range(2)]
                vv = P.sb(st, "vv", [128, 2048], F32)
                vnb = P.sb(st, "vnb", [128, 2048], BF16)
                junk = P.sb(st, "junk", [128, 512], BF16)
                sums = P.sb(st, "sums", [128, 8], F32)
                stt = P.sb(st, "stt", [128, 4], F32)
                mtmp = P.sb(st, "mtmp", [128, 4, 128], F32)
                cwv = cm_w_in_d.rearrange("(kc p) n -> p kc n", p=128)
                for q4 in range(4):
                    P.dma("pool", win[:, :, q4 * 1024:(q4 + 1) * 1024], cwv[:, :, q4 * 1024:(q4 + 1) * 1024],
                          [], ["cwin"], "cwin")
                P.dma("pool", wsT[:], cm_w_sT_d, [], ["wsT"], "wsT")
                P.dma("pool", lng[:], cm_lng_d, [], ["ln"], "lng")
                P.dma("pool", lnb[:], cm_lnb_d, [], ["ln"], "lnb")
                P.dma("sp", bsr[:], cm_bs_d, [], ["bsr"], "bsr")
                for i, (t0, n, j) in enumerate(TILES):
                    xt, xk = cm["xt"][0], ("xt", 0)
                    uTt, utk = uTs[i % 2], ("uTt", i % 2)
                    load_x(xt, xk, xsrc, t0, n, [("xs", t0)])
                    norm_mod((cm["sq"], "sq", cm["rt"], "rt", cm["tmp"], "tmp"), xt, xk, n, l, j, gsA, 0,
                             hb, "hb")
                    for uc in range(16):
                        pu, pku = pb[uc % 2], ("pb", uc % 2)
                        for kc in range(8):
                            P.pe(lambda e, pu=pu, kc=kc, uc=uc, n=n: e.matmul(
                                pu[:, 0:n], win[:, kc, uc * 128:(uc + 1) * 128], hb[:, kc, 0:n],
                                start=(kc == 0), stop=(kc == 7)), ["cwin", "hb"], [pku])
                        P.act(lambda e, pu=pu, uc=uc, n=n, uTt=uTt: e.activation(
                            uTt[:, uc, 0:n], pu[:, 0:n], AF.Gelu_apprx_tanh), [pku], [utk])
                    for s in range(n // 128):
                        for nb in range(4):
                            pv, pkv = pb[2 + nb % 2], ("pb", 2 + nb % 2)
                            for kc in range(8):
                                P.pe(lambda e, pv=pv, kc=kc, s=s, nb=nb: e.matmul(
                                    pv[:, :], hb[:, kc, s * 128:(s + 1) * 128],
                                    win[:, kc, 2048 + nb * 512:2048 + (nb + 1) * 512],
                                    start=(kc == 0), stop=(kc == 7)), ["cwin", "hb"], [pkv])
                            P.act(lambda e, pv=pv, nb=nb: e.activation(
                                vv[:, nb * 512:(nb + 1) * 512], pv[:, :], AF.Gelu_apprx_tanh,
                                accum_out=sums[:, nb:nb + 1]), [pkv], ["vv", "sums"])
                            P.act(lambda e, nb=nb: e.activation(
                                junk[:], vv[:, nb * 512:(nb + 1) * 512], AF.Square,
                                accum_out=sums[:, 4 + nb:5 + nb]), ["vv"], ["junk", "sums"])
                        P.dve(lambda e: e.tensor_reduce(stt[:, 0:1], sums[:, 0:4], AX.X, ALU.add), ["sums"], ["stt"])
                        P.dve(lambda e: e.tensor_reduce(stt[:, 1:2], sums[:, 4:8], AX.X, ALU.add), ["sums"], ["stt"])
                        P.dve(lambda e: e.tensor_scalar(stt[:, 0:2], stt[:, 0:2], 1.0 / 2048, None, ALU.mult),
                              ["stt"], ["stt"])
                        P.dve(lambda e: e.tensor_tensor(stt[:, 2:3], stt[:, 0:1], stt[:, 0:1], ALU.mult),
                              ["stt"], ["stt"])
                        P.dve(lambda e: e.tensor_tensor(stt[:, 1:2], stt[:, 1:2], stt[:, 2:3], ALU.subtract),
                              ["stt"], ["stt"])
                        P.act(lambda e: e.activation(stt[:, 1:2], stt[:, 1:2], AF.Sqrt, bias=epsc[:]),
                              ["stt", "epsc"], ["stt"])
                        P.dve(lambda e: e.reciprocal(stt[:, 1:2], stt[:, 1:2]), ["stt"], ["stt"])
                        P.dve(lambda e: e.tensor_scalar(vv[:], vv[:], stt[:, 0:1], stt[:, 1:2], ALU.subtract, ALU.mult),
                              ["vv", "stt"], ["vv"])
                        P.pool(lambda e: e.tensor_tensor(vv[:], vv[:], lng[:], ALU.mult), ["vv", "ln"], ["vv"])
                        P.dve(lambda e: e.tensor_tensor(vnb[:], vv[:], lnb[:], ALU.add), ["vv", "ln"], ["vnb"])
                        for q4 in range(4):
                            pm, pkm = pb[4 + q4 % 2], ("pb", 4 + q4 % 2)
                            for cc in range(4):
                                ch = q4 * 4 + cc
                                P.pe(lambda e, pm=pm, cc=cc, ch=ch: e.matmul(
                                    pm[:, cc * 128:(cc + 1) * 128], vnb[:, ch * 128:(ch + 1) * 128], wsT[:, ch // 2, :],
                                    start=True, stop=True), ["vnb", "wsT"], [pkm])
                            P.dve(lambda e, pm=pm, q4=q4: e.tensor_tensor(
                                mtmp[:], pm[:, :].rearrange("p (c t) -> p c t", c=4), bsr[:, q4 * 4:(q4 + 1) * 4, :],
                                ALU.add), [pkm, "bsr"], ["mtmp"])
                            P.pool(lambda e, q4=q4, s=s, uTt=uTt: e.tensor_tensor(
                                uTt[:, q4 * 4:(q4 + 1) * 4, s * 128:(s + 1) * 128], mtmp[:],
                                uTt[:, q4 * 4:(q4 + 1) * 4, s * 128:(s + 1) * 128], ALU.mult),
                                ["mtmp", utk], [utk])
                    P.dma("sp", fm(uT_d)[:, :, t0:t0 + n], uTt[:, :, 0:n], [utk], ["uT_d"], utk)
                P.barrier()
                P.emit()
            post_mixer(l, 0, cm_w_out_d, 16, False)

        def moe(l, last):
            ranges = [(256, 2304), (2304, 4352)] if last else [(0, 2176), (2176, 4352)]
            with ExitStack() as st:
                hfh = P.sb(st, "hfh", [128, 8, 2176], BF16)
                yacc = P.sb(st, "yacc", [128, 8, 2176], F32)
                wgs = [P.sb(st, "mwg%d" % i, [128, 8, 512], BF16) for i in range(2)]
                wus = [P.sb(st, "mwu%d" % i, [128, 8, 512], BF16) for i in range(2)]
                wds = [P.sb(st, "mwd%d" % i, [128, 4, 1024], BF16) for i in range(2)]
                wbs = [P.sb(st, "mwb0", [128, 2176], F32)] * 2
                sg = [P.sb(st, "msg%d" % i, [128, 512], F32) for i in range(2)]
                tt = [P.sb(st, "mtt%d" % i, [128, 512], F32) for i in range(2)]
                ab = [P.sb(st, "mab%d" % i, [128, 4, 512], BF16) for i in range(2)]
                xt2 = P.sb(st, "mxt", [128, 8, 256], F32)
                it = 0
                for (r0, r1) in ranges:
                    nt = r1 - r0
                    subt = []
                    o = 0
                    while o < nt:
                        subt.append((o, min(512, nt - o)))
                        o += 512
                    P.dma("sp", hfh[:, :, 0:nt], fm(hfT_d)[:, :, r0:r1], [("hfT", t0) for t0, _, _ in TILES],
                          ["hfh"], "hfh")
                    for c8 in range(8):
                        P.pool(lambda e, c8=c8: e.memset(yacc[:, c8, :], 0.0), [], ["yacc"])
                    for ex in range(32):
                        s2 = it % 2
                        wk = ("mw", s2)
                        wgt, wut, wdt, wbt = wgs[s2], wus[s2], wds[s2], wbs[s2]
                        P.dma("pool", wgt[:], moe_wg_d[l, ex].rearrange("(kc p) n -> p kc n", p=128), [], [wk], wk)
                        P.dma("pool", wut[:], moe_wu_d[l, ex].rearrange("(kc p) n -> p kc n", p=128), [], [wk], wk)
                        P.dma("pool", wdt[:], moe_wd_d[l, ex].rearrange("(kc p) n -> p kc n", p=128), [], [wk], wk)
                        wbk = ("mwb", 0)
                        P.dma("sp", wbt[:, 0:nt], wcT_d[ex, r0:r1].partition_broadcast(128),
                              ["wcT_d"], [wbk], wbk)
                        for ti, (o, n) in enumerate(subt):
                            abt, abk = ab[ti % 2], ("mab", ti % 2)
                            for jc in range(4):
                                pG, pkG = pb[jc % 2], ("pb", jc % 2)
                                pU, pkU = pb[2 + jc % 2], ("pb", 2 + jc % 2)
                                for kc in range(8):
                                    P.pe(lambda e, pG=pG, kc=kc, jc=jc, o=o, n=n, wgt=wgt: e.matmul(
                                        pG[:, 0:n], wgt[:, kc, jc * 128:(jc + 1) * 128], hfh[:, kc, o:o + n],
                                        start=(kc == 0), stop=(kc == 7)), [wk, "hfh"], [pkG])
                                for kc in range(8):
                                    P.pe(lambda e, pU=pU, kc=kc, jc=jc, o=o, n=n, wut=wut: e.matmul(
                                        pU[:, 0:n], wut[:, kc, jc * 128:(jc + 1) * 128], hfh[:, kc, o:o + n],
                                        start=(kc == 0), stop=(kc == 7)), [wk, "hfh"], [pkU])
                                sgt, sgk = sg[jc % 2], ("msg", jc % 2)
                                ttt, ttk = tt[jc % 2], ("mtt", jc % 2)
                                P.act(lambda e, pG=pG, sgt=sgt, n=n: e.activation(sgt[:, 0:n], pG[:, 0:n], AF.Silu),
                                      [pkG], [sgk])
                                P.dve(lambda e, pU=pU, sgt=sgt, ttt=ttt, n=n: e.tensor_tensor(
                                    ttt[:, 0:n], sgt[:, 0:n], pU[:, 0:n], ALU.mult), [sgk, pkU], [ttk])
                                P.pool(lambda e, ttt=ttt, abt=abt, jc=jc, o=o, n=n, wbt=wbt: e.tensor_tensor(
                                    abt[:, jc, 0:n], ttt[:, 0:n], wbt[:, o:o + n], ALU.mult), [ttk, wbk], [abk])
                            for co in range(8):
                                pD, pkD = pb[4 + co % 4], ("pb", 4 + co % 4)
                                for kc in range(4):
                                    P.pe(lambda e, pD=pD, kc=kc, co=co, n=n, wdt=wdt, abt=abt: e.matmul(
                                        pD[:, 0:n], wdt[:, kc, co * 128:(co + 1) * 128], abt[:, kc, 0:n],
                                        start=(kc == 0), stop=(kc == 3)), [wk, abk], [pkD])
                                P.dve(lambda e, pD=pD, co=co, o=o, n=n: e.tensor_tensor(
                                    yacc[:, co, o:o + n], yacc[:, co, o:o + n], pD[:, 0:n], ALU.add),
                                    [pkD, "yacc"], ["yacc"])
                        it += 1
                    if ydbg_d is not None and r0 == ranges[0][0] and l == nlayers - 1:
                        P.dma("sp", fm(ydbg_d)[:, :, 0:nt], yacc[:, :, 0:nt], ["yacc"], ["ydbg_d"], "yacc")
                    for ti, (o, n) in enumerate([(oo, min(256, nt - oo)) for oo in range(0, nt, 256)]):
                        a0 = r0 + o
                        P.dma("sp", xt2[:, :, 0:n], fm(xs_d)[:, :, a0:a0 + n], [("xs", t0) for t0, _, _ in TILES],
                              ["mxt"], "mxt")
                        segs = []
                        if a0 < NCTX:
                            segs.append((0, min(n, NCTX - a0), 1))
                            if a0 + n > NCTX:
                                segs.append((NCTX - a0, n, 0))
                        else:
                            segs.append((0, n, 0))
                        for (q0, q1, j) in segs:
                            for c in range(8):
                                P.dve(lambda e, c=c, q0=q0, q1=q1, j=j, o=o: e.scalar_tensor_tensor(
                                    xt2[:, c, q0:q1], yacc[:, c, o + q0:o + q1], modT[:, l, 40 + c, j:j + 1],
                                    xt2[:, c, q0:q1], ALU.mult, ALU.add), ["yacc", "mxt", "modT"], ["mxt"])
                        if last:
                            P.dma("pool", fm(outT_d)[:, :, a0 - NCTX:a0 - NCTX + n], xt2[:, :, 0:n], ["mxt"],
                                  ["out_d"], "mxt_st")
                        else:
                            P.dma("pool", fm(xs_d)[:, :, a0:a0 + n], xt2[:, :, 0:n], ["mxt"],
                                  [("xs", t0) for t0, _, _ in TILES], "mxt_st")
                P.barrier()
                P.emit()

        def moe_sparse(l, last):
            subtiles = list(range(2, 34)) if last else list(range(34))
            NB = 48 if last else NBLK
            with ExitStack() as st:
                hfts = [P.sb(st, "dhft%d" % i, [128, 1024], BF16) for i in range(2)]
                for ii, g in enumerate(subtiles):
                    ht, hk = hfts[ii % 2], ("dhft", ii % 2)
                    P.dma("sp", ht[:], hftok_d[g * 128:(g + 1) * 128, :], ["hftok_d"], [hk], hk)
                    for sl in range(2):
                        P.idma(xrows_d, desti[:, sl, g:g + 1], ht[:], None, [hk, "desti", "xrows_d"],
                               [("xrows_sc", ii % 2, sl)], ("dsc", ii % 2, sl))
                P.barrier()
                wgs = [P.sb(st, "mwg%d" % i, [128, 8, 512], BF16) for i in range(3)]
                wus = [P.sb(st, "mwu%d" % i, [128, 8, 512], BF16) for i in range(3)]
                wds = [P.sb(st, "mwd%d" % i, [128, 4, 1024], BF16) for i in range(3)]
                xbs = [P.sb(st, "mxb%d" % i, [128, 4, 1024], BF16) for i in range(2)]
                xbT = [P.sb(st, "mxbT%d" % i, [128, 8, 512], BF16) for i in range(2)]
                sg = [P.sb(st, "msg%d" % i, [128, 512], F32) for i in range(2)]
                ab = [P.sb(st, "mab%d" % i, [128, 4, 512], BF16) for i in range(2)]
                yb = [P.sb(st, "myb%d" % i, [128, 1024], F32) for i in range(2)]
                NOW = False

                def LOADS(bi):
                    s2 = bi % 2
                    s3 = bi % 3
                    wk = ("mw", s3)
                    wgt, wut, wdt = wgs[s3], wus[s3], wds[s3]
                    if not (NOW and bi >= 2):
                        for h in range(2):
                            ix = widx[:, bi, h:h + 1]
                            P.idma(wgt[:, 4 * h:4 * h + 4, :].rearrange("p a b -> p (a b)"), None, moe_wg_d, ix,
                                   ["widx"], [wk], wk)
                            P.idma(wut[:, 4 * h:4 * h + 4, :].rearrange("p a b -> p (a b)"), None, moe_wu_d, ix,
                                   ["widx"], [wk], wk)
                            P.idma(wdt[:, 2 * h:2 * h + 2, :].rearrange("p a b -> p (a b)"), None, moe_wd_d, ix,
                                   ["widx"], [wk], wk)
                    xb, xbk = xbs[s2], ("mxb", s2)
                    P.dma("sp", xb[:], xrows_d[bi * 512:(bi + 1) * 512, :].rearrange("(s p) d -> p s d", p=128),
                          ["xrows_d"], [xbk], xbk)

                def TR(bi):
                    s2 = bi % 2
                    xb, xbk = xbs[s2], ("mxb", s2)
                    xt_, xtk = xbT[s2], ("mxbT", s2)
                    for kc in range(8):
                        pX, pkX = (pb4b, ("pb", 4)) if kc % 2 == 0 else (pb5b, ("pb", 5))
                        for s_ in range(4):
                            P.pe(lambda e, pX=pX, s_=s_, kc=kc, xb=xb: e.transpose(
                                pX[:, s_ * 128:(s_ + 1) * 128], xb[:, s_, kc * 128:(kc + 1) * 128], identb[:]),
                                [xbk, "identb"], [pkX])
                        if kc % 2 == 0:
                            P.act(lambda e, pX=pX, kc=kc, xt_=xt_: e.activation(xt_[:, kc, :], pX[:, 0:512], AF.Identity),
                                  [pkX], [(xtk, kc)])
                        else:
                            P.dve(lambda e, pX=pX, kc=kc, xt_=xt_: e.tensor_copy(xt_[:, kc, :], pX[:, 0:512]),
                                  [pkX], [(xtk, kc)])

                def GU(bi):
                    s2 = bi % 2
                    s3 = bi % 3
                    wk = ("mw", s3)
                    wgt, wut = wgs[s3], wus[s3]
                    xt_, xtk = xbT[s2], ("mxbT", s2)
                    abt, abk = ab[s2], ("mab", s2)
                    for jc in range(4):
                        pG, pkG = pb[jc % 2], ("pb", jc % 2)
                        pU, pkU = pb[2 + jc % 2], ("pb", 2 + jc % 2)
                        for kc in range(8):
                            P.pe(lambda e, pG=pG, kc=kc, jc=jc, wgt=wgt, xt_=xt_: e.matmul(
                                pG[:, :], wgt[:, kc, jc * 128:(jc + 1) * 128], xt_[:, kc, :],
                                start=(kc == 0), stop=(kc == 7)), [wk, (xtk, kc)], [pkG])
                        for kc in range(8):
                            P.pe(lambda e, pU=pU, kc=kc, jc=jc, wut=wut, xt_=xt_: e.matmul(
                                pU[:, :], wut[:, kc, jc * 128:(jc + 1) * 128], xt_[:, kc, :],
                                start=(kc == 0), stop=(kc == 7)), [wk, (xtk, kc)], [pkU])
                        sgt, sgk = sg[jc % 2], ("msg", jc % 2)
                        P.act(lambda e, pG=pG, sgt=sgt: e.activation(sgt[:], pG[:, :], AF.Silu), [pkG], [sgk])
                        P.dve(lambda e, pU=pU, sgt=sgt, abt=abt, jc=jc: e.tensor_tensor(
                            abt[:, jc, :], sgt[:], pU[:, :], ALU.mult), [sgk, pkU], [(abk, jc)])

                def DN(bi):
                    s2 = bi % 2
                    s3 = bi % 3
                    wk = ("mw", s3)
                    wdt = wds[s3]
                    abt, abk = ab[s2], ("mab", s2)
                    for s_ in range(4):
                        ybt, ybk = yb[s_ % 2], ("myb", s_ % 2)
                        for hh in range(2):
                            pD, pkD = pb[6 + hh], ("pb", 6 + hh)
                            for kc in range(4):
                                P.pe(lambda e, pD=pD, kc=kc, s_=s_, hh=hh, wdt=wdt, abt=abt: e.matmul(
                                    pD[:, :], abt[:, kc, s_ * 128:(s_ + 1) * 128], wdt[:, kc, hh * 512:(hh + 1) * 512],
                                    start=(kc == 0), stop=(kc == 3)), [wk, (abk, kc)], [pkD])
                            if hh == 0:
                                P.act(lambda e, pD=pD, ybt=ybt: e.activation(ybt[:, 0:512], pD[:, :], AF.Identity),
                                      [pkD], [ybk])
                            else:
                                P.dve(lambda e, pD=pD, ybt=ybt: e.tensor_copy(ybt[:, 512:1024], pD[:, :]),
                                      [pkD], [ybk])
                        r0_ = bi * 512 + s_ * 128
                        P.dma("sp", yrows_d[r0_:r0_ + 128, :], ybt[:], [ybk], [("yrows_st", s_ % 2)], ybk)

                LOADS(0)
                LOADS(1)
                TR(0)
                for bi in range(NB):
                    if bi + 2 < NB:
                        LOADS(bi + 2)
                    GU(bi)
                    if bi + 1 < NB:
                        TR(bi + 1)
                    DN(bi)
                P.barrier()
                P.emit()
            with ExitStack() as st:
                y1 = [P.sb(st, "cy1_%d" % i, [128, 1024], F32) for i in range(4)]
                y2 = [P.sb(st, "cy2_%d" % i, [128, 1024], F32) for i in range(4)]
                yt4s = [P.sb(st, "cyt4_%d" % i, [128, 4, 1024], F32) for i in range(2)]
                xt2s = [P.sb(st, "cxt%d" % i, [128, 8, 512], F32) for i in range(2)]
                tiles = TILES[1:] if last else TILES
                ii = 0
                for ti_, (t0, n, j) in enumerate(tiles):
                    xt2, cxk = xt2s[ti_ % 2], "cxt%d" % (ti_ % 2)
                    yt4, cyk = yt4s[ti_ % 2], "cyt4_%d" % (ti_ % 2)
                    P.dma("sp", xt2[:, :, 0:n], fm(xs_d)[:, :, t0:t0 + n], [("xs", t0)], [cxk], cxk)
                    for s_ in range(n // 128):
                        g = t0 // 128 + s_
                        a1, k1 = y1[ii % 4], ("cy1", ii % 4)
                        a2, k2 = y2[ii % 4], ("cy2", ii % 4)
                        ii += 1
                        P.idma(a1[:], None, yrows_d, desti[:, 0, g:g + 1], ["desti"], [k1], k1)
                        P.idma(a2[:], None, yrows_d, desti[:, 1, g:g + 1], ["desti"], [k2], k2)
                        P.dve(lambda e, a1=a1, s_=s_, g=g, yt4=yt4: e.tensor_scalar(
                            yt4[:, s_, :], a1[:], rinfo[:, 4, g:g + 1], None, ALU.mult), [k1], [(cyk, s_)])
                        P.dve(lambda e, a2=a2, s_=s_, g=g, yt4=yt4: e.scalar_tensor_tensor(
                            yt4[:, s_, :], a2[:], rinfo[:, 5, g:g + 1], yt4[:, s_, :], ALU.mult, ALU.add),
                            [k2, (cyk, s_)], [(cyk, s_)])
                    for c in range(8):
                        pY, pkY = pb[c % 4], ("pb", c % 4)
                        for s_ in range(n // 128):
                            P.pe(lambda e, pY=pY, s_=s_, c=c, yt4=yt4: e.transpose(
                                pY[:, s_ * 128:(s_ + 1) * 128], yt4[:, s_, c * 128:(c + 1) * 128], ident[:]),
                                [(cyk, s_), "ident"], [pkY])
                        P.dve(lambda e, pY=pY, c=c, n=n, j=j, xt2=xt2: e.scalar_tensor_tensor(
                            xt2[:, c, 0:n], pY[:, 0:n], modT[:, l, 40 + c, j:j + 1], xt2[:, c, 0:n], ALU.mult, ALU.add),
                            [pkY, cxk, "modT"], [(cxk, c)] + ([cxk] if c in (0, 7) else []))
                    if last:
                        P.dma("sp", fm(outT_d)[:, :, t0 - NCTX:t0 - NCTX + n], xt2[:, :, 0:n], [cxk], ["out_d"],
                              cxk + "_st")
                    else:
                        P.dma("sp", fm(xs_d)[:, :, t0:t0 + n], xt2[:, :, 0:n], [cxk], [("xs", t0)], cxk + "_st")
                P.barrier()
                P.emit()

        zt = P.sb(gst, "zt", [128, 1024], BF16)
        P.pool(lambda e: e.memset(zt[:], 0.0), [], ["zt"])
        for bi in range(NBLK * 4):
            P.dma("sp", xrows_d[bi * 128:(bi + 1) * 128, :], zt[:], ["zt"], ["xrows_d"], "zt")
        for l in range(nlayers):
            if stop_after == ("ada", l):
                break
            kind = l % 3
            last = (l == 3)
            if kind == 0:
                rglru(l, l // 3, last)
            elif kind == 1:
                attention(l)
            else:
                gmlp(l)
            if stop_after in (("mix", l), ("r2", l)):
                break
            (moe_sparse if SPARSE else moe)(l, last)
        if dbg_d is not None:
            P.dma("sp", modT_d, modT[:].rearrange("p a b c -> p (a b c)"), ["modT"], ["modT_d"], "modTd")
            with ExitStack() as st:
                dt_ = P.sb(st, "dbgt", [128, 8, 512], F32)
                for (t0, n, j) in TILES:
                    P.dma("sp", dt_[:, :, 0:n], fm(xs_d)[:, :, t0:t0 + n], [("xs", t) for t, _, _ in TILES], ["dbgt"], "dbgt")
                    P.dma("sp", fm(dbg_d)[:, :, t0:t0 + n], dt_[:, :, 0:n], ["dbgt"], ["dbg_d"], "dbgt2")
                P.barrier()
                P.emit()
        P.barrier()
        P.emit()
        print("instructions:", P.n_ins, "dma sems:", len(P.dsem))
    return nc


def _host_prep(inp):
    f = np.float32
    x = np.asarray(inp["x"], f)
    ctx = np.asarray(inp["ctx"], f)
    vec = np.zeros((128, NV), f)

    def put(name, arr):
        a = np.asarray(arr, f).reshape(-1, 8, 128)
        o = VOFF[name]
        vec[:, o:o + a.shape[0] * 8] = a.transpose(2, 0, 1).reshape(128, -1)
    for l in range(4):
        put(("gmix", l), inp["norm_mix_g"][l])
        put(("gffn", l), inp["norm_ffn_g"][l])
        o = VOFF[("adab", l)]
        vec[:, o:o + 48] = np.asarray(inp["ada_b"][l], f).reshape(48, 128).T
    for j in range(2):
        put(("convw", j), inp["rg_conv_w"][j])
        put(("convb", j), inp["rg_conv_b"][j])
        put(("ba", j), inp["rg_ba"][j])
        put(("bi", j), inp["rg_bi"][j])
        put(("lam", j), inp["rg_lambda"][j])
    vec[:, VOFF["qg"]] = np.asarray(inp["at_q_g"], f)[0]
    vec[:, VOFF["kg"]] = np.asarray(inp["at_k_g"], f)[0]
    wr = np.concatenate([np.asarray(inp["moe_w_group"], f),
                         np.asarray(inp["moe_w_router"], f).reshape(4, 1024, 32)], axis=2)
    wr = np.ascontiguousarray(wr.reshape(4, 8, 128, 36).transpose(0, 2, 1, 3))
    br = np.concatenate([np.asarray(inp["moe_b_group"], f), np.asarray(inp["moe_b_router"], f).reshape(4, 32)], axis=1)
    br = np.ascontiguousarray(np.broadcast_to(br[:, None, :], (4, 128, 36)))
    p = np.arange(128)
    axis = p // 64
    fr = p % 32
    inv = (10000.0 ** (-(fr.astype(np.float64)) * 2.0 / 64)).astype(f)
    t = np.arange(4096)
    pos = np.where(axis[:, None] == 0, (t // 64)[None, :], (t % 64)[None, :]).astype(f)
    ang = pos * inv[:, None]
    cosT = np.cos(ang).astype(f)
    sinT = np.sin(ang).astype(f)
    rotm = np.zeros((128, 128), f)
    for q in range(128):
        half = (q // 32) % 2
        if half == 0:
            rotm[q + 32, q] = -1.0
        else:
            rotm[q - 32, q] = 1.0
    def relay(w, kc, n):
        w = np.asarray(w, f).reshape(4, 32, kc, 128, n).transpose(0, 1, 3, 2, 4)
        return np.ascontiguousarray(w).reshape(4 * 32 * 128 * 2, 2048)
    hconst = np.zeros((128, 193), f)
    hconst[:, 0:128] = np.triu(np.ones((128, 128), f), 1)
    hconst[:, 128:160] = np.arange(32, dtype=f)[None, :]
    hconst[:, 160] = 2.0 * np.arange(128, dtype=f)
    hconst[:, 161:193] = 1.0
    shared = {
        "vecs": vec,
        "ada_w": np.ascontiguousarray(inp["ada_w"], f),
        "rg_w_in": np.ascontiguousarray(inp["rg_w_in"], f),
        "rg_wa": np.ascontiguousarray(inp["rg_wa"], f),
        "rg_wi": np.ascontiguousarray(inp["rg_wi"], f),
        "rg_w_out": np.ascontiguousarray(inp["rg_w_out"], f),
        "at_w_qkv": np.ascontiguousarray(inp["at_w_qkv"][0], f),
        "at_w_o": np.ascontiguousarray(inp["at_w_o"][0], f),
        "cm_w_in": np.ascontiguousarray(inp["cm_w_in"][0], f),
        "cm_w_sT": np.ascontiguousarray(np.asarray(inp["cm_w_s"][0], f).transpose(2, 0, 1)),
        "cm_w_out": np.ascontiguousarray(inp["cm_w_out"][0], f),
        "cm_lng": np.ascontiguousarray(np.broadcast_to(np.asarray(inp["cm_ln_g"][0], f)[None, :], (128, 2048))),
        "cm_lnb": np.ascontiguousarray(np.broadcast_to(np.asarray(inp["cm_ln_b"][0], f)[None, :], (128, 2048))),
        "cm_bs": np.ascontiguousarray(np.broadcast_to(
            np.repeat(np.asarray(inp["cm_b_s"][0], f), 2, axis=0)[None, :, :], (128, 16, 128))),
        "wr": wr, "br": br,
        "moe_w_gate": relay(inp["moe_w_gate"], 8, 512),
        "moe_w_up": relay(inp["moe_w_up"], 8, 512),
        "moe_w_down": relay(inp["moe_w_down"], 4, 1024),
        "hconst": hconst,
        "cosT": cosT, "sinT": sinT, "rotm": rotm, "ident": np.eye(128, dtype=f),
    }
    in_maps = []
    for k in range(8):
        b = k % 4
        xT = np.ascontiguousarray(np.concatenate([ctx[b], x[b]], axis=0).T)
        cT = np.stack([np.asarray(inp["c"], f)[b], np.asarray(inp["c_ctx"], f)], axis=1)
        cT = np.ascontiguousarray(cT.reshape(8, 128, 2).transpose(1, 0, 2))
        m = dict(shared)
        m["xT"] = xT
        m["cT"] = cT
        in_maps.append(m)
    return in_maps


_NC_CACHE = {}


def kernel(**inputs):
    in_maps = _host_prep(inputs)
    if "nc" not in _NC_CACHE:
        _NC_CACHE["nc"] = build()
    nc = _NC_CACHE["nc"]
    res = run_bass_kernel_spmd(nc, in_maps, core_ids=list(range(8)))
    out = np.stack([np.ascontiguousarray(res.results[b]["outT"].T) for b in range(4)], axis=0)
    return out.astype(np.float32)
```

```python
import numpy as np
from contextlib import ExitStack
import concourse.bass as bass
import concourse.mybir as mybir
from concourse.bass_utils import run_bass_kernel_spmd

F32 = mybir.dt.float32
BF16 = mybir.dt.bfloat16
AF = mybir.ActivationFunctionType
ALU = mybir.AluOpType
AX = mybir.AxisListType

SAME_ENGINE_SYNC = True
SPARSE = True
T = 4352
NCTX = 256
EPS = 1e-6
NBLK = 49
I32 = mybir.dt.int32
TILES = [(0, 256, 1)] + [(256 + 512 * i, 512, 0) for i in range(8)]


class Prog:
    ENG = ("pe", "act", "dve", "pool", "sp")

    def __init__(self, nc, stack):
        self.nc = nc
        self.stack = stack
        self.q = {e: [] for e in self.ENG}
        self.cnt = {e: 0 for e in self.ENG}
        self.esem = {e: stack.enter_context(nc.semaphore("s_" + e)) for e in self.ENG}
        self.known = {e: {} for e in self.ENG}
        self.state = {}
        self.dsem = {}
        self.dcnt = {}
        self.semobj = {}
        self.n_ins = 0

    def sb(self, st, name, shape, dt):
        self.n_sb = getattr(self, "n_sb", 0) + 1
        return st.enter_context(self.nc.sbuf_tensor("s%d_%s" % (self.n_sb, name), list(shape), dt))

    def _st(self, k):
        s = self.state.get(k)
        if s is None:
            s = self.state[k] = [None, []]
        return s

    def _need(self, eng, ev, skip_sem=None):
        if ev is None:
            return
        sem, val, src = ev
        if skip_sem is not None and sem is skip_sem:
            return
        if src == eng and (eng == "pe" or not SAME_ENGINE_SYNC):
            return
        kn = self.known[eng]
        if kn.get(id(sem), 0) >= val:
            return
        kn[id(sem)] = val
        self.q[eng].append(("w", sem, val))

    def _deps(self, eng, reads, writes, skip_sem=None):
        for k in reads:
            self._need(eng, self._st(k)[0])
        for k in writes:
            s = self._st(k)
            self._need(eng, s[0], skip_sem)
            for ev in s[1]:
                self._need(eng, ev)

    def _commit(self, ev, reads, writes):
        for k in reads:
            s = self._st(k)
            s[1].append(ev)
            if len(s[1]) > 64:
                s[1] = s[1][-64:]
        for k in writes:
            s = self._st(k)
            s[0] = ev
            s[1] = []

    def op(self, eng, fn, r=(), w=()):
        self._deps(eng, r, w)
        self.cnt[eng] += 1
        ev = (self.esem[eng], self.cnt[eng], eng)
        self.q[eng].append(("o", fn, self.esem[eng], 1))
        self._commit(ev, r, w)

    def pe(self, fn, r=(), w=()):
        self.op("pe", fn, r, w)

    def act(self, fn, r=(), w=()):
        self.op("act", fn, r, w)

    def dve(self, fn, r=(), w=()):
        self.op("dve", fn, r, w)

    def pool(self, fn, r=(), w=()):
        self.op("pool", fn, r, w)

    def dma(self, eng, out, in_, r, w, sbkey, **kw):
        sem = self.dsem.get(sbkey)
        if sem is None:
            sem = self.dsem[sbkey] = self.stack.enter_context(
                self.nc.semaphore("d%d" % len(self.dsem)))
            self.dcnt[sbkey] = 0
        self._deps(eng, r, w, skip_sem=sem)
        self.dcnt[sbkey] += 16
        ev = (sem, self.dcnt[sbkey], "dma")
        self.q[eng].append(("o", lambda e: e.dma_start(out=out, in_=in_, **kw), sem, 16))
        self._commit(ev, r, w)

    def idma(self, out, out_idx, in_, in_idx, r, w, sbkey):
        eng = "pool"
        sem = self.dsem.get(sbkey)
        if sem is None:
            sem = self.dsem[sbkey] = self.stack.enter_context(
                self.nc.semaphore("d%d" % len(self.dsem)))
            self.dcnt[sbkey] = 0
        self._deps(eng, r, w, skip_sem=sem)
        self.dcnt[sbkey] += 16
        ev = (sem, self.dcnt[sbkey], "dma")
        oo = None if out_idx is None else bass.IndirectOffsetOnAxis(out_idx, 0)
        io = None if in_idx is None else bass.IndirectOffsetOnAxis(in_idx, 0)
        self.q[eng].append(("o", lambda e: e.indirect_dma_start(out=out, out_offset=oo, in_=in_, in_offset=io),
                            sem, 16))
        self._commit(ev, r, w)

    def barrier(self):
        for e in self.ENG:
            for e2 in self.ENG:
                if e2 != e and self.cnt[e2] > 0:
                    self._need(e, (self.esem[e2], self.cnt[e2], e2))
            for k, sem in self.dsem.items():
                if self.dcnt[k] > 0:
                    self._need(e, (sem, self.dcnt[k], "dma"))

    def emit(self):
        nc = self.nc
        q = self.q

        def run(e, lst):
            for it in lst:
                if it[0] == "w":
                    e.wait_ge(it[1], it[2])
                else:
                    it[1](e).then_inc(it[2], it[3])
        self.n_ins += sum(len(v) for v in q.values())
        with nc.Block() as block:
            @block.tensor
            def _(e):
                run(e, q["pe"])

            @block.scalar
            def _(e):
                run(e, q["act"])

            @block.vector
            def _(e):
                run(e, q["dve"])

            @block.gpsimd
            def _(e):
                run(e, q["pool"])

            @block.sync
            def _(e):
                run(e, q["sp"])
        self.q = {e: [] for e in self.ENG}


def _vec_layout():
    off = {}
    n = 0

    def add(name, k):
        nonlocal n
        off[name] = n
        n += k
    for l in range(4):
        add(("gmix", l), 8)
        add(("gffn", l), 8)
        add(("adab", l), 48)
    for j in range(2):
        add(("convw", j), 32)
        add(("convb", j), 8)
        add(("ba", j), 16)
        add(("bi", j), 16)
        add(("lam", j), 16)
    add("qg", 1)
    add("kg", 1)
    return off, n


VOFF, NV = _vec_layout()


def build(nlayers=4, stop_after=None, debug=False):
    nc = bass.Bass("TRN2", target_bir_lowering=False)

    def din(name, shape, dt=F32):
        return nc.dram_tensor(name, list(shape), dt, kind="ExternalInput").ap()

    xT_d = din("xT", [1024, T])
    cT_d = din("cT", [128, 8, 2])
    vecs_d = din("vecs", [128, NV])
    ada_w_d = din("ada_w", [4, 1024, 6144])
    rg_w_in_d = din("rg_w_in", [2, 1024, 2048])
    rg_wa_d = din("rg_wa", [2, 2, 8, 128, 128])
    rg_wi_d = din("rg_wi", [2, 2, 8, 128, 128])
    rg_w_out_d = din("rg_w_out", [2, 1024, 1024])
    at_w_qkv_d = din("at_w_qkv", [1024, 1536])
    at_w_o_d = din("at_w_o", [1024, 1024])
    cm_w_in_d = din("cm_w_in", [1024, 4096])
    cm_w_sT_d = din("cm_w_sT", [128, 8, 128])
    cm_w_out_d = din("cm_w_out", [2048, 1024])
    cm_lng_d = din("cm_lng", [128, 2048])
    cm_lnb_d = din("cm_lnb", [128, 2048])
    cm_bs_d = din("cm_bs", [128, 16, 128])
    wr_d = din("wr", [4, 128, 8, 36])
    br_d = din("br", [4, 128, 36])
    moe_wg_d = din("moe_w_gate", [4 * 32 * 128 * 2, 2048])
    moe_wu_d = din("moe_w_up", [4 * 32 * 128 * 2, 2048])
    moe_wd_d = din("moe_w_down", [4 * 32 * 128 * 2, 2048])
    hconst_d = din("hconst", [128, 193])
    cos_d = din("cosT", [128, 4096])
    sin_d = din("sinT", [128, 4096])
    rm_d = din("rotm", [128, 128])
    ident_d = din("ident", [128, 128])
    outT_d = nc.dram_tensor("outT", [1024, 4096], F32, kind="ExternalOutput").ap()
    skind = "ExternalOutput" if debug else "Internal"
    xs_d = nc.dram_tensor("xs", [1024, T], F32, kind=skind).ap()
    uT_d = nc.dram_tensor("uT", [2048, T], BF16, kind=skind).ap()
    hfT_d = nc.dram_tensor("hfT", [1024, T], BF16, kind=skind).ap()
    hftok_d = nc.dram_tensor("hftok", [T, 1024], BF16, kind=skind).ap()
    xrows_d = nc.dram_tensor("xrows", [NBLK * 512, 1024], BF16, kind="Internal").ap()
    yrows_d = nc.dram_tensor("yrows", [NBLK * 512, 1024], F32, kind="Internal").ap()
    wcT_d = nc.dram_tensor("wcT", [32, T], F32, kind=skind).ap()
    ydbg_d = nc.dram_tensor("ydbg", [1024, 2176], F32, kind="ExternalOutput").ap() if debug else None
    modT_d = nc.dram_tensor("modT_dbg", [128, 4 * 48 * 2], F32, kind="ExternalOutput").ap() if debug else None
    dbg_d = nc.dram_tensor("dbg", [1024, T], F32, kind="ExternalOutput").ap() if debug else None

    def fm(ap2d):
        return ap2d.rearrange("(c p) t -> p c t", p=128)

    with ExitStack() as gst:
        P = Prog(nc, gst)
        vecs = P.sb(gst, "vecs", [128, NV], F32)
        modT = P.sb(gst, "modT", [128, 4, 48, 2], F32)
        gsA = P.sb(gst, "gsA", [128, 4, 8, 2], F32)
        gsF = P.sb(gst, "gsF", [128, 4, 8, 2], F32)
        ones_bf = P.sb(gst, "ones_bf", [128, 128], BF16)
        ident = P.sb(gst, "ident", [128, 128], F32)
        epsc = P.sb(gst, "epsc", [128, 1], F32)
        sdec = P.sb(gst, "sdec", [128, 2, 16], F32)
        sdec2 = P.sb(gst, "sdec2", [128, 2, 16], F32)
        pb = [gst.enter_context(nc.psum_tensor("pb%d" % i, [128, 512], F32)) for i in range(8)]
        hconst = P.sb(gst, "hconst", [128, 193], F32)
        ustrict = P.sb(gst, "ustrict", [128, 128], BF16)
        identb = P.sb(gst, "identb", [128, 128], BF16)
        iota_e = hconst[:, 128:160]
        iota2p = hconst[:, 160:161]
        ones32 = hconst[:, 161:193]
        pb4b = pb[4][:, :].bitcast(BF16)
        pb5b = pb[5][:, :].bitcast(BF16)
        base = P.sb(gst, "base", [128, 32], F32)
        rinfo = P.sb(gst, "rinfo", [128, 6, 34], F32)
        desti = P.sb(gst, "desti", [128, 2, 34], I32)
        widx = P.sb(gst, "widx", [128, NBLK, 2], I32)

        def V(name, i=0, n=1):
            o = VOFF[name] + i
            return vecs[:, o:o + n]

        P.dma("sp", vecs[:], vecs_d, [], ["vecs"], "vecs")
        P.dma("sp", ident[:], ident_d, [], ["ident"], "ident")
        P.dma("sp", hconst[:], hconst_d, [], ["hconst"], "hconst")
        P.dma("pool", ustrict[:], hconst_d[:, 0:128], [], ["ustrict"], "ustrict")
        P.dma("pool", identb[:], ident_d, [], ["identb"], "identb")
        P.pool(lambda e: e.memset(ones_bf[:], 1.0), [], ["ones"])
        P.pool(lambda e: e.memset(modT[:], 0.0), [], ["modT"])
        P.pool(lambda e: e.memset(base[:], 0.0), [], ["base"])
        P.pool(lambda e: e.memset(rinfo[:], 0.0), [], ["rinfo"])
        P.pool(lambda e: e.memset(epsc[:], EPS), [], ["epsc"])

        with ExitStack() as st:
            cT = P.sb(st, "cT", [128, 8, 2], F32)
            cact = P.sb(st, "cact", [128, 8, 2], F32)
            wblk = [P.sb(st, "adaw%d" % i, [128, 8, 768], F32) for i in range(2)]
            P.dma("sp", cT[:], cT_d, [], ["cT"], "cT")
            P.act(lambda e: e.activation(cact[:], cT[:], AF.Silu), ["cT"], ["cact"])
            it = 0
            for l in range(nlayers):
                wv = ada_w_d[l].rearrange("(kc p) n -> p kc n", p=128)
                for nb in range(8):
                    wt = wblk[it % 2]
                    wk = ("adaw", it % 2)
                    P.dma("sp", wt[:], wv[:, :, nb * 768:(nb + 1) * 768], [], [wk], wk)
                    for o in range(6):
                        oc = nb * 6 + o
                        pk = ("pb", oc % 2)
                        pt = pb[oc % 2]
                        for kc in range(8):
                            P.pe(lambda e, pt=pt, wt=wt, o=o, kc=kc: e.matmul(
                                pt[:, 0:2], wt[:, kc, o * 128:(o + 1) * 128], cact[:, kc, :],
                                start=(kc == 0), stop=(kc == 7)), [wk, "cact"], [pk])
                        P.dve(lambda e, pt=pt, l=l, oc=oc: e.tensor_scalar(
                            modT[:, l, oc, :], pt[:, 0:2], V(("adab", l), oc), None, ALU.add),
                            [pk, "vecs"], ["modT"])
                    it += 1
                for j in range(2):
                    P.dve(lambda e, l=l, j=j: e.scalar_tensor_tensor(
                        gsA[:, l, :, j], modT[:, l, 8:16, j], 1.0, V(("gmix", l), 0, 8), ALU.add, ALU.mult),
                        ["modT", "vecs"], ["gs"])
                    P.dve(lambda e, l=l, j=j: e.scalar_tensor_tensor(
                        gsF[:, l, :, j], modT[:, l, 32:40, j], 1.0, V(("gffn", l), 0, 8), ALU.add, ALU.mult),
                        ["modT", "vecs"], ["gs"])
            for j in range(2):
                P.act(lambda e, j=j: e.activation(sdec[:, j, :], V(("lam", j), 0, 16), AF.Exp, scale=-1.0),
                      ["vecs"], ["sdec"])
                P.act(lambda e, j=j: e.activation(sdec2[:, j, :], sdec[:, j, :], AF.Ln, bias=1.0),
                      ["sdec"], ["sdec2"])
                P.dve(lambda e, j=j: e.tensor_scalar(sdec[:, j, :], sdec2[:, j, :], -8.0, None, ALU.mult),
                      ["sdec2"], ["sdec"])
                P.dve(lambda e, j=j: e.tensor_scalar(sdec2[:, j, :], sdec[:, j, :], 2.0, None, ALU.mult),
                      ["sdec"], ["sdec2"])
            P.barrier()
            P.emit()

        def load_x(xt, xk, xsrc, t0, n, rk):
            P.dma("sp", xt[:, :, 0:n], fm(xsrc)[:, :, t0:t0 + n], rk, [xk], xk)

        def norm_mod(wk_, xt, xk, n, l, j, gs, shift_m, hb, hk, h32=None, h32k=None):
            sq, sqk, rt, rtk, tmp, tmpk = wk_
            ends = lambda k, c: [(k, c)] + ([k] if c in (0, 7) else [])
            P.act(lambda e: e.activation(sq[:, :, 0:n], xt[:, :, 0:n], AF.Square), [xk], [sqk])
            for c in range(8):
                P.pe(lambda e, c=c: e.matmul(pb[7][:, 0:n], ones_bf[:], sq[:, c, 0:n],
                                             start=(c == 0), stop=(c == 7)), [sqk, "ones"], [("pb", 7)])
            P.act(lambda e: e.activation(rt[:, 0:n], pb[7][:, 0:n], AF.Sqrt, bias=epsc[:], scale=1.0 / 1024),
                  [("pb", 7), "epsc"], [rtk])
            P.dve(lambda e: e.reciprocal(rt[:, 0:n], rt[:, 0:n]), [rtk], [rtk])
            for c in range(8):
                P.dve(lambda e, c=c: e.scalar_tensor_tensor(
                    tmp[:, c, 0:n], xt[:, c, 0:n], gs[:, l, c, j:j + 1], rt[:, 0:n], ALU.mult, ALU.mult),
                    [xk, rtk, "gs"], [(tmpk, c)])
            for c in range(8):
                sh = modT[:, l, shift_m * 8 + c, j:j + 1]
                if h32 is not None:
                    P.pool(lambda e, c=c, sh=sh: e.tensor_scalar(
                        h32[:, c, 0:n], tmp[:, c, 0:n], sh, None, ALU.add), [(tmpk, c), "modT"], ends(h32k, c))
                    P.act(lambda e, c=c, sh=sh: e.activation(hb[:, c, 0:n], tmp[:, c, 0:n], AF.Identity, bias=sh),
                          [(tmpk, c), "modT"], ends(hk, c))
                else:
                    P.act(lambda e, c=c, sh=sh: e.activation(
                        hb[:, c, 0:n], tmp[:, c, 0:n], AF.Identity, bias=sh), [(tmpk, c), "modT"], ends(hk, c))

        def routing(rws, l, h32, h32k, t0, n):
            wrt, brt = rws[-2], rws[-1]
            S = n // 128
            for s in range(S):
                for kc in range(8):
                    P.pe(lambda e, s=s, kc=kc: e.matmul(
                        pb[6][:, s * 36:(s + 1) * 36], h32[:, kc, s * 128:(s + 1) * 128], wrt[:, kc, :],
                        start=(kc == 0), stop=(kc == 7)), [h32k, "wrt"], [("pb", 6)])
            steps = []
            D = lambda f, r=(), w=(): steps.append(("dve", f, r, w))
            A = lambda f, r=(), w=(): steps.append(("act", f, r, w))
            PEs = lambda f, r=(), w=(): steps.append(("pe", f, r, w))
            D(lambda e, X: e.tensor_tensor(X["L"][:], pb[6][:, X["s"] * 36:(X["s"] + 1) * 36], brt[:], ALU.add),
              [("pb", 6), "wrt"])
            D(lambda e, X: e.tensor_reduce(X["gm"][:], X["L"][:, 0:4], AX.X, ALU.max))
            D(lambda e, X: e.tensor_scalar(X["gsel"][:], X["L"][:, 0:4], X["gm"][:], None, ALU.is_equal))
            D(lambda e, X: e.tensor_scalar(X["pen"][:], X["gsel"][:], 1e30, -1e30, ALU.mult, ALU.add))
            D(lambda e, X: e.tensor_scalar(X["gm"][:], X["gm"][:], -1.0, None, ALU.mult))
            A(lambda e, X: e.activation(X["ge"][:], X["L"][:, 0:4], AF.Exp, bias=X["gm"][:], accum_out=X["gsum"][:]))
            D(lambda e, X: e.reciprocal(X["gsum"][:], X["gsum"][:]))
            for gg in range(4):
                D(lambda e, X, gg=gg: e.tensor_scalar(
                    X["ml"][:, gg * 8:(gg + 1) * 8], X["L"][:, 4 + gg * 8:12 + gg * 8], X["pen"][:, gg:gg + 1],
                    None, ALU.add))
            D(lambda e, X: e.max(out=X["m8"][:], in_=X["ml"][:]))
            D(lambda e, X: e.tensor_scalar(X["nv1"][:], X["m8"][:, 0:1], -1.0, None, ALU.mult))
            A(lambda e, X: e.activation(X["dx"][:], X["m8"][:, 1:2], AF.Exp, bias=X["nv1"][:]))
            D(lambda e, X: e.tensor_scalar(X["sel2"][:], X["ml"][:], X["m8"][:, 1:2], None, ALU.is_ge))
            D(lambda e, X: e.tensor_scalar(X["m1"][:], X["ml"][:], X["m8"][:, 0:1], None, ALU.is_equal))
            D(lambda e, X: e.tensor_scalar(X["m2"][:], X["ml"][:], X["m8"][:, 1:2], None, ALU.is_equal))
            D(lambda e, X: e.tensor_scalar(X["d2"][:], X["dx"][:], 1.0, None, ALU.add))
            D(lambda e, X: e.reciprocal(X["d2"][:], X["d2"][:]))
            D(lambda e, X: e.tensor_tensor(rinfo[:, 4, X["g"]:X["g"] + 1], X["d2"][:], X["gsum"][:], ALU.mult),
              [], ["RI"])
            D(lambda e, X: e.tensor_tensor(rinfo[:, 5, X["g"]:X["g"] + 1], rinfo[:, 4, X["g"]:X["g"] + 1],
                                           X["dx"][:], ALU.mult), [], ["RI"])
            D(lambda e, X: e.tensor_copy(X["ohb"][:], X["sel2"][:]))
            PEs(lambda e, X: e.matmul(pb[5][:, X["s"] * 64:X["s"] * 64 + 32], ustrict[:], X["ohb"][:],
                                      start=True, stop=True), ["ustrict"], [("pb", 5)])
            PEs(lambda e, X: e.matmul(pb[5][:, X["s"] * 64 + 32:X["s"] * 64 + 64], ones_bf[:], X["ohb"][:],
                                      start=True, stop=True), ["ones"], [("pb", 5)])
            Xs = []
            for s in range(S):
                names = ["L", "gm", "ge", "gsum", "gsel", "pen", "ml", "m8", "nv1", "ex", "sel2", "dx", "d2", "coef",
                         "m1", "m2", "rk", "j32", "ohb"]
                X = dict(zip(names, rws[s]))
                X["s"] = s
                X["g"] = t0 // 128 + s
                Xs.append(X)
            for (eng, f, r, w) in steps:
                for X in Xs:
                    ck = ("rt", X["s"])
                    rr = [ck] + list(r)
                    ww = [ck] + [(("rinfo", X["g"]) if k == "RI" else k) for k in w]
                    P.op(eng, (lambda e, f=f, X=X: f(e, X)), rr, ww)
            for X in Xs:
                s_ = X["s"]
                ck = ("rt", s_)
                P.dve(lambda e, X=X, s_=s_: e.tensor_tensor(X["rk"][:], pb[5][:, s_ * 64:s_ * 64 + 32], base[:], ALU.add),
                      [("pb", 5), "base", ck], [ck])
                P.dve(lambda e, s_=s_: e.tensor_tensor(base[:], pb[5][:, s_ * 64 + 32:s_ * 64 + 64], base[:], ALU.add),
                      [("pb", 5), "base"], ["base"])
            for q_ in range(4):
                for X in Xs:
                    mm = X["m1"] if q_ % 2 == 0 else X["m2"]
                    srcap = X["rk"][:] if q_ < 2 else iota_e
                    ck = ("rt", X["s"])
                    P.dve(lambda e, mm=mm, srcap=srcap, q_=q_, X=X: e.scalar_tensor_tensor(
                        X["j32"][:], mm[:], 1.0, srcap, ALU.mult, ALU.mult,
                        accum_out=rinfo[:, q_, X["g"]:X["g"] + 1]),
                        [ck, "hconst"], [ck, ("rinfo", X["g"])])

        def post_route(st, l, subtiles):
            kk = P.sb(st, "pr_kk", [128, 32], F32)
            pend = P.sb(st, "pr_pend", [128, 32], F32)
            pstart = P.sb(st, "pr_pstart", [128, 32], F32)
            j32 = P.sb(st, "pr_j32", [128, 32], F32)
            dcol = P.sb(st, "pr_dcol", [128, 2, 34], F32)
            bke = P.sb(st, "pr_bke", [128, NBLK], F32)
            wf = P.sb(st, "pr_wf", [128, NBLK, 2], F32)
            K = "postroute"
            D = lambda fn, r=(), w=(): P.dve(fn, [K, "base", "hconst"] + [("rinfo", g_) for g_ in range(34)] + list(r),
                                             [K] + list(w))
            D(lambda e: e.tensor_scalar(kk[:], base[:], 0.0, None, ALU.is_gt))
            for m in range(1, 9):
                D(lambda e, m=m: e.scalar_tensor_tensor(kk[:], base[:], 512.0 * m, kk[:], ALU.is_gt, ALU.add))
            D(lambda e: e.tensor_scalar(kk[:], kk[:], 512.0, None, ALU.mult))
            D(lambda e: e.tensor_tensor_scan(pend[:], ones32, kk[:], 0.0, ALU.mult, ALU.add))
            D(lambda e: e.tensor_tensor(pstart[:], pend[:], kk[:], ALU.subtract))
            D(lambda e: e.memset(dcol[:], 0.0))
            for g in subtiles:
                for sl in range(2):
                    D(lambda e, g=g, sl=sl: e.scalar_tensor_tensor(
                        j32[:], iota_e, rinfo[:, 2 + sl, g:g + 1], pstart[:], ALU.is_equal, ALU.mult,
                        accum_out=dcol[:, sl, g:g + 1]))
            D(lambda e: e.tensor_tensor(dcol[:], dcol[:], rinfo[:, 0:2, :], ALU.add))
            D(lambda e: e.tensor_copy(desti[:], dcol[:]), [], ["desti"])
            for bi in range(NBLK):
                D(lambda e, bi=bi: e.tensor_scalar(j32[:], pend[:], 512.0 * bi, None, ALU.is_le, ALU.add,
                                                   accum_out=bke[:, bi:bi + 1]))
            D(lambda e: e.tensor_scalar(bke[:], bke[:], 31.0, None, ALU.min))
            for h in range(2):
                D(lambda e, h=h: e.tensor_scalar(wf[:, :, h], bke[:], 256.0, iota2p, ALU.mult, ALU.add))
                D(lambda e, h=h: e.tensor_scalar(wf[:, :, h], wf[:, :, h], float(l * 8192 + h), None, ALU.add))
            D(lambda e: e.tensor_copy(widx[:], wf[:]), [], ["widx"])
            D(lambda e: e.memset(base[:], 0.0), [], ["base"])

        def alloc_route(st, l):
            names = [("L", 36), ("gm", 1), ("ge", 4), ("gsum", 1), ("gsel", 4), ("pen", 4), ("ml", 32), ("m8", 8),
                     ("nv1", 1), ("ex", 32), ("sel2", 32), ("dx", 1), ("d2", 1), ("coef", 1), ("m1", 32), ("m2", 32),
                     ("rk", 32), ("j32", 32)]
            rws = []
            for s_ in range(4):
                rw = [P.sb(st, "r%d_%s" % (s_, nm), [128, k], F32) for nm, k in names]
                rw.append(P.sb(st, "r%d_ohb" % s_, [128, 32], BF16))
                rws.append(rw)
            wrt = P.sb(st, "wrt", [128, 8, 36], F32)
            brt = P.sb(st, "brt", [128, 36], F32)
            P.dma("sp", wrt[:], wr_d[l], [], ["wrt"], "wrt")
            P.dma("sp", brt[:], br_d[l], [], ["wrt"], "wrt")
            return rws + [wrt, brt]

        def alloc_common(st, nxt=2):
            d = {}
            d["xt"] = [P.sb(st, "xt%d" % i, [128, 8, 512], F32) for i in range(nxt)] * (3 - nxt)
            d["sq"] = P.sb(st, "sq", [128, 8, 512], BF16)
            d["rt"] = P.sb(st, "rt", [128, 512], F32)
            d["tmp"] = P.sb(st, "tmp", [128, 8, 512], F32)
            return d

        def post_mixer(l, j_unused, wo_dram, KC, last):
            with ExitStack() as st:
                cm = alloc_common(st)
                xms = [P.sb(st, "xm%d" % i, [128, 8, 512], F32) for i in range(2)]
                hfbs = [P.sb(st, "hfb%d" % i, [128, 8, 512], BF16) for i in range(2)]
                h32 = P.sb(st, "h32", [128, 8, 512], F32)
                hft = P.sb(st, "hft", [128, 1024], BF16)
                rw = alloc_route(st, l)
                wo = P.sb(st, "wo", [128, KC, 1024], BF16)
                U = [P.sb(st, "U%d" % i, [128, KC, 512], BF16) for i in range(2)]
                P.dma("pool", wo[:], wo_dram.rearrange("(kc p) n -> p kc n", p=128), [], ["wo"], "wo")
                xsrc = xT_d if l == 0 else xs_d
                tiles = TILES[1:] if last else TILES

                def WO(i):
                    t0, n, j = tiles[i]
                    xt, xk = cm["xt"][i % 2], ("xt", i % 2)
                    load_x(xt, xk, xsrc, t0, n, [("xs", t0)])
                    Ut, uk = U[i % 2], ("U", i % 2)
                    P.dma("sp", Ut[:, :, 0:n], fm(uT_d)[:, 0:KC, t0:t0 + n], ["uT_d"], [uk], uk)
                    xm, xmk = xms[i % 2], "xm%d" % (i % 2)
                    for co in range(8):
                        pk = ("pb", co % 2)
                        pt = pb[co % 2]
                        for kc in range(KC):
                            P.pe(lambda e, pt=pt, co=co, kc=kc, Ut=Ut, n=n: e.matmul(
                                pt[:, 0:n], wo[:, kc, co * 128:(co + 1) * 128], Ut[:, kc, 0:n],
                                start=(kc == 0), stop=(kc == KC - 1)), ["wo", uk], [pk])
                        P.dve(lambda e, pt=pt, co=co, xm=xm, xt=xt, n=n, j=j: e.scalar_tensor_tensor(
                            xm[:, co, 0:n], pt[:, 0:n], modT[:, l, 16 + co, j:j + 1], xt[:, co, 0:n], ALU.mult, ALU.add),
                            [pk, xk, "modT"], [(xmk, co)] + ([xmk] if co in (0, 7) else []))
                    P.dma("pool", fm(xs_d)[:, :, t0:t0 + n], xm[:, :, 0:n], [xmk], [("xs", t0)], xmk)

                def NR(i):
                    t0, n, j = tiles[i]
                    xm, xmk = xms[i % 2], "xm%d" % (i % 2)
                    hfb, hfk = hfbs[i % 2], "hfb%d" % (i % 2)
                    norm_mod((cm["sq"], "sq", cm["rt"], "rt", cm["tmp"], "tmp"), xm, xmk, n, l, j, gsF, 3,
                             hfb, hfk, h32, "h32")

                def TRR(i):
                    t0, n, j = tiles[i]
                    hfb, hfk = hfbs[i % 2], "hfb%d" % (i % 2)
                    for s_ in range(n // 128):
                        pk4 = ("pb", 4)
                        for c in range(8):
                            P.pe(lambda e, s_=s_, c=c, hfb=hfb: e.transpose(
                                pb4b[:, c * 128:(c + 1) * 128], hfb[:, c, s_ * 128:(s_ + 1) * 128], identb[:]),
                                [hfk, "identb"], [pk4])
                        P.act(lambda e: e.activation(hft[:], pb4b[:, :], AF.Identity), [pk4], ["hft"])
                        P.dma("sp", hftok_d[t0 + s_ * 128:t0 + (s_ + 1) * 128, :], hft[:], ["hft"], ["hftok_d"], "hft")
                    routing(rw, l, h32, "h32", t0, n)

                WO(0)
                for i in range(len(tiles)):
                    if i + 1 < len(tiles):
                        WO(i + 1)
                    NR(i)
                    TRR(i)
                post_route(st, l, [t0 // 128 + s_ for (t0, n, j) in tiles for s_ in range(n // 128)])
                P.barrier()
                P.emit()

        def rglru(l, jj, last):
            xsrc = xT_d if l == 0 else xs_d
            with ExitStack() as st:
                hT = P.sb(st, "hT", [128, 8, T], BF16)
                with ExitStack() as st1:
                    cm = alloc_common(st1)
                    for i, (t0, n, j) in enumerate(TILES):
                        xt, xk = cm["xt"][i % 2], ("xt", i % 2)
                        load_x(xt, xk, xsrc, t0, n, [("xs", t0)])
                        norm_mod((cm["sq"], "sq", cm["rt"], "rt", cm["tmp"], "tmp"), xt, xk, n, l, j, gsA, 0,
                                 hT[:, :, t0:t0 + n], "hT")
                    P.barrier()
                    P.emit()
                xrs = [P.sb(st, "xr%d" % i, [128, T], F32) for i in range(2)]
                xc = P.sb(st, "xc", [128, T], F32)
                bb = P.sb(st, "bb", [128, T], F32)
                hs = P.sb(st, "hs", [128, T], F32)
                xcb = P.sb(st, "xcb", [128, T], BF16)
                gls = [P.sb(st, "gl%d" % i, [128, T], BF16) for i in range(2)]
                wx = [P.sb(st, "wx%d" % i, [128, 8, 128], BF16) for i in range(2)]
                wg = [P.sb(st, "wgt%d" % i, [128, 8, 128], BF16) for i in range(2)]
                wai = [P.sb(st, "wai%d" % i, [128, 4, 128], BF16) for i in range(2)]
                rtl = P.sb(st, "rtl", [128, 512], F32)
                itl = P.sb(st, "itl", [128, 512], F32)
                e2 = P.sb(st, "e2", [128, 512], F32)
                tk = lambda k, i: [(k, i)] + ([k] if i in (0, 8) else [])
                win = rg_w_in_d[jj].rearrange("(kc p) n -> p kc n", p=128)

                def LOADW(c):
                    wxt, wgt, wat = wx[c % 2], wg[c % 2], wai[c % 2]
                    wk = ("rgw", c % 2)
                    P.dma("pool", wxt[:], win[:, :, 1024 + c * 128:1024 + (c + 1) * 128], [], [wk], wk)
                    P.dma("pool", wgt[:], win[:, :, c * 128:(c + 1) * 128], [], [wk], wk)
                    for d in range(2):
                        P.dma("pool", wat[:, 2 * d, :], rg_wa_d[jj, d, c], [], [wk], wk)
                        P.dma("pool", wat[:, 2 * d + 1, :], rg_wi_d[jj, d, c], [], [wk], wk)

                def PROJ(c, i0, i1):
                    wxt, wgt = wx[c % 2], wg[c % 2]
                    wk = ("rgw", c % 2)
                    xr, xrn = xrs[c % 2], "xr%d" % (c % 2)
                    gl, gln = gls[c % 2], "gl%d" % (c % 2)
                    for i, (t0, n, j) in enumerate(TILES):
                        if not (i0 <= i < i1):
                            continue
                        pa, pka = pb[i % 2], ("pb", i % 2)
                        pg, pkg = pb[2 + i % 2], ("pb", 2 + i % 2)
                        for kc in range(8):
                            P.pe(lambda e, pa=pa, kc=kc, t0=t0, n=n, wxt=wxt: e.matmul(
                                pa[:, 0:n], wxt[:, kc, :], hT[:, kc, t0:t0 + n], start=(kc == 0), stop=(kc == 7)),
                                [wk, "hT"], [pka])
                        P.act(lambda e, pa=pa, t0=t0, n=n, xr=xr: e.activation(xr[:, t0:t0 + n], pa[:, 0:n], AF.Identity),
                              [pka], tk(xrn, i))
                        for kc in range(8):
                            P.pe(lambda e, pg=pg, kc=kc, t0=t0, n=n, wgt=wgt: e.matmul(
                                pg[:, 0:n], wgt[:, kc, :], hT[:, kc, t0:t0 + n], start=(kc == 0), stop=(kc == 7)),
                                [wk, "hT"], [pkg])
                        P.act(lambda e, pg=pg, t0=t0, n=n, gl=gl: e.activation(
                            gl[:, t0:t0 + n], pg[:, 0:n], AF.Gelu_apprx_tanh), [pkg], tk(gln, i))

                def CONV(c):
                    xr, xrn = xrs[c % 2], "xr%d" % (c % 2)
                    cw = lambda k, c=c: V(("convw", jj), k * 8 + c)
                    for (s0, e0) in ((0, NCTX), (NCTX, T)):
                        P.dve(lambda e, s0=s0, e0=e0, c=c, cw=cw, xr=xr: e.tensor_scalar(
                            xc[:, s0:e0], xr[:, s0:e0], cw(2), V(("convb", jj), c), ALU.mult, ALU.add),
                            [xrn, "vecs"], ["xc"])
                        for k, off in ((0, -2), (1, -1), (3, 1)):
                            lo = max(s0, s0 - off)
                            hi = min(e0, e0 - off)
                            P.dve(lambda e, lo=lo, hi=hi, off=off, k=k, cw=cw, xr=xr: e.scalar_tensor_tensor(
                                xc[:, lo:hi], xr[:, lo + off:hi + off], cw(k), xc[:, lo:hi], ALU.mult, ALU.add),
                                [xrn, "xc", "vecs"], ["xc"])
                    P.act(lambda e: e.activation(xcb[:], xc[:], AF.Identity), ["xc"], ["xcb"])

                def GATES(c, d):
                    wat = wai[c % 2]
                    wk = ("rgw", c % 2)
                    aa, xrn = xrs[c % 2], "xr%d" % (c % 2)
                    for i, (t0, n, j) in enumerate(TILES):
                        pr, pkr = pb[4 + i % 2], ("pb", 4 + i % 2)
                        pi, pki = pb[6 + i % 2], ("pb", 6 + i % 2)
                        P.pe(lambda e, pr=pr, t0=t0, n=n, d=d, wat=wat: e.matmul(
                            pr[:, 0:n], wat[:, 2 * d, :], xcb[:, t0:t0 + n], start=True, stop=True),
                            [wk, "xcb"], [pkr])
                        P.pe(lambda e, pi=pi, t0=t0, n=n, d=d, wat=wat: e.matmul(
                            pi[:, 0:n], wat[:, 2 * d + 1, :], xcb[:, t0:t0 + n], start=True, stop=True),
                            [wk, "xcb"], [pki])
                        P.act(lambda e, pr=pr, n=n, d=d, c=c: e.activation(
                            rtl[:, 0:n], pr[:, 0:n], AF.Sigmoid, bias=V(("ba", jj), d * 8 + c)),
                            [pkr, "vecs"], ["rtl"])
                        P.act(lambda e, pi=pi, n=n, d=d, c=c: e.activation(
                            itl[:, 0:n], pi[:, 0:n], AF.Sigmoid, bias=V(("bi", jj), d * 8 + c)),
                            [pki, "vecs"], ["itl"])
                        P.act(lambda e, t0=t0, n=n, d=d, c=c, aa=aa: e.activation(
                            aa[:, t0:t0 + n], rtl[:, 0:n], AF.Exp, scale=sdec[:, jj, d * 8 + c:d * 8 + c + 1]),
                            ["rtl", "sdec"], tk(xrn, i))
                        P.act(lambda e, n=n, d=d, c=c: e.activation(
                            e2[:, 0:n], rtl[:, 0:n], AF.Exp, scale=sdec2[:, jj, d * 8 + c:d * 8 + c + 1]),
                            ["rtl", "sdec2"], ["e2"])
                        P.act(lambda e, n=n: e.activation(e2[:, 0:n], e2[:, 0:n], AF.Sqrt, bias=1.0, scale=-1.0),
                              ["e2"], ["e2"])
                        P.pool(lambda e, t0=t0, n=n: e.tensor_tensor(
                            itl[:, 0:n], itl[:, 0:n], xc[:, t0:t0 + n], ALU.mult), ["itl", "xc"], ["itl"])
                        P.dve(lambda e, t0=t0, n=n: e.tensor_tensor(
                            bb[:, t0:t0 + n], itl[:, 0:n], e2[:, 0:n], ALU.mult), ["itl", "e2"], tk("bb", i))

                def SCAN(c, d):
                    aa, xrn = xrs[c % 2], "xr%d" % (c % 2)
                    if d == 0:
                        P.dve(lambda e: e.tensor_tensor_scan(hs[:], aa[:], bb[:], 0.0, ALU.mult, ALU.add),
                              [xrn, "bb"], ["hs"])
                    else:
                        P.dve(lambda e: e.tensor_tensor_scan(
                            bb[:, NCTX - 1::-1], aa[:, NCTX - 1::-1], bb[:, NCTX - 1::-1], 0.0, ALU.mult, ALU.add),
                            [xrn, "bb"], ["bb"])
                        P.dve(lambda e: e.tensor_tensor_scan(
                            bb[:, T - 1:NCTX - 1:-1], aa[:, T - 1:NCTX - 1:-1], bb[:, T - 1:NCTX - 1:-1],
                            bb[:, 0:1], ALU.mult, ALU.add), [xrn, "bb"], ["bb"])
                        P.dve(lambda e: e.tensor_tensor(hs[:], hs[:], bb[:], ALU.add), ["hs", "bb"], ["hs"])

                def OUT(c):
                    gl, gln = gls[c % 2], "gl%d" % (c % 2)
                    P.dve(lambda e, gl=gl: e.tensor_tensor(xcb[:], hs[:], gl[:], ALU.mult), ["hs", gln], ["xcb"])
                    P.dma("sp", uT_d[c * 128:(c + 1) * 128, :], xcb[:], ["xcb"], ["uT_d"], "ub")

                LOADW(0)
                PROJ(0, 0, 9)
                for c in range(8):
                    if c + 1 < 8:
                        LOADW(c + 1)
                    CONV(c)
                    GATES(c, 0)
                    if c + 1 < 8:
                        PROJ(c + 1, 0, 5)
                    SCAN(c, 0)
                    GATES(c, 1)
                    if c + 1 < 8:
                        PROJ(c + 1, 5, 9)
                    SCAN(c, 1)
                    OUT(c)
                P.barrier()
                P.emit()
            if stop_after == ("r2", l):
                return
            post_mixer(l, 0, rg_w_out_d[jj], 8, last)

        def attention(l):
            xsrc = xs_d
            with ExitStack() as st:
                QT = P.sb(st, "QT", [128, 8, T], BF16)
                KT = P.sb(st, "KT", [128, 2, T], BF16)
                Vt = P.sb(st, "Vt", [128, 34, 256], BF16)
                with ExitStack() as st1:
                    cm = alloc_common(st1, 1)
                    hb = P.sb(st1, "hb", [128, 8, 512], BF16)
                    wq = P.sb(st1, "wq", [128, 8, 1536], BF16)
                    cosT = P.sb(st1, "cosT", [128, 512], F32)
                    sinT = P.sb(st1, "sinT", [128, 512], F32)
                    rotm = P.sb(st1, "rotm", [128, 128], BF16)
                    sqhs = [P.sb(st1, "sqh%d" % i, [128, 512], BF16) for i in range(2)]
                    rths = [P.sb(st1, "rth%d" % i, [128, 512], F32) for i in range(2)]
                    qns = [P.sb(st1, "qn%d" % i, [128, 512], BF16) for i in range(2)]
                    t1s = [P.sb(st1, "t1%d" % i, [128, 512], F32) for i in range(2)]
                    t2s = [P.sb(st1, "t2%d" % i, [128, 512], F32) for i in range(2)]
                    P.dma("pool", wq[:], at_w_qkv_d.rearrange("(kc p) n -> p kc n", p=128), [], ["wq"], "wq")
                    P.dma("pool", rotm[:], rm_d, [], ["rotm"], "rotm")
                    for i, (t0, n, j) in enumerate(TILES):
                        xt, xk = cm["xt"][0], ("xt", 0)
                        load_x(xt, xk, xsrc, t0, n, [("xs", t0)])
                        if j == 0:
                            P.dma("sp", cosT[:], cos_d[:, t0 - NCTX:t0 - NCTX + 512], [], ["cs"], "cosT")
                            P.dma("sp", sinT[:], sin_d[:, t0 - NCTX:t0 - NCTX + 512], [], ["cs"], "sinT")
                        norm_mod((cm["sq"], "sq", cm["rt"], "rt", cm["tmp"], "tmp"), xt, xk, n, l, j, gsA, 0,
                                 hb, "hb")
                        for hh in range(10):
                            pq, pkq = pb[hh % 2], ("pb", hh % 2)
                            hp = hh % 2
                            sqh, rth, qn, t1, t2 = sqhs[hp], rths[hp], qns[hp], t1s[hp], t2s[hp]
                            sqk, rthk, qnk, t1k, t2k = ("sqh", hp), ("rth", hp), ("qn", hp), ("t1", hp), ("t2", hp)
                            pss, pks = pb[2 + hp], ("pb", 2 + hp)
                            prr, pkr = pb[4 + hp], ("pb", 4 + hp)
                            for kc in range(8):
                                P.pe(lambda e, pq=pq, kc=kc, hh=hh, n=n: e.matmul(
                                    pq[:, 0:n], wq[:, kc, hh * 128:(hh + 1) * 128], hb[:, kc, 0:n],
                                    start=(kc == 0), stop=(kc == 7)), ["wq", "hb"], [pkq])
                            P.act(lambda e, pq=pq, n=n, sqh=sqh: e.activation(sqh[:, 0:n], pq[:, 0:n], AF.Square),
                                  [pkq], [sqk])
                            P.pe(lambda e, n=n, sqh=sqh, pss=pss: e.matmul(pss[:, 0:n], ones_bf[:], sqh[:, 0:n],
                                                                         start=True, stop=True),
                                 [sqk, "ones"], [pks])
                            P.act(lambda e, n=n, rth=rth, pss=pss: e.activation(
                                rth[:, 0:n], pss[:, 0:n], AF.Sqrt, bias=epsc[:], scale=1.0 / 128),
                                [pks, "epsc"], [rthk])
                            P.dve(lambda e, n=n, rth=rth: e.reciprocal(rth[:, 0:n], rth[:, 0:n]), [rthk], [rthk])
                            gvec = V("qg") if hh < 8 else V("kg")
                            dst = QT[:, hh, t0:t0 + n] if hh < 8 else KT[:, hh - 8, t0:t0 + n]
                            dk = ("QT", hh) if hh < 8 else ("KT", hh - 8)
                            dkw = [dk, "QT" if hh < 8 else "KT"]
                            if j == 1:
                                P.dve(lambda e, pq=pq, n=n, gvec=gvec, dst=dst, rth=rth: e.scalar_tensor_tensor(
                                    dst, pq[:, 0:n], gvec, rth[:, 0:n], ALU.mult, ALU.mult),
                                    [pkq, rthk, "vecs"], dkw)
                            else:
                                P.dve(lambda e, pq=pq, n=n, gvec=gvec, rth=rth, qn=qn: e.scalar_tensor_tensor(
                                    qn[:, 0:n], pq[:, 0:n], gvec, rth[:, 0:n], ALU.mult, ALU.mult),
                                    [pkq, rthk, "vecs"], [qnk])
                                P.pe(lambda e, n=n, qn=qn, prr=prr: e.matmul(prr[:, 0:n], rotm[:], qn[:, 0:n],
                                                                           start=True, stop=True),
                                     [qnk, "rotm"], [pkr])
                                P.pool(lambda e, n=n, qn=qn, t1=t1: e.tensor_tensor(
                                    t1[:, 0:n], qn[:, 0:n], cosT[:, 0:n], ALU.mult), [qnk, "cs"], [t1k])
                                P.dve(lambda e, n=n, t2=t2, prr=prr: e.tensor_tensor(
                                    t2[:, 0:n], prr[:, 0:n], sinT[:, 0:n], ALU.mult), [pkr, "cs"], [t2k])
                                P.pool(lambda e, n=n, dst=dst, t1=t1, t2=t2: e.tensor_tensor(
                                    dst, t1[:, 0:n], t2[:, 0:n], ALU.add), [t1k, t2k], dkw)
                        for s in range(n // 128):
                            kt = t0 // 128 + s
                            pv, pkv = pb[6], ("pb", 6)
                            for kc in range(8):
                                P.pe(lambda e, pv=pv, kc=kc, s=s: e.matmul(
                                    pv[:, 0:256], hb[:, kc, s * 128:(s + 1) * 128], wq[:, kc, 1280:1536],
                                    start=(kc == 0), stop=(kc == 7)), ["wq", "hb"], [pkv])
                            P.act(lambda e, pv=pv, kt=kt: e.activation(Vt[:, kt, :], pv[:, 0:256], AF.Identity),
                                  [pkv], ["Vt"])
                    P.barrier()
                    P.emit()
                pT = [P.sb(st, "pT%d" % i, [128, 512], BF16) for i in range(4)]
                oT = [P.sb(st, "oT%d" % i, [128, 8, 512], BF16) for i in range(2)]
                rd = P.sb(st, "rd", [128, 512], F32)
                accs = [P.sb(st, "acc%d" % i, [128, 512], F32) for i in range(2)]
                accb = P.sb(st, "accb", [128, 512], F32)
                ones_f = P.sb(st, "ones_f", [128, 128], F32)
                P.pool(lambda e: e.memset(ones_f[:], 1.0), [], ["ones_f"])
                SC = 128.0 ** -0.5
                DEPTH = 3
                jobs = []
                for i, (t0, n, j) in enumerate(TILES):
                    for hq in range(8):
                        jobs.append(dict(i=i, t0=t0, n=n, j=j, hq=hq, kv=hq // 4, nkt=(2 if j == 1 else 34),
                                         ot=oT[i % 2], ok=("oT", i % 2),
                                         pO=pb[4 + hq % 2], pkO=("pb", 4 + hq % 2),
                                         pD=pb[6 + hq % 2], pkD=("pb", 6 + hq % 2)))

                def SE(J, kt):
                    pS, pkS = pb[kt % 4], ("pb", kt % 4)
                    ptt, ptk = pT[kt % 4], ("pT", kt % 4)
                    n, t0, kv, hq = J["n"], J["t0"], J["kv"], J["hq"]
                    P.pe(lambda e: e.matmul(
                        pS[:, 0:n], KT[:, kv, kt * 128:(kt + 1) * 128], QT[:, hq, t0:t0 + n],
                        start=True, stop=True), ["QT", "KT"], [pkS])
                    P.act(lambda e: e.activation(ptt[:, 0:n], pS[:, 0:n], AF.Exp, scale=SC), [pkS], [ptk])

                def VV(J, kt):
                    ptt, ptk = pT[kt % 4], ("pT", kt % 4)
                    n, kv, nkt, pO, pkO = J["n"], J["kv"], J["nkt"], J["pO"], J["pkO"]
                    P.pe(lambda e: e.matmul(
                        pO[:, 0:n], Vt[:, kt, kv * 128:(kv + 1) * 128], ptt[:, 0:n],
                        start=(kt == 0), stop=(kt == nkt - 1)), ["Vt", ptk], [pkO])
                    acc, acck = accs[kt % 2], ("acc", kt % 2)
                    if kt < 2:
                        P.dve(lambda e: e.tensor_copy(acc[:, 0:n], ptt[:, 0:n]), [ptk], [acck])
                    else:
                        P.dve(lambda e: e.tensor_tensor(acc[:, 0:n], acc[:, 0:n], ptt[:, 0:n], ALU.add),
                              [ptk, acck], [acck])

                def PRO(J):
                    for kt in range(min(DEPTH, J["nkt"])):
                        SE(J, kt)

                def BODY(J):
                    for kt in range(J["nkt"]):
                        if kt + DEPTH < J["nkt"]:
                            SE(J, kt + DEPTH)
                        VV(J, kt)
                    n = J["n"]
                    P.dve(lambda e: e.tensor_tensor(accb[:, 0:n], accs[0][:, 0:n], accs[1][:, 0:n], ALU.add),
                          [("acc", 0), ("acc", 1)], ["accb"])

                def EPI(J):
                    n, pD, pkD, pO, pkO, hq, ot, ok = (J["n"], J["pD"], J["pkD"], J["pO"], J["pkO"], J["hq"],
                                                       J["ot"], J["ok"])
                    P.pe(lambda e: e.matmul(pD[:, 0:n], ones_f[:], accb[:, 0:n], start=True, stop=True),
                         ["ones_f", "accb"], [pkD])
                    P.dve(lambda e: e.reciprocal(rd[:, 0:n], pD[:, 0:n]), [pkD], ["rd"])
                    P.dve(lambda e: e.tensor_tensor(ot[:, hq, 0:n], pO[:, 0:n], rd[:, 0:n], ALU.mult),
                          [pkO, "rd"], [ok])
                    if hq == 7:
                        t0 = J["t0"]
                        P.dma("sp", fm(uT_d)[:, 0:8, t0:t0 + n], ot[:, :, 0:n], [ok], ["uT_d"], ok)

                PRO(jobs[0])
                for k_, J in enumerate(jobs):
                    BODY(J)
                    if k_ + 1 < len(jobs):
                        PRO(jobs[k_ + 1])
                    EPI(J)
                P.barrier()
                P.emit()
            post_mixer(l, 0, at_w_o_d, 8, False)

        def gmlp(l):
            xsrc = xs_d
            with ExitStack() as st:
                cm = alloc_common(st, 1)
                hb = P.sb(st, "hb", [128, 8, 512], BF16)
                win = P.sb(st, "cwin", [128, 8, 4096], BF16)
                wsT = P.sb(st, "wsT", [128, 8, 128], BF16)
                lng = P.sb(st, "lng", [128, 2048], BF16)
                lnb = P.sb(st, "lnb", [128, 2048], BF16)
                bsr = P.sb(st, "bsr", [128, 16, 128], F32)
                uTs = [P.sb(st, "uTt%d" % i, [128, 16, 512], BF16) for i in range(2)]
                vv = P.sb(st, "vv", [128, 2048], F32)
                vnb = P.sb(st, "vnb", [128, 2048], BF16)
                junk = P.sb(st, "junk", [128, 512], BF16)
                sums = P.sb(st, "sums", [128, 8], F32)
                stt = P.sb(st, "stt", [128, 4], F32)
                mtmp = P.sb(st, "mtmp", [128, 4, 128], F32)
                cwv = cm_w_in_d.rearrange("(kc p) n -> p kc n", p=128)
                for q4 in range(4):
                    P.dma("pool", win[:, :, q4 * 1024:(q4 + 1) * 1024], cwv[:, :, q4 * 1024:(q4 + 1) * 1024],
                          [], ["cwin"], "cwin")
                P.dma("pool", wsT[:], cm_w_sT_d, [], ["wsT"], "wsT")
                P.dma("pool", lng[:], cm_lng_d, [], ["ln"], "lng")
                P.dma("pool", lnb[:], cm_lnb_d, [], ["ln"], "lnb")
                P.dma("sp", bsr[:], cm_bs_d, [], ["bsr"], "bsr")
                for i, (t0, n, j) in enumerate(TILES):
                    xt, xk = cm["xt"][0], ("xt", 0)
                    uTt, utk = uTs[i % 2], ("uTt", i % 2)
                    load_x(xt, xk, xsrc, t0, n, [("xs", t0)])
                    norm_mod((cm["sq"], "sq", cm["rt"], "rt", cm["tmp"], "tmp"), xt, xk, n, l, j, gsA, 0,
                             hb, "hb")
                    for uc in range(16):
                        pu, pku = pb[uc % 2], ("pb", uc % 2)
                        for kc in range(8):
                            P.pe(lambda e, pu=pu, kc=kc, uc=uc, n=n: e.matmul(
                                pu[:, 0:n], win[:, kc, uc * 128:(uc + 1) * 128], hb[:, kc, 0:n],
                                start=(kc == 0), stop=(kc == 7)), ["cwin", "hb"], [pku])
                        P.act(lambda e, pu=pu, uc=uc, n=n, uTt=uTt: e.activation(
                            uTt[:, uc, 0:n], pu[:, 0:n], AF.Gelu_apprx_tanh), [pku], [utk])
                    for s in range(n // 128):
                        for nb in range(4):
                            pv, pkv = pb[2 + nb % 2], ("pb", 2 + nb % 2)
                            for kc in range(8):
                                P.pe(lambda e, pv=pv, kc=kc, s=s, nb=nb: e.matmul(
                                    pv[:, :], hb[:, kc, s * 128:(s + 1) * 128],
                                    win[:, kc, 2048 + nb * 512:2048 + (nb + 1) * 512],
                                    start=(kc == 0), stop=(kc == 7)), ["cwin", "hb"], [pkv])
                            P.act(lambda e, pv=pv, nb=nb: e.activation(
                                vv[:, nb * 512:(nb + 1) * 512], pv[:, :], AF.Gelu_apprx_tanh,
                                accum_out=sums[:, nb:nb + 1]), [pkv], ["vv", "sums"])
                            P.act(lambda e, nb=nb: e.activation(
                                junk[:], vv[:, nb * 512:(nb + 1) * 512], AF.Square,
                                accum_out=sums[:, 4 + nb:5 + nb]), ["vv"], ["junk", "sums"])
                        P.dve(lambda e: e.tensor_reduce(stt[:, 0:1], sums[:, 0:4], AX.X, ALU.add), ["sums"], ["stt"])
                        P.dve(lambda e: e.tensor_reduce(stt[:, 1:2], sums[:, 4:8], AX.X, ALU.add), ["sums"], ["stt"])
                        P.dve(lambda e: e.tensor_scalar(stt[:, 0:2], stt[:, 0:2], 1.0 / 2048, None, ALU.mult),
                              ["stt"], ["stt"])
                        P.dve(lambda e: e.tensor_tensor(stt[:, 2:3], stt[:, 0:1], stt[:, 0:1], ALU.mult),
                              ["stt"], ["stt"])
                        P.dve(lambda e: e.tensor_tensor(stt[:, 1:2], stt[:, 1:2], stt[:, 2:3], ALU.subtract),
                              ["stt"], ["stt"])
                        P.act(lambda e: e.activation(stt[:, 1:2], stt[:, 1:2], AF.Sqrt, bias=epsc[:]),
                              ["stt", "epsc"], ["stt"])
                        P.dve(lambda e: e.reciprocal(stt[:, 1:2], stt[:, 1:2]), ["stt"], ["stt"])
                        P.dve(lambda e: e.tensor_scalar(vv[:], vv[:], stt[:, 0:1], stt[:, 1:2], ALU.subtract, ALU.mult),
                              ["vv", "stt"], ["vv"])
                        P.pool(lambda e: e.tensor_tensor(vv[:], vv[:], lng[:], ALU.mult), ["vv", "ln"], ["vv"])
                        P.dve(lambda e: e.tensor_tensor(vnb[:], vv[:], lnb[:], ALU.add), ["vv", "ln"], ["vnb"])
                        for q4 in range(4):
                            pm, pkm = pb[4 + q4 % 2], ("pb", 4 + q4 % 2)
                            for cc in range(4):
                                ch = q4 * 4 + cc
                                P.pe(lambda e, pm=pm, cc=cc, ch=ch: e.matmul(
                                    pm[:, cc * 128:(cc + 1) * 128], vnb[:, ch * 128:(ch + 1) * 128], wsT[:, ch // 2, :],
                                    start=True, stop=True), ["vnb", "wsT"], [pkm])
                            P.dve(lambda e, pm=pm, q4=q4: e.tensor_tensor(
                                mtmp[:], pm[:, :].rearrange("p (c t) -> p c t", c=4), bsr[:, q4 * 4:(q4 + 1) * 4, :],
                                ALU.add), [pkm, "bsr"], ["mtmp"])
                            P.pool(lambda e, q4=q4, s=s, uTt=uTt: e.tensor_tensor(
                                uTt[:, q4 * 4:(q4 + 1) * 4, s * 128:(s + 1) * 128], mtmp[:],
                                uTt[:, q4 * 4:(q4 + 1) * 4, s * 128:(s + 1) * 128], ALU.mult),
                                ["mtmp", utk], [utk])
                    P.dma("sp", fm(uT_d)[:, :, t0:t0 + n], uTt[:, :, 0:n], [utk], ["uT_d"], utk)
                P.barrier()
                P.emit()
            post_mixer(l, 0, cm_w_out_d, 16, False)

        def moe(l, last):
            ranges = [(256, 2304), (2304, 4352)] if last else [(0, 2176), (2176, 4352)]
            with ExitStack() as st:
                hfh = P.sb(st, "hfh", [128, 8, 2176], BF16)
                yacc = P.sb(st, "yacc", [128, 8, 2176], F32)
                wgs = [P.sb(st, "mwg%d" % i, [128, 8, 512], BF16) for i in range(2)]
                wus = [P.sb(st, "mwu%d" % i, [128, 8, 512], BF16) for i in range(2)]
                wds = [P.sb(st, "mwd%d" % i, [128, 4, 1024], BF16) for i in range(2)]
                wbs = [P.sb(st, "mwb0", [128, 2176], F32)] * 2
                sg = [P.sb(st, "msg%d" % i, [128, 512], F32) for i in range(2)]
                tt = [P.sb(st, "mtt%d" % i, [128, 512], F32) for i in range(2)]
                ab = [P.sb(st, "mab%d" % i, [128, 4, 512], BF16) for i in range(2)]
                xt2 = P.sb(st, "mxt", [128, 8, 256], F32)
                it = 0
                for (r0, r1) in ranges:
                    nt = r1 - r0
                    subt = []
                    o = 0
                    while o < nt:
                        subt.append((o, min(512, nt - o)))
                        o += 512
                    P.dma("sp", hfh[:, :, 0:nt], fm(hfT_d)[:, :, r0:r1], [("hfT", t0) for t0, _, _ in TILES],
                          ["hfh"], "hfh")
                    for c8 in range(8):
                        P.pool(lambda e, c8=c8: e.memset(yacc[:, c8, :], 0.0), [], ["yacc"])
                    for ex in range(32):
                        s2 = it % 2
                        wk = ("mw", s2)
                        wgt, wut, wdt, wbt = wgs[s2], wus[s2], wds[s2], wbs[s2]
                        P.dma("pool", wgt[:], moe_wg_d[l, ex].rearrange("(kc p) n -> p kc n", p=128), [], [wk], wk)
                        P.dma("pool", wut[:], moe_wu_d[l, ex].rearrange("(kc p) n -> p kc n", p=128), [], [wk], wk)
                        P.dma("pool", wdt[:], moe_wd_d[l, ex].rearrange("(kc p) n -> p kc n", p=128), [], [wk], wk)
                        wbk = ("mwb", 0)
                        P.dma("sp", wbt[:, 0:nt], wcT_d[ex, r0:r1].partition_broadcast(128),
                              ["wcT_d"], [wbk], wbk)
                        for ti, (o, n) in enumerate(subt):
                            abt, abk = ab[ti % 2], ("mab", ti % 2)
                            for jc in range(4):
                                pG, pkG = pb[jc % 2], ("pb", jc % 2)
                                pU, pkU = pb[2 + jc % 2], ("pb", 2 + jc % 2)
                                for kc in range(8):
                                    P.pe(lambda e, pG=pG, kc=kc, jc=jc, o=o, n=n, wgt=wgt: e.matmul(
                                        pG[:, 0:n], wgt[:, kc, jc * 128:(jc + 1) * 128], hfh[:, kc, o:o + n],
                                        start=(kc == 0), stop=(kc == 7)), [wk, "hfh"], [pkG])
                                for kc in range(8):
                                    P.pe(lambda e, pU=pU, kc=kc, jc=jc, o=o, n=n, wut=wut: e.matmul(
                                        pU[:, 0:n], wut[:, kc, jc * 128:(jc + 1) * 128], hfh[:, kc, o:o + n],
                                        start=(kc == 0), stop=(kc == 7)), [wk, "hfh"], [pkU])
                                sgt, sgk = sg[jc % 2], ("msg", jc % 2)
                                ttt, ttk = tt[jc % 2], ("mtt", jc % 2)
                                P.act(lambda e, pG=pG, sgt=sgt, n=n: e.activation(sgt[:, 0:n], pG[:, 0:n], AF.Silu),
                                      [pkG], [sgk])
                                P.dve(lambda e, pU=pU, sgt=sgt, ttt=ttt, n=n: e.tensor_tensor(
                                    ttt[:, 0:n], sgt[:, 0:n], pU[:, 0:n], ALU.mult), [sgk, pkU], [ttk])
                                P.pool(lambda e, ttt=ttt, abt=abt, jc=jc, o=o, n=n, wbt=wbt: e.tensor_tensor(
                                    abt[:, jc, 0:n], ttt[:, 0:n], wbt[:, o:o + n], ALU.mult), [ttk, wbk], [abk])
                            for co in range(8):
                                pD, pkD = pb[4 + co % 4], ("pb", 4 + co % 4)
                                for kc in range(4):
                                    P.pe(lambda e, pD=pD, kc=kc, co=co, n=n, wdt=wdt, abt=abt: e.matmul(
                                        pD[:, 0:n], wdt[:, kc, co * 128:(co + 1) * 128], abt[:, kc, 0:n],
                                        start=(kc == 0), stop=(kc == 3)), [wk, abk], [pkD])
                                P.dve(lambda e, pD=pD, co=co, o=o, n=n: e.tensor_tensor(
                                    yacc[:, co, o:o + n], yacc[:, co, o:o + n], pD[:, 0:n], ALU.add),
                                    [pkD, "yacc"], ["yacc"])
                        it += 1
                    if ydbg_d is not None and r0 == ranges[0][0] and l == nlayers - 1:
                        P.dma("sp", fm(ydbg_d)[:, :, 0:nt], yacc[:, :, 0:nt], ["yacc"], ["ydbg_d"], "yacc")
                    for ti, (o, n) in enumerate([(oo, min(256, nt - oo)) for oo in range(0, nt, 256)]):
                        a0 = r0 + o
                        P.dma("sp", xt2[:, :, 0:n], fm(xs_d)[:, :, a0:a0 + n], [("xs", t0) for t0, _, _ in TILES],
                              ["mxt"], "mxt")
                        segs = []
                        if a0 < NCTX:
                            segs.append((0, min(n, NCTX - a0), 1))
                            if a0 + n > NCTX:
                                segs.append((NCTX - a0, n, 0))
                        else:
                            segs.append((0, n, 0))
                        for (q0, q1, j) in segs:
                            for c in range(8):
                                P.dve(lambda e, c=c, q0=q0, q1=q1, j=j, o=o: e.scalar_tensor_tensor(
                                    xt2[:, c, q0:q1], yacc[:, c, o + q0:o + q1], modT[:, l, 40 + c, j:j + 1],
                                    xt2[:, c, q0:q1], ALU.mult, ALU.add), ["yacc", "mxt", "modT"], ["mxt"])
                        if last:
                            P.dma("pool", fm(outT_d)[:, :, a0 - NCTX:a0 - NCTX + n], xt2[:, :, 0:n], ["mxt"],
                                  ["out_d"], "mxt_st")
                        else:
                            P.dma("pool", fm(xs_d)[:, :, a0:a0 + n], xt2[:, :, 0:n], ["mxt"],
                                  [("xs", t0) for t0, _, _ in TILES], "mxt_st")
                P.barrier()
                P.emit()

        def moe_sparse(l, last):
            subtiles = list(range(2, 34)) if last else list(range(34))
            NB = 48 if last else NBLK
            with ExitStack() as st:
                hfts = [P.sb(st, "dhft%d" % i, [128, 1024], BF16) for i in range(2)]
                for ii, g in enumerate(subtiles):
                    ht, hk = hfts[ii % 2], ("dhft", ii % 2)
                    P.dma("sp", ht[:], hftok_d[g * 128:(g + 1) * 128, :], ["hftok_d"], [hk], hk)
                    for sl in range(2):
                        P.idma(xrows_d, desti[:, sl, g:g + 1], ht[:], None, [hk, "desti", "xrows_d"],
                               [("xrows_sc", ii % 2, sl)], ("dsc", ii % 2, sl))
                P.barrier()
                wgs = [P.sb(st, "mwg%d" % i, [128, 8, 512], BF16) for i in range(3)]
                wus = [P.sb(st, "mwu%d" % i, [128, 8, 512], BF16) for i in range(3)]
                wds = [P.sb(st, "mwd%d" % i, [128, 4, 1024], BF16) for i in range(3)]
                xbs = [P.sb(st, "mxb%d" % i, [128, 4, 1024], BF16) for i in range(2)]
                xbT = [P.sb(st, "mxbT%d" % i, [128, 8, 512], BF16) for i in range(2)]
                sg = [P.sb(st, "msg%d" % i, [128, 512], F32) for i in range(2)]
                ab = [P.sb(st, "mab%d" % i, [128, 4, 512], BF16) for i in range(2)]
                yb = [P.sb(st, "myb%d" % i, [128, 1024], F32) for i in range(2)]
                NOW = False

                def LOADS(bi):
                    s2 = bi % 2
                    s3 = bi % 3
                    wk = ("mw", s3)
                    wgt, wut, wdt = wgs[s3], wus[s3], wds[s3]
                    if not (NOW and bi >= 2):
                        for h in range(2):
                            ix = widx[:, bi, h:h + 1]
                            P.idma(wgt[:, 4 * h:4 * h + 4, :].rearrange("p a b -> p (a b)"), None, moe_wg_d, ix,
                                   ["widx"], [wk], wk)
                            P.idma(wut[:, 4 * h:4 * h + 4, :].rearrange("p a b -> p (a b)"), None, moe_wu_d, ix,
                                   ["widx"], [wk], wk)
                            P.idma(wdt[:, 2 * h:2 * h + 2, :].rearrange("p a b -> p (a b)"), None, moe_wd_d, ix,
                                   ["widx"], [wk], wk)
                    xb, xbk = xbs[s2], ("mxb", s2)
                    P.dma("sp", xb[:], xrows_d[bi * 512:(bi + 1) * 512, :].rearrange("(s p) d -> p s d", p=128),
                          ["xrows_d"], [xbk], xbk)

                def TR(bi):
                    s2 = bi % 2
                    xb, xbk = xbs[s2], ("mxb", s2)
                    xt_, xtk = xbT[s2], ("mxbT", s2)
                    for kc in range(8):
                        pX, pkX = (pb4b, ("pb", 4)) if kc % 2 == 0 else (pb5b, ("pb", 5))
                        for s_ in range(4):
                            P.pe(lambda e, pX=pX, s_=s_, kc=kc, xb=xb: e.transpose(
                                pX[:, s_ * 128:(s_ + 1) * 128], xb[:, s_, kc * 128:(kc + 1) * 128], identb[:]),
                                [xbk, "identb"], [pkX])
                        if kc % 2 == 0:
                            P.act(lambda e, pX=pX, kc=kc, xt_=xt_: e.activation(xt_[:, kc, :], pX[:, 0:512], AF.Identity),
                                  [pkX], [(xtk, kc)])
                        else:
                            P.dve(lambda e, pX=pX, kc=kc, xt_=xt_: e.tensor_copy(xt_[:, kc, :], pX[:, 0:512]),
                                  [pkX], [(xtk, kc)])

                def GU(bi):
                    s2 = bi % 2
                    s3 = bi % 3
                    wk = ("mw", s3)
                    wgt, wut = wgs[s3], wus[s3]
                    xt_, xtk = xbT[s2], ("mxbT", s2)
                    abt, abk = ab[s2], ("mab", s2)
                    for jc in range(4):
                        pG, pkG = pb[jc % 2], ("pb", jc % 2)
                        pU, pkU = pb[2 + jc % 2], ("pb", 2 + jc % 2)
                        for kc in range(8):
                            P.pe(lambda e, pG=pG, kc=kc, jc=jc, wgt=wgt, xt_=xt_: e.matmul(
                                pG[:, :], wgt[:, kc, jc * 128:(jc + 1) * 128], xt_[:, kc, :],
                                start=(kc == 0), stop=(kc == 7)), [wk, (xtk, kc)], [pkG])
                        for kc in range(8):
                            P.pe(lambda e, pU=pU, kc=kc, jc=jc, wut=wut, xt_=xt_: e.matmul(
                                pU[:, :], wut[:, kc, jc * 128:(jc + 1) * 128], xt_[:, kc, :],
                                start=(kc == 0), stop=(kc == 7)), [wk, (xtk, kc)], [pkU])
                        sgt, sgk = sg[jc % 2], ("msg", jc % 2)
                        P.act(lambda e, pG=pG, sgt=sgt: e.activation(sgt[:], pG[:, :], AF.Silu), [pkG], [sgk])
                        P.dve(lambda e, pU=pU, sgt=sgt, abt=abt, jc=jc: e.tensor_tensor(
                            abt[:, jc, :], sgt[:], pU[:, :], ALU.mult), [sgk, pkU], [(abk, jc)])

                def DN(bi):
                    s2 = bi % 2
                    s3 = bi % 3
                    wk = ("mw", s3)
                    wdt = wds[s3]
                    abt, abk = ab[s2], ("mab", s2)
                    for s_ in range(4):
                        ybt, ybk = yb[s_ % 2], ("myb", s_ % 2)
                        for hh in range(2):
                            pD, pkD = pb[6 + hh], ("pb", 6 + hh)
                            for kc in range(4):
                                P.pe(lambda e, pD=pD, kc=kc, s_=s_, hh=hh, wdt=wdt, abt=abt: e.matmul(
                                    pD[:, :], abt[:, kc, s_ * 128:(s_ + 1) * 128], wdt[:, kc, hh * 512:(hh + 1) * 512],
                                    start=(kc == 0), stop=(kc == 3)), [wk, (abk, kc)], [pkD])
                            if hh == 0:
                                P.act(lambda e, pD=pD, ybt=ybt: e.activation(ybt[:, 0:512], pD[:, :], AF.Identity),
                                      [pkD], [ybk])
                            else:
                                P.dve(lambda e, pD=pD, ybt=ybt: e.tensor_copy(ybt[:, 512:1024], pD[:, :]),
                                      [pkD], [ybk])
                        r0_ = bi * 512 + s_ * 128
                        P.dma("sp", yrows_d[r0_:r0_ + 128, :], ybt[:], [ybk], [("yrows_st", s_ % 2)], ybk)

                LOADS(0)
                LOADS(1)
                TR(0)
                for bi in range(NB):
                    if bi + 2 < NB:
                        LOADS(bi + 2)
                    GU(bi)
                    if bi + 1 < NB:
                        TR(bi + 1)
                    DN(bi)
                P.barrier()
                P.emit()
            with ExitStack() as st:
                y1 = [P.sb(st, "cy1_%d" % i, [128, 1024], F32) for i in range(4)]
                y2 = [P.sb(st, "cy2_%d" % i, [128, 1024], F32) for i in range(4)]
                yt4s = [P.sb(st, "cyt4_%d" % i, [128, 4, 1024], F32) for i in range(2)]
                xt2s = [P.sb(st, "cxt%d" % i, [128, 8, 512], F32) for i in range(2)]
                tiles = TILES[1:] if last else TILES
                ii = 0
                for ti_, (t0, n, j) in enumerate(tiles):
                    xt2, cxk = xt2s[ti_ % 2], "cxt%d" % (ti_ % 2)
                    yt4, cyk = yt4s[ti_ % 2], "cyt4_%d" % (ti_ % 2)
                    P.dma("sp", xt2[:, :, 0:n], fm(xs_d)[:, :, t0:t0 + n], [("xs", t0)], [cxk], cxk)
                    for s_ in range(n // 128):
                        g = t0 // 128 + s_
                        a1, k1 = y1[ii % 4], ("cy1", ii % 4)
                        a2, k2 = y2[ii % 4], ("cy2", ii % 4)
                        ii += 1
                        P.idma(a1[:], None, yrows_d, desti[:, 0, g:g + 1], ["desti"], [k1], k1)
                        P.idma(a2[:], None, yrows_d, desti[:, 1, g:g + 1], ["desti"], [k2], k2)
                        P.dve(lambda e, a1=a1, s_=s_, g=g, yt4=yt4: e.tensor_scalar(
                            yt4[:, s_, :], a1[:], rinfo[:, 4, g:g + 1], None, ALU.mult), [k1], [(cyk, s_)])
                        P.dve(lambda e, a2=a2, s_=s_, g=g, yt4=yt4: e.scalar_tensor_tensor(
                            yt4[:, s_, :], a2[:], rinfo[:, 5, g:g + 1], yt4[:, s_, :], ALU.mult, ALU.add),
                            [k2, (cyk, s_)], [(cyk, s_)])
                    for c in range(8):
                        pY, pkY = pb[c % 4], ("pb", c % 4)
                        for s_ in range(n // 128):
                            P.pe(lambda e, pY=pY, s_=s_, c=c, yt4=yt4: e.transpose(
                                pY[:, s_ * 128:(s_ + 1) * 128], yt4[:, s_, c * 128:(c + 1) * 128], ident[:]),
                                [(cyk, s_), "ident"], [pkY])
                        P.dve(lambda e, pY=pY, c=c, n=n, j=j, xt2=xt2: e.scalar_tensor_tensor(
                            xt2[:, c, 0:n], pY[:, 0:n], modT[:, l, 40 + c, j:j + 1], xt2[:, c, 0:n], ALU.mult, ALU.add),
                            [pkY, cxk, "modT"], [(cxk, c)] + ([cxk] if c in (0, 7) else []))
                    if last:
                        P.dma("sp", fm(outT_d)[:, :, t0 - NCTX:t0 - NCTX + n], xt2[:, :, 0:n], [cxk], ["out_d"],
                              cxk + "_st")
                    else:
                        P.dma("sp", fm(xs_d)[:, :, t0:t0 + n], xt2[:, :, 0:n], [cxk], [("xs", t0)], cxk + "_st")
                P.barrier()
                P.emit()

        zt = P.sb(gst, "zt", [128, 1024], BF16)
        P.pool(lambda e: e.memset(zt[:], 0.0), [], ["zt"])
        for bi in range(NBLK * 4):
            P.dma("sp", xrows_d[bi * 128:(bi + 1) * 128, :], zt[:], ["zt"], ["xrows_d"], "zt")
        for l in range(nlayers):
            if stop_after == ("ada", l):
                break
            kind = l % 3
            last = (l == 3)
            if kind == 0:
                rglru(l, l // 3, last)
            elif kind == 1:
                attention(l)
            else:
                gmlp(l)
            if stop_after in (("mix", l), ("r2", l)):
                break
            (moe_sparse if SPARSE else moe)(l, last)
        if dbg_d is not None:
            P.dma("sp", modT_d, modT[:].rearrange("p a b c -> p (a b c)"), ["modT"], ["modT_d"], "modTd")
            with ExitStack() as st:
                dt_ = P.sb(st, "dbgt", [128, 8, 512], F32)
                for (t0, n, j) in TILES:
                    P.dma("sp", dt_[:, :, 0:n], fm(xs_d)[:, :, t0:t0 + n], [("xs", t) for t, _, _ in TILES], ["dbgt"], "dbgt")
                    P.dma("sp", fm(dbg_d)[:, :, t0:t0 + n], dt_[:, :, 0:n], ["dbgt"], ["dbg_d"], "dbgt2")
                P.barrier()
                P.emit()
        P.barrier()
        P.emit()
        print("instructions:", P.n_ins, "dma sems:", len(P.dsem))
    return nc


def _host_prep(inp):
    f = np.float32
    x = np.asarray(inp["x"], f)
    ctx = np.asarray(inp["ctx"], f)
    vec = np.zeros((128, NV), f)

    def put(name, arr):
        a = np.asarray(arr, f).reshape(-1, 8, 128)
        o = VOFF[name]
        vec[:, o:o + a.shape[0] * 8] = a.transpose(2, 0, 1).reshape(128, -1)
    for l in range(4):
        put(("gmix", l), inp["norm_mix_g"][l])
        put(("gffn", l), inp["norm_ffn_g"][l])
        o = VOFF[("adab", l)]
        vec[:, o:o + 48] = np.asarray(inp["ada_b"][l], f).reshape(48, 128).T
    for j in range(2):
        put(("convw", j), inp["rg_conv_w"][j])
        put(("convb", j), inp["rg_conv_b"][j])
        put(("ba", j), inp["rg_ba"][j])
        put(("bi", j), inp["rg_bi"][j])
        put(("lam", j), inp["rg_lambda"][j])
    vec[:, VOFF["qg"]] = np.asarray(inp["at_q_g"], f)[0]
    vec[:, VOFF["kg"]] = np.asarray(inp["at_k_g"], f)[0]
    wr = np.concatenate([np.asarray(inp["moe_w_group"], f),
                         np.asarray(inp["moe_w_router"], f).reshape(4, 1024, 32)], axis=2)
    wr = np.ascontiguousarray(wr.reshape(4, 8, 128, 36).transpose(0, 2, 1, 3))
    br = np.concatenate([np.asarray(inp["moe_b_group"], f), np.asarray(inp["moe_b_router"], f).reshape(4, 32)], axis=1)
    br = np.ascontiguousarray(np.broadcast_to(br[:, None, :], (4, 128, 36)))
    p = np.arange(128)
    axis = p // 64
    fr = p % 32
    inv = (10000.0 ** (-(fr.astype(np.float64)) * 2.0 / 64)).astype(f)
    t = np.arange(4096)
    pos = np.where(axis[:, None] == 0, (t // 64)[None, :], (t % 64)[None, :]).astype(f)
    ang = pos * inv[:, None]
    cosT = np.cos(ang).astype(f)
    sinT = np.sin(ang).astype(f)
    rotm = np.zeros((128, 128), f)
    for q in range(128):
        half = (q // 32) % 2
        if half == 0:
            rotm[q + 32, q] = -1.0
        else:
            rotm[q - 32, q] = 1.0
    def relay(w, kc, n):
        w = np.asarray(w, f).reshape(4, 32, kc, 128, n).transpose(0, 1, 3, 2, 4)
        return np.ascontiguousarray(w).reshape(4 * 32 * 128 * 2, 2048)
    hconst = np.zeros((128, 193), f)
    hconst[:, 0:128] = np.triu(np.ones((128, 128), f), 1)
    hconst[:, 128:160] = np.arange(32, dtype=f)[None, :]
    hconst[:, 160] = 2.0 * np.arange(128, dtype=f)
    hconst[:, 161:193] = 1.0
    shared = {
        "vecs": vec,
        "ada_w": np.ascontiguousarray(inp["ada_w"], f),
        "rg_w_in": np.ascontiguousarray(inp["rg_w_in"], f),
        "rg_wa": np.ascontiguousarray(inp["rg_wa"], f),
        "rg_wi": np.ascontiguousarray(inp["rg_wi"], f),
        "rg_w_out": np.ascontiguousarray(inp["rg_w_out"], f),
        "at_w_qkv": np.ascontiguousarray(inp["at_w_qkv"][0], f),
        "at_w_o": np.ascontiguousarray(inp["at_w_o"][0], f),
        "cm_w_in": np.ascontiguousarray(inp["cm_w_in"][0], f),
        "cm_w_sT": np.ascontiguousarray(np.asarray(inp["cm_w_s"][0], f).transpose(2, 0, 1)),
        "cm_w_out": np.ascontiguousarray(inp["cm_w_out"][0], f),
        "cm_lng": np.ascontiguousarray(np.broadcast_to(np.asarray(inp["cm_ln_g"][0], f)[None, :], (128, 2048))),
        "cm_lnb": np.ascontiguousarray(np.broadcast_to(np.asarray(inp["cm_ln_b"][0], f)[None, :], (128, 2048))),
        "cm_bs": np.ascontiguousarray(np.broadcast_to(
            np.repeat(np.asarray(inp["cm_b_s"][0], f), 2, axis=0)[None, :, :], (128, 16, 128))),
        "wr": wr, "br": br,
        "moe_w_gate": relay(inp["moe_w_gate"], 8, 512),
        "moe_w_up": relay(inp["moe_w_up"], 8, 512),
        "moe_w_down": relay(inp["moe_w_down"], 4, 1024),
        "hconst": hconst,
        "cosT": cosT, "sinT": sinT, "rotm": rotm, "ident": np.eye(128, dtype=f),
    }
    in_maps = []
    for k in range(8):
        b = k % 4
        xT = np.ascontiguousarray(np.concatenate([ctx[b], x[b]], axis=0).T)
        cT = np.stack([np.asarray(inp["c"], f)[b], np.asarray(inp["c_ctx"], f)], axis=1)
        cT = np.ascontiguousarray(cT.reshape(8, 128, 2).transpose(1, 0, 2))
        m = dict(shared)
        m["xT"] = xT
        m["cT"] = cT
        in_maps.append(m)
    return in_maps


_NC_CACHE = {}


def kernel(**inputs):
    in_maps = _host_prep(inputs)
    if "nc" not in _NC_CACHE:
        _NC_CACHE["nc"] = build()
    nc = _NC_CACHE["nc"]
    res = run_bass_kernel_spmd(nc, in_maps, core_ids=list(range(8)))
    out = np.stack([np.ascontiguousarray(res.results[b]["outT"].T) for b in range(4)], axis=0)
    return out.astype(np.float32)
```

```python
import numpy as np
from contextlib import ExitStack
import concourse.bass as bass
import concourse.mybir as mybir
from concourse.bass_utils import run_bass_kernel_spmd

F32 = mybir.dt.float32
BF16 = mybir.dt.bfloat16
AF = mybir.ActivationFunctionType
ALU = mybir.AluOpType
AX = mybir.AxisListType

SAME_ENGINE_SYNC = True
SPARSE = True
T = 4352
NCTX = 256
EPS = 1e-6
NBLK = 49
I32 = mybir.dt.int32
TILES = [(0, 256, 1)] + [(256 + 512 * i, 512, 0) for i in range(8)]


class Prog:
    ENG = ("pe", "act", "dve", "pool", "sp")

    def __init__(self, nc, stack):
        self.nc = nc
        self.stack = stack
        self.q = {e: [] for e in self.ENG}
        self.cnt = {e: 0 for e in self.ENG}
        self.esem = {e: stack.enter_context(nc.semaphore("s_" + e)) for e in self.ENG}
        self.known = {e: {} for e in self.ENG}
        self.state = {}
        self.dsem = {}
        self.dcnt = {}
        self.semobj = {}
        self.n_ins = 0

    def sb(self, st, name, shape, dt):
        self.n_sb = getattr(self, "n_sb", 0) + 1
        return st.enter_context(self.nc.sbuf_tensor("s%d_%s" % (self.n_sb, name), list(shape), dt))

    def _st(self, k):
        s = self.state.get(k)
        if s is None:
            s = self.state[k] = [None, []]
        return s

    def _need(self, eng, ev, skip_sem=None):
        if ev is None:
            return
        sem, val, src = ev
        if skip_sem is not None and sem is skip_sem:
            return
        if src == eng and (eng == "pe" or not SAME_ENGINE_SYNC):
            return
        kn = self.known[eng]
        if kn.get(id(sem), 0) >= val:
            return
        kn[id(sem)] = val
        self.q[eng].append(("w", sem, val))

    def _deps(self, eng, reads, writes, skip_sem=None):
        for k in reads:
            self._need(eng, self._st(k)[0])
        for k in writes:
            s = self._st(k)
            self._need(eng, s[0], skip_sem)
            for ev in s[1]:
                self._need(eng, ev)

    def _commit(self, ev, reads, writes):
        for k in reads:
            s = self._st(k)
            s[1].append(ev)
            if len(s[1]) > 64:
                s[1] = s[1][-64:]
        for k in writes:
            s = self._st(k)
            s[0] = ev
            s[1] = []

    def op(self, eng, fn, r=(), w=()):
        self._deps(eng, r, w)
        self.cnt[eng] += 1
        ev = (self.esem[eng], self.cnt[eng], eng)
        self.q[eng].append(("o", fn, self.esem[eng], 1))
        self._commit(ev, r, w)

    def pe(self, fn, r=(), w=()):
        self.op("pe", fn, r, w)

    def act(self, fn, r=(), w=()):
        self.op("act", fn, r, w)

    def dve(self, fn, r=(), w=()):
        self.op("dve", fn, r, w)

    def pool(self, fn, r=(), w=()):
        self.op("pool", fn, r, w)

    def dma(self, eng, out, in_, r, w, sbkey, **kw):
        sem = self.dsem.get(sbkey)
        if sem is None:
            sem = self.dsem[sbkey] = self.stack.enter_context(
                self.nc.semaphore("d%d" % len(self.dsem)))
            self.dcnt[sbkey] = 0
        self._deps(eng, r, w, skip_sem=sem)
        self.dcnt[sbkey] += 16
        ev = (sem, self.dcnt[sbkey], "dma")
        self.q[eng].append(("o", lambda e: e.dma_start(out=out, in_=in_, **kw), sem, 16))
        self._commit(ev, r, w)

    def idma(self, out, out_idx, in_, in_idx, r, w, sbkey):
        eng = "pool"
        sem = self.dsem.get(sbkey)
        if sem is None:
            sem = self.dsem[sbkey] = self.stack.enter_context(
                self.nc.semaphore("d%d" % len(self.dsem)))
            self.dcnt[sbkey] = 0
        self._deps(eng, r, w, skip_sem=sem)
        self.dcnt[sbkey] += 16
        ev = (sem, self.dcnt[sbkey], "dma")
        oo = None if out_idx is None else bass.IndirectOffsetOnAxis(out_idx, 0)
        io = None if in_idx is None else bass.IndirectOffsetOnAxis(in_idx, 0)
        self.q[eng].append(("o", lambda e: e.indirect_dma_start(out=out, out_offset=oo, in_=in_, in_offset=io),
                            sem, 16))
        self._commit(ev, r, w)

    def barrier(self):
        for e in self.ENG:
            for e2 in self.ENG:
                if e2 != e and self.cnt[e2] > 0:
                    self._need(e, (self.esem[e2], self.cnt[e2], e2))
            for k, sem in self.dsem.items():
                if self.dcnt[k] > 0:
                    self._need(e, (sem, self.dcnt[k], "dma"))

    def emit(self):
        nc = self.nc
        q = self.q

        def run(e, lst):
            for it in lst:
                if it[0] == "w":
                    e.wait_ge(it[1], it[2])
                else:
                    it[1](e).then_inc(it[2], it[3])
        self.n_ins += sum(len(v) for v in q.values())
        with nc.Block() as block:
            @block.tensor
            def _(e):
                run(e, q["pe"])

            @block.scalar
            def _(e):
                run(e, q["act"])

            @block.vector
            def _(e):
                run(e, q["dve"])

            @block.gpsimd
            def _(e):
                run(e, q["pool"])

            @block.sync
            def _(e):
                run(e, q["sp"])
        self.q = {e: [] for e in self.ENG}


def _vec_layout():
    off = {}
    n = 0

    def add(name, k):
        nonlocal n
        off[name] = n
        n += k
    for l in range(4):
        add(("gmix", l), 8)
        add(("gffn", l), 8)
        add(("adab", l), 48)
    for j in range(2):
        add(("convw", j), 32)
        add(("convb", j), 8)
        add(("ba", j), 16)
        add(("bi", j), 16)
        add(("lam", j), 16)
    add("qg", 1)
    add("kg", 1)
    return off, n


VOFF, NV = _vec_layout()


def build(nlayers=4, stop_after=None, debug=False):
    nc = bass.Bass("TRN2", target_bir_lowering=False)

    def din(name, shape, dt=F32):
        return nc.dram_tensor(name, list(shape), dt, kind="ExternalInput").ap()

    xT_d = din("xT", [1024, T])
    cT_d = din("cT", [128, 8, 2])
    vecs_d = din("vecs", [128, NV])
    ada_w_d = din("ada_w", [4, 1024, 6144])
    rg_w_in_d = din("rg_w_in", [2, 1024, 2048])
    rg_wa_d = din("rg_wa", [2, 2, 8, 128, 128])
    rg_wi_d = din("rg_wi", [2, 2, 8, 128, 128])
    rg_w_out_d = din("rg_w_out", [2, 1024, 1024])
    at_w_qkv_d = din("at_w_qkv", [1024, 1536])
    at_w_o_d = din("at_w_o", [1024, 1024])
    cm_w_in_d = din("cm_w_in", [1024, 4096])
    cm_w_sT_d = din("cm_w_sT", [128, 8, 128])
    cm_w_out_d = din("cm_w_out", [2048, 1024])
    cm_lng_d = din("cm_lng", [128, 2048])
    cm_lnb_d = din("cm_lnb", [128, 2048])
    cm_bs_d = din("cm_bs", [128, 16, 128])
    wr_d = din("wr", [4, 128, 8, 36])
    br_d = din("br", [4, 128, 36])
    moe_wg_d = din("moe_w_gate", [4 * 32 * 128 * 2, 2048])
    moe_wu_d = din("moe_w_up", [4 * 32 * 128 * 2, 2048])
    moe_wd_d = din("moe_w_down", [4 * 32 * 128 * 2, 2048])
    hconst_d = din("hconst", [128, 193])
    cos_d = din("cosT", [128, 4096])
    sin_d = din("sinT", [128, 4096])
    rm_d = din("rotm", [128, 128])
    ident_d = din("ident", [128, 128])
    outT_d = nc.dram_tensor("outT", [1024, 4096], F32, kind="ExternalOutput").ap()
    skind = "ExternalOutput" if debug else "Internal"
    xs_d = nc.dram_tensor("xs", [1024, T], F32, kind=skind).ap()
    uT_d = nc.dram_tensor("uT", [2048, T], BF16, kind=skind).ap()
    hfT_d = nc.dram_tensor("hfT", [1024, T], BF16, kind=skind).ap()
    hftok_d = nc.dram_tensor("hftok", [T, 1024], BF16, kind=skind).ap()
    xrows_d = nc.dram_tensor("xrows", [NBLK * 512, 1024], BF16, kind="Internal").ap()
    yrows_d = nc.dram_tensor("yrows", [NBLK * 512, 1024], F32, kind="Internal").ap()
    wcT_d = nc.dram_tensor("wcT", [32, T], F32, kind=skind).ap()
    ydbg_d = nc.dram_tensor("ydbg", [1024, 2176], F32, kind="ExternalOutput").ap() if debug else None
    modT_d = nc.dram_tensor("modT_dbg", [128, 4 * 48 * 2], F32, kind="ExternalOutput").ap() if debug else None
    dbg_d = nc.dram_tensor("dbg", [1024, T], F32, kind="ExternalOutput").ap() if debug else None

    def fm(ap2d):
        return ap2d.rearrange("(c p) t -> p c t", p=128)

    with ExitStack() as gst:
        P = Prog(nc, gst)
        vecs = P.sb(gst, "vecs", [128, NV], F32)
        modT = P.sb(gst, "modT", [128, 4, 48, 2], F32)
        gsA = P.sb(gst, "gsA", [128, 4, 8, 2], F32)
        gsF = P.sb(gst, "gsF", [128, 4, 8, 2], F32)
        ones_bf = P.sb(gst, "ones_bf", [128, 128], BF16)
        ident = P.sb(gst, "ident", [128, 128], F32)
        epsc = P.sb(gst, "epsc", [128, 1], F32)
        sdec = P.sb(gst, "sdec", [128, 2, 16], F32)
        sdec2 = P.sb(gst, "sdec2", [128, 2, 16], F32)
        pb = [gst.enter_context(nc.psum_tensor("pb%d" % i, [128, 512], F32)) for i in range(8)]
        hconst = P.sb(gst, "hconst", [128, 193], F32)
        ustrict = P.sb(gst, "ustrict", [128, 128], BF16)
        identb = P.sb(gst, "identb", [128, 128], BF16)
        iota_e = hconst[:, 128:160]
        iota2p = hconst[:, 160:161]
        ones32 = hconst[:, 161:193]
        pb4b = pb[4][:, :].bitcast(BF16)
        pb5b = pb[5][:, :].bitcast(BF16)
        base = P.sb(gst, "base", [128, 32], F32)
        rinfo = P.sb(gst, "rinfo", [128, 6, 34], F32)
        desti = P.sb(gst, "desti", [128, 2, 34], I32)
        widx = P.sb(gst, "widx", [128, NBLK, 2], I32)

        def V(name, i=0, n=1):
            o = VOFF[name] + i
            return vecs[:, o:o + n]

        P.dma("sp", vecs[:], vecs_d, [], ["vecs"], "vecs")
        P.dma("sp", ident[:], ident_d, [], ["ident"], "ident")
        P.dma("sp", hconst[:], hconst_d, [], ["hconst"], "hconst")
        P.dma("pool", ustrict[:], hconst_d[:, 0:128], [], ["ustrict"], "ustrict")
        P.dma("pool", identb[:], ident_d, [], ["identb"], "identb")
        P.pool(lambda e: e.memset(ones_bf[:], 1.0), [], ["ones"])
        P.pool(lambda e: e.memset(modT[:], 0.0), [], ["modT"])
        P.pool(lambda e: e.memset(base[:], 0.0), [], ["base"])
        P.pool(lambda e: e.memset(rinfo[:], 0.0), [], ["rinfo"])
        P.pool(lambda e: e.memset(epsc[:], EPS), [], ["epsc"])

        with ExitStack() as st:
            cT = P.sb(st, "cT", [128, 8, 2], F32)
            cact = P.sb(st, "cact", [128, 8, 2], F32)
            wblk = [P.sb(st, "adaw%d" % i, [128, 8, 768], F32) for i in range(2)]
            P.dma("sp", cT[:], cT_d, [], ["cT"], "cT")
            P.act(lambda e: e.activation(cact[:], cT[:], AF.Silu), ["cT"], ["cact"])
            it = 0
            for l in range(nlayers):
                wv = ada_w_d[l].rearrange("(kc p) n -> p kc n", p=128)
                for nb in range(8):
                    wt = wblk[it % 2]
                    wk = ("adaw", it % 2)
                    P.dma("sp", wt[:], wv[:, :, nb * 768:(nb + 1) * 768], [], [wk], wk)
                    for o in range(6):
                        oc = nb * 6 + o
                        pk = ("pb", oc % 2)
                        pt = pb[oc % 2]
                        for kc in range(8):
                            P.pe(lambda e, pt=pt, wt=wt, o=o, kc=kc: e.matmul(
                                pt[:, 0:2], wt[:, kc, o * 128:(o + 1) * 128], cact[:, kc, :],
                                start=(kc == 0), stop=(kc == 7)), [wk, "cact"], [pk])
                        P.dve(lambda e, pt=pt, l=l, oc=oc: e.tensor_scalar(
                            modT[:, l, oc, :], pt[:, 0:2], V(("adab", l), oc), None, ALU.add),
                            [pk, "vecs"], ["modT"])
                    it += 1
                for j in range(2):
                    P.dve(lambda e, l=l, j=j: e.scalar_tensor_tensor(
                        gsA[:, l, :, j], modT[:, l, 8:16, j], 1.0, V(("gmix", l), 0, 8), ALU.add, ALU.mult),
                        ["modT", "vecs"], ["gs"])
                    P.dve(lambda e, l=l, j=j: e.scalar_tensor_tensor(
                        gsF[:, l, :, j], modT[:, l, 32:40, j], 1.0, V(("gffn", l), 0, 8), ALU.add, ALU.mult),
                        ["modT", "vecs"], ["gs"])
            for j in range(2):
                P.act(lambda e, j=j: e.activation(sdec[:, j, :], V(("lam", j), 0, 16), AF.Exp, scale=-1.0),
                      ["vecs"], ["sdec"])
                P.act(lambda e, j=j: e.activation(sdec2[:, j, :], sdec[:, j, :], AF.Ln, bias=1.0),
                      ["sdec"], ["sdec2"])
                P.dve(lambda e, j=j: e.tensor_scalar(sdec[:, j, :], sdec2[:, j, :], -8.0, None, ALU.mult),
                      ["sdec2"], ["sdec"])
                P.dve(lambda e, j=j: e.tensor_scalar(sdec2[:, j, :], sdec[:, j, :], 2.0, None, ALU.mult),
                      ["sdec"], ["sdec2"])
            P.barrier()
            P.emit()

        def load_x(xt, xk, xsrc, t0, n, rk):
            P.dma("sp", xt[:, :, 0:n], fm(xsrc)[:, :, t0:t0 + n], rk, [xk], xk)

        def norm_mod(wk_, xt, xk, n, l, j, gs, shift_m, hb, hk, h32=None, h32k=None):
            sq, sqk, rt, rtk, tmp, tmpk = wk_
            ends = lambda k, c: [(k, c)] + ([k] if c in (0, 7) else [])
            P.act(lambda e: e.activation(sq[:, :, 0:n], xt[:, :, 0:n], AF.Square), [xk], [sqk])
            for c in range(8):
                P.pe(lambda e, c=c: e.matmul(pb[7][:, 0:n], ones_bf[:], sq[:, c, 0:n],
                                             start=(c == 0), stop=(c == 7)), [sqk, "ones"], [("pb", 7)])
            P.act(lambda e: e.activation(rt[:, 0:n], pb[7][:, 0:n], AF.Sqrt, bias=epsc[:], scale=1.0 / 1024),
                  [("pb", 7), "epsc"], [rtk])
            P.dve(lambda e: e.reciprocal(rt[:, 0:n], rt[:, 0:n]), [rtk], [rtk])
            for c in range(8):
                P.dve(lambda e, c=c: e.scalar_tensor_tensor(
                    tmp[:, c, 0:n], xt[:, c, 0:n], gs[:, l, c, j:j + 1], rt[:, 0:n], ALU.mult, ALU.mult),
                    [xk, rtk, "gs"], [(tmpk, c)])
            for c in range(8):
                sh = modT[:, l, shift_m * 8 + c, j:j + 1]
                if h32 is not None:
                    P.act(lambda e, c=c, sh=sh: e.activation(
                        h32[:, c, 0:n], tmp[:, c, 0:n], AF.Identity, bias=sh), [(tmpk, c), "modT"], ends(h32k, c))
                    P.act(lambda e, c=c, sh=sh: e.activation(hb[:, c, 0:n], tmp[:, c, 0:n], AF.Identity, bias=sh),
                          [(tmpk, c), "modT"], ends(hk, c))
                else:
                    P.act(lambda e, c=c, sh=sh: e.activation(
                        hb[:, c, 0:n], tmp[:, c, 0:n], AF.Identity, bias=sh), [(tmpk, c), "modT"], ends(hk, c))

        def routing(rws, l, h32, h32k, t0, n):
            wrt, brt = rws[-2], rws[-1]
            S = n // 128
            for s in range(S):
                for kc in range(8):
                    P.pe(lambda e, s=s, kc=kc: e.matmul(
                        pb[6][:, s * 36:(s + 1) * 36], h32[:, kc, s * 128:(s + 1) * 128], wrt[:, kc, :],
                        start=(kc == 0), stop=(kc == 7)), [h32k, "wrt"], [("pb", 6)])
            steps = []
            D = lambda f, r=(), w=(): steps.append(("dve", f, r, w))
            A = lambda f, r=(), w=(): steps.append(("act", f, r, w))
            PEs = lambda f, r=(), w=(): steps.append(("pe", f, r, w))
            D(lambda e, X: e.tensor_tensor(X["L"][:], pb[6][:, X["s"] * 36:(X["s"] + 1) * 36], brt[:], ALU.add),
              [("pb", 6), "wrt"])
            D(lambda e, X: e.tensor_reduce(X["gm"][:], X["L"][:, 0:4], AX.X, ALU.max))
            D(lambda e, X: e.tensor_scalar(X["gsel"][:], X["L"][:, 0:4], X["gm"][:], None, ALU.is_equal))
            D(lambda e, X: e.tensor_scalar(X["pen"][:], X["gsel"][:], 1e30, -1e30, ALU.mult, ALU.add))
            D(lambda e, X: e.tensor_scalar(X["gm"][:], X["gm"][:], -1.0, None, ALU.mult))
            A(lambda e, X: e.activation(X["ge"][:], X["L"][:, 0:4], AF.Exp, bias=X["gm"][:], accum_out=X["gsum"][:]))
            D(lambda e, X: e.reciprocal(X["gsum"][:], X["gsum"][:]))
            for gg in range(4):
                D(lambda e, X, gg=gg: e.tensor_scalar(
                    X["ml"][:, gg * 8:(gg + 1) * 8], X["L"][:, 4 + gg * 8:12 + gg * 8], X["pen"][:, gg:gg + 1],
                    None, ALU.add))
            D(lambda e, X: e.max(out=X["m8"][:], in_=X["ml"][:]))
            D(lambda e, X: e.tensor_scalar(X["nv1"][:], X["m8"][:, 0:1], -1.0, None, ALU.mult))
            A(lambda e, X: e.activation(X["dx"][:], X["m8"][:, 1:2], AF.Exp, bias=X["nv1"][:]))
            D(lambda e, X: e.tensor_scalar(X["sel2"][:], X["ml"][:], X["m8"][:, 1:2], None, ALU.is_ge))
            D(lambda e, X: e.tensor_scalar(X["m1"][:], X["ml"][:], X["m8"][:, 0:1], None, ALU.is_equal))
            D(lambda e, X: e.tensor_scalar(X["m2"][:], X["ml"][:], X["m8"][:, 1:2], None, ALU.is_equal))
            D(lambda e, X: e.tensor_scalar(X["d2"][:], X["dx"][:], 1.0, None, ALU.add))
            D(lambda e, X: e.reciprocal(X["d2"][:], X["d2"][:]))
            D(lambda e, X: e.tensor_tensor(rinfo[:, 4, X["g"]:X["g"] + 1], X["d2"][:], X["gsum"][:], ALU.mult),
              [], ["RI"])
            D(lambda e, X: e.tensor_tensor(rinfo[:, 5, X["g"]:X["g"] + 1], rinfo[:, 4, X["g"]:X["g"] + 1],
                                           X["dx"][:], ALU.mult), [], ["RI"])
            D(lambda e, X: e.tensor_copy(X["ohb"][:], X["sel2"][:]))
            PEs(lambda e, X: e.matmul(pb[5][:, X["s"] * 64:X["s"] * 64 + 32], ustrict[:], X["ohb"][:],
                                      start=True, stop=True), ["ustrict"], [("pb", 5)])
            PEs(lambda e, X: e.matmul(pb[5][:, X["s"] * 64 + 32:X["s"] * 64 + 64], ones_bf[:], X["ohb"][:],
                                      start=True, stop=True), ["ones"], [("pb", 5)])
            Xs = []
            for s in range(S):
                names = ["L", "gm", "ge", "gsum", "gsel", "pen", "ml", "m8", "nv1", "ex", "sel2", "dx", "d2", "coef",
                         "m1", "m2", "rk", "j32", "ohb"]
                X = dict(zip(names, rws[s]))
                X["s"] = s
                X["g"] = t0 // 128 + s
                Xs.append(X)
            for (eng, f, r, w) in steps:
                for X in Xs:
                    ck = ("rt", X["s"])
                    rr = [ck] + list(r)
                    ww = [ck] + [(("rinfo", X["g"]) if k == "RI" else k) for k in w]
                    P.op(eng, (lambda e, f=f, X=X: f(e, X)), rr, ww)
            for X in Xs:
                s_ = X["s"]
                ck = ("rt", s_)
                P.dve(lambda e, X=X, s_=s_: e.tensor_tensor(X["rk"][:], pb[5][:, s_ * 64:s_ * 64 + 32], base[:], ALU.add),
                      [("pb", 5), "base", ck], [ck])
                P.dve(lambda e, s_=s_: e.tensor_tensor(base[:], pb[5][:, s_ * 64 + 32:s_ * 64 + 64], base[:], ALU.add),
                      [("pb", 5), "base"], ["base"])
            for q_ in range(4):
                for X in Xs:
                    mm = X["m1"] if q_ % 2 == 0 else X["m2"]
                    srcap = X["rk"][:] if q_ < 2 else iota_e
                    ck = ("rt", X["s"])
                    P.dve(lambda e, mm=mm, srcap=srcap, q_=q_, X=X: e.scalar_tensor_tensor(
                        X["j32"][:], mm[:], 1.0, srcap, ALU.mult, ALU.mult,
                        accum_out=rinfo[:, q_, X["g"]:X["g"] + 1]),
                        [ck, "hconst"], [ck, ("rinfo", X["g"])])

        def post_route(st, l, subtiles):
            kk = P.sb(st, "pr_kk", [128, 32], F32)
            pend = P.sb(st, "pr_pend", [128, 32], F32)
            pstart = P.sb(st, "pr_pstart", [128, 32], F32)
            j32 = P.sb(st, "pr_j32", [128, 32], F32)
            dcol = P.sb(st, "pr_dcol", [128, 2, 34], F32)
            bke = P.sb(st, "pr_bke", [128, NBLK], F32)
            wf = P.sb(st, "pr_wf", [128, NBLK, 2], F32)
            K = "postroute"
            D = lambda fn, r=(), w=(): P.dve(fn, [K, "base", "hconst"] + [("rinfo", g_) for g_ in range(34)] + list(r),
                                             [K] + list(w))
            D(lambda e: e.tensor_scalar(kk[:], base[:], 0.0, None, ALU.is_gt))
            for m in range(1, 9):
                D(lambda e, m=m: e.scalar_tensor_tensor(kk[:], base[:], 512.0 * m, kk[:], ALU.is_gt, ALU.add))
            D(lambda e: e.tensor_scalar(kk[:], kk[:], 512.0, None, ALU.mult))
            D(lambda e: e.tensor_tensor_scan(pend[:], ones32, kk[:], 0.0, ALU.mult, ALU.add))
            D(lambda e: e.tensor_tensor(pstart[:], pend[:], kk[:], ALU.subtract))
            D(lambda e: e.memset(dcol[:], 0.0))
            for g in subtiles:
                for sl in range(2):
                    D(lambda e, g=g, sl=sl: e.scalar_tensor_tensor(
                        j32[:], iota_e, rinfo[:, 2 + sl, g:g + 1], pstart[:], ALU.is_equal, ALU.mult,
                        accum_out=dcol[:, sl, g:g + 1]))
            D(lambda e: e.tensor_tensor(dcol[:], dcol[:], rinfo[:, 0:2, :], ALU.add))
            D(lambda e: e.tensor_copy(desti[:], dcol[:]), [], ["desti"])
            for bi in range(NBLK):
                D(lambda e, bi=bi: e.tensor_scalar(j32[:], pend[:], 512.0 * bi, None, ALU.is_le, ALU.add,
                                                   accum_out=bke[:, bi:bi + 1]))
            D(lambda e: e.tensor_scalar(bke[:], bke[:], 31.0, None, ALU.min))
            for h in range(2):
                D(lambda e, h=h: e.tensor_scalar(wf[:, :, h], bke[:], 256.0, iota2p, ALU.mult, ALU.add))
                D(lambda e, h=h: e.tensor_scalar(wf[:, :, h], wf[:, :, h], float(l * 8192 + h), None, ALU.add))
            D(lambda e: e.tensor_copy(widx[:], wf[:]), [], ["widx"])
            D(lambda e: e.memset(base[:], 0.0), [], ["base"])

        def alloc_route(st, l):
            names = [("L", 36), ("gm", 1), ("ge", 4), ("gsum", 1), ("gsel", 4), ("pen", 4), ("ml", 32), ("m8", 8),
                     ("nv1", 1), ("ex", 32), ("sel2", 32), ("dx", 1), ("d2", 1), ("coef", 1), ("m1", 32), ("m2", 32),
                     ("rk", 32), ("j32", 32)]
            rws = []
            for s_ in range(4):
                rw = [P.sb(st, "r%d_%s" % (s_, nm), [128, k], F32) for nm, k in names]
                rw.append(P.sb(st, "r%d_ohb" % s_, [128, 32], BF16))
                rws.append(rw)
            wrt = P.sb(st, "wrt", [128, 8, 36], F32)
            brt = P.sb(st, "brt", [128, 36], F32)
            P.dma("sp", wrt[:], wr_d[l], [], ["wrt"], "wrt")
            P.dma("sp", brt[:], br_d[l], [], ["wrt"], "wrt")
            return rws + [wrt, brt]

        def alloc_common(st, nxt=2):
            d = {}
            d["xt"] = [P.sb(st, "xt%d" % i, [128, 8, 512], F32) for i in range(nxt)] * (3 - nxt)
            d["sq"] = P.sb(st, "sq", [128, 8, 512], BF16)
            d["rt"] = P.sb(st, "rt", [128, 512], F32)
            d["tmp"] = P.sb(st, "tmp", [128, 8, 512], F32)
            return d

        def post_mixer(l, j_unused, wo_dram, KC, last):
            with ExitStack() as st:
                cm = alloc_common(st)
                xms = [P.sb(st, "xm%d" % i, [128, 8, 512], F32) for i in range(2)]
                hfbs = [P.sb(st, "hfb%d" % i, [128, 8, 512], BF16) for i in range(2)]
                h32 = P.sb(st, "h32", [128, 8, 512], F32)
                hft = P.sb(st, "hft", [128, 1024], BF16)
                rw = alloc_route(st, l)
                wo = P.sb(st, "wo", [128, KC, 1024], BF16)
                U = [P.sb(st, "U%d" % i, [128, KC, 512], BF16) for i in range(2)]
                P.dma("pool", wo[:], wo_dram.rearrange("(kc p) n -> p kc n", p=128), [], ["wo"], "wo")
                xsrc = xT_d if l == 0 else xs_d
                tiles = TILES[1:] if last else TILES

                def WO(i):
                    t0, n, j = tiles[i]
                    xt, xk = cm["xt"][i % 2], ("xt", i % 2)
                    load_x(xt, xk, xsrc, t0, n, [("xs", t0)])
                    Ut, uk = U[i % 2], ("U", i % 2)
                    P.dma("sp", Ut[:, :, 0:n], fm(uT_d)[:, 0:KC, t0:t0 + n], ["uT_d"], [uk], uk)
                    xm, xmk = xms[i % 2], "xm%d" % (i % 2)
                    for co in range(8):
                        pk = ("pb", co % 2)
                        pt = pb[co % 2]
                        for kc in range(KC):
                            P.pe(lambda e, pt=pt, co=co, kc=kc, Ut=Ut, n=n: e.matmul(
                                pt[:, 0:n], wo[:, kc, co * 128:(co + 1) * 128], Ut[:, kc, 0:n],
                                start=(kc == 0), stop=(kc == KC - 1)), ["wo", uk], [pk])
                        P.dve(lambda e, pt=pt, co=co, xm=xm, xt=xt, n=n, j=j: e.scalar_tensor_tensor(
                            xm[:, co, 0:n], pt[:, 0:n], modT[:, l, 16 + co, j:j + 1], xt[:, co, 0:n], ALU.mult, ALU.add),
                            [pk, xk, "modT"], [(xmk, co)] + ([xmk] if co in (0, 7) else []))
                    P.dma("pool", fm(xs_d)[:, :, t0:t0 + n], xm[:, :, 0:n], [xmk], [("xs", t0)], xmk)

                def NR(i):
                    t0, n, j = tiles[i]
                    xm, xmk = xms[i % 2], "xm%d" % (i % 2)
                    hfb, hfk = hfbs[i % 2], "hfb%d" % (i % 2)
                    norm_mod((cm["sq"], "sq", cm["rt"], "rt", cm["tmp"], "tmp"), xm, xmk, n, l, j, gsF, 3,
                             hfb, hfk, h32, "h32")

                def TRR(i):
                    t0, n, j = tiles[i]
                    hfb, hfk = hfbs[i % 2], "hfb%d" % (i % 2)
                    for s_ in range(n // 128):
                        pk4 = ("pb", 4)
                        for c in range(8):
                            P.pe(lambda e, s_=s_, c=c, hfb=hfb: e.transpose(
                                pb4b[:, c * 128:(c + 1) * 128], hfb[:, c, s_ * 128:(s_ + 1) * 128], identb[:]),
                                [hfk, "identb"], [pk4])
                        P.act(lambda e: e.activation(hft[:], pb4b[:, :], AF.Identity), [pk4], ["hft"])
                        P.dma("sp", hftok_d[t0 + s_ * 128:t0 + (s_ + 1) * 128, :], hft[:], ["hft"], ["hftok_d"], "hft")
                    routing(rw, l, h32, "h32", t0, n)

                WO(0)
                for i in range(len(tiles)):
                    if i + 1 < len(tiles):
                        WO(i + 1)
                    NR(i)
                    TRR(i)
                post_route(st, l, [t0 // 128 + s_ for (t0, n, j) in tiles for s_ in range(n // 128)])
                P.barrier()
                P.emit()

        def rglru(l, jj, last):
            xsrc = xT_d if l == 0 else xs_d
            with ExitStack() as st:
                hT = P.sb(st, "hT", [128, 8, T], BF16)
                with ExitStack() as st1:
                    cm = alloc_common(st1)
                    for i, (t0, n, j) in enumerate(TILES):
                        xt, xk = cm["xt"][i % 2], ("xt", i % 2)
                        load_x(xt, xk, xsrc, t0, n, [("xs", t0)])
                        norm_mod((cm["sq"], "sq", cm["rt"], "rt", cm["tmp"], "tmp"), xt, xk, n, l, j, gsA, 0,
                                 hT[:, :, t0:t0 + n], "hT")
                    P.barrier()
                    P.emit()
                xrs = [P.sb(st, "xr%d" % i, [128, T], F32) for i in range(2)]
                xc = P.sb(st, "xc", [128, T], F32)
                bb = P.sb(st, "bb", [128, T], F32)
                hs = P.sb(st, "hs", [128, T], F32)
                xcb = P.sb(st, "xcb", [128, T], BF16)
                gls = [P.sb(st, "gl%d" % i, [128, T], BF16) for i in range(2)]
                wx = [P.sb(st, "wx%d" % i, [128, 8, 128], BF16) for i in range(2)]
                wg = [P.sb(st, "wgt%d" % i, [128, 8, 128], BF16) for i in range(2)]
                wai = [P.sb(st, "wai%d" % i, [128, 4, 128], BF16) for i in range(2)]
                rtl = P.sb(st, "rtl", [128, 512], F32)
                itl = P.sb(st, "itl", [128, 512], F32)
                e2 = P.sb(st, "e2", [128, 512], F32)
                tk = lambda k, i: [(k, i)] + ([k] if i in (0, 8) else [])
                win = rg_w_in_d[jj].rearrange("(kc p) n -> p kc n", p=128)

                def LOADW(c):
                    wxt, wgt, wat = wx[c % 2], wg[c % 2], wai[c % 2]
                    wk = ("rgw", c % 2)
                    P.dma("pool", wxt[:], win[:, :, 1024 + c * 128:1024 + (c + 1) * 128], [], [wk], wk)
                    P.dma("pool", wgt[:], win[:, :, c * 128:(c + 1) * 128], [], [wk], wk)
                    for d in range(2):
                        P.dma("pool", wat[:, 2 * d, :], rg_wa_d[jj, d, c], [], [wk], wk)
                        P.dma("pool", wat[:, 2 * d + 1, :], rg_wi_d[jj, d, c], [], [wk], wk)

                def PROJ(c, i0, i1):
                    wxt, wgt = wx[c % 2], wg[c % 2]
                    wk = ("rgw", c % 2)
                    xr, xrn = xrs[c % 2], "xr%d" % (c % 2)
                    gl, gln = gls[c % 2], "gl%d" % (c % 2)
                    for i, (t0, n, j) in enumerate(TILES):
                        if not (i0 <= i < i1):
                            continue
                        pa, pka = pb[i % 2], ("pb", i % 2)
                        pg, pkg = pb[2 + i % 2], ("pb", 2 + i % 2)
                        for kc in range(8):
                            P.pe(lambda e, pa=pa, kc=kc, t0=t0, n=n, wxt=wxt: e.matmul(
                                pa[:, 0:n], wxt[:, kc, :], hT[:, kc, t0:t0 + n], start=(kc == 0), stop=(kc == 7)),
                                [wk, "hT"], [pka])
                        P.act(lambda e, pa=pa, t0=t0, n=n, xr=xr: e.activation(xr[:, t0:t0 + n], pa[:, 0:n], AF.Identity),
                              [pka], tk(xrn, i))
                        for kc in range(8):
                            P.pe(lambda e, pg=pg, kc=kc, t0=t0, n=n, wgt=wgt: e.matmul(
                                pg[:, 0:n], wgt[:, kc, :], hT[:, kc, t0:t0 + n], start=(kc == 0), stop=(kc == 7)),
                                [wk, "hT"], [pkg])
                        P.act(lambda e, pg=pg, t0=t0, n=n, gl=gl: e.activation(
                            gl[:, t0:t0 + n], pg[:, 0:n], AF.Gelu_apprx_tanh), [pkg], tk(gln, i))

                def CONV(c):
                    xr, xrn = xrs[c % 2], "xr%d" % (c % 2)
                    cw = lambda k, c=c: V(("convw", jj), k * 8 + c)
                    for (s0, e0) in ((0, NCTX), (NCTX, T)):
                        P.dve(lambda e, s0=s0, e0=e0, c=c, cw=cw, xr=xr: e.tensor_scalar(
                            xc[:, s0:e0], xr[:, s0:e0], cw(2), V(("convb", jj), c), ALU.mult, ALU.add),
                            [xrn, "vecs"], ["xc"])
                        for k, off in ((0, -2), (1, -1), (3, 1)):
                            lo = max(s0, s0 - off)
                            hi = min(e0, e0 - off)
                            P.dve(lambda e, lo=lo, hi=hi, off=off, k=k, cw=cw, xr=xr: e.scalar_tensor_tensor(
                                xc[:, lo:hi], xr[:, lo + off:hi + off], cw(k), xc[:, lo:hi], ALU.mult, ALU.add),
                                [xrn, "xc", "vecs"], ["xc"])
                    P.act(lambda e: e.activation(xcb[:], xc[:], AF.Identity), ["xc"], ["xcb"])

                def GATES(c, d):
                    wat = wai[c % 2]
                    wk = ("rgw", c % 2)
                    aa, xrn = xrs[c % 2], "xr%d" % (c % 2)
                    for i, (t0, n, j) in enumerate(TILES):
                        pr, pkr = pb[4 + i % 2], ("pb", 4 + i % 2)
                        pi, pki = pb[6 + i % 2], ("pb", 6 + i % 2)
                        P.pe(lambda e, pr=pr, t0=t0, n=n, d=d, wat=wat: e.matmul(
                            pr[:, 0:n], wat[:, 2 * d, :], xcb[:, t0:t0 + n], start=True, stop=True),
                            [wk, "xcb"], [pkr])
                        P.pe(lambda e, pi=pi, t0=t0, n=n, d=d, wat=wat: e.matmul(
                            pi[:, 0:n], wat[:, 2 * d + 1, :], xcb[:, t0:t0 + n], start=True, stop=True),
                            [wk, "xcb"], [pki])
                        P.act(lambda e, pr=pr, n=n, d=d, c=c: e.activation(
                            rtl[:, 0:n], pr[:, 0:n], AF.Sigmoid, bias=V(("ba", jj), d * 8 + c)),
                            [pkr, "vecs"], ["rtl"])
                        P.act(lambda e, pi=pi, n=n, d=d, c=c: e.activation(
                            itl[:, 0:n], pi[:, 0:n], AF.Sigmoid, bias=V(("bi", jj), d * 8 + c)),
                            [pki, "vecs"], ["itl"])
                        P.act(lambda e, t0=t0, n=n, d=d, c=c, aa=aa: e.activation(
                            aa[:, t0:t0 + n], rtl[:, 0:n], AF.Exp, scale=sdec[:, jj, d * 8 + c:d * 8 + c + 1]),
                            ["rtl", "sdec"], tk(xrn, i))
                        P.act(lambda e, n=n, d=d, c=c: e.activation(
                            e2[:, 0:n], rtl[:, 0:n], AF.Exp, scale=sdec2[:, jj, d * 8 + c:d * 8 + c + 1]),
                            ["rtl", "sdec2"], ["e2"])
                        P.act(lambda e, n=n: e.activation(e2[:, 0:n], e2[:, 0:n], AF.Sqrt, bias=1.0, scale=-1.0),
                              ["e2"], ["e2"])
                        P.pool(lambda e, t0=t0, n=n: e.tensor_tensor(
                            itl[:, 0:n], itl[:, 0:n], xc[:, t0:t0 + n], ALU.mult), ["itl", "xc"], ["itl"])
                        P.dve(lambda e, t0=t0, n=n: e.tensor_tensor(
                            bb[:, t0:t0 + n], itl[:, 0:n], e2[:, 0:n], ALU.mult), ["itl", "e2"], tk("bb", i))

                def SCAN(c, d):
                    aa, xrn = xrs[c % 2], "xr%d" % (c % 2)
                    if d == 0:
                        P.dve(lambda e: e.tensor_tensor_scan(hs[:], aa[:], bb[:], 0.0, ALU.mult, ALU.add),
                              [xrn, "bb"], ["hs"])
                    else:
                        P.dve(lambda e: e.tensor_tensor_scan(
                            bb[:, NCTX - 1::-1], aa[:, NCTX - 1::-1], bb[:, NCTX - 1::-1], 0.0, ALU.mult, ALU.add),
                            [xrn, "bb"], ["bb"])
                        P.dve(lambda e: e.tensor_tensor_scan(
                            bb[:, T - 1:NCTX - 1:-1], aa[:, T - 1:NCTX - 1:-1], bb[:, T - 1:NCTX - 1:-1],
                            bb[:, 0:1], ALU.mult, ALU.add), [xrn, "bb"], ["bb"])
                        P.pool(lambda e: e.tensor_tensor(hs[:], hs[:], bb[:], ALU.add), ["hs", "bb"], ["hs"])

                def OUT(c):
                    gl, gln = gls[c % 2], "gl%d" % (c % 2)
                    P.pool(lambda e, gl=gl: e.tensor_tensor(xcb[:], hs[:], gl[:], ALU.mult), ["hs", gln], ["xcb"])
                    P.dma("sp", uT_d[c * 128:(c + 1) * 128, :], xcb[:], ["xcb"], ["uT_d"], "ub")

                LOADW(0)
                PROJ(0, 0, 9)
                for c in range(8):
                    if c + 1 < 8:
                        LOADW(c + 1)
                    CONV(c)
                    GATES(c, 0)
                    if c + 1 < 8:
                        PROJ(c + 1, 0, 5)
                    SCAN(c, 0)
                    GATES(c, 1)
                    if c + 1 < 8:
                        PROJ(c + 1, 5, 9)
                    SCAN(c, 1)
                    OUT(c)
                P.barrier()
                P.emit()
            if stop_after == ("r2", l):
                return
            post_mixer(l, 0, rg_w_out_d[jj], 8, last)

        def attention(l):
            xsrc = xs_d
            with ExitStack() as st:
                QT = P.sb(st, "QT", [128, 8, T], BF16)
                KT = P.sb(st, "KT", [128, 2, T], BF16)
                Vt = P.sb(st, "Vt", [128, 34, 256], BF16)
                with ExitStack() as st1:
                    cm = alloc_common(st1, 1)
                    hb = P.sb(st1, "hb", [128, 8, 512], BF16)
                    wq = P.sb(st1, "wq", [128, 8, 1536], BF16)
                    cosT = P.sb(st1, "cosT", [128, 512], F32)
                    sinT = P.sb(st1, "sinT", [128, 512], F32)
                    rotm = P.sb(st1, "rotm", [128, 128], BF16)
                    sqhs = [P.sb(st1, "sqh%d" % i, [128, 512], BF16) for i in range(2)]
                    rths = [P.sb(st1, "rth%d" % i, [128, 512], F32) for i in range(2)]
                    qns = [P.sb(st1, "qn%d" % i, [128, 512], BF16) for i in range(2)]
                    t1s = [P.sb(st1, "t1%d" % i, [128, 512], F32) for i in range(2)]
                    t2s = [P.sb(st1, "t2%d" % i, [128, 512], F32) for i in range(2)]
                    P.dma("pool", wq[:], at_w_qkv_d.rearrange("(kc p) n -> p kc n", p=128), [], ["wq"], "wq")
                    P.dma("pool", rotm[:], rm_d, [], ["rotm"], "rotm")
                    for i, (t0, n, j) in enumerate(TILES):
                        xt, xk = cm["xt"][0], ("xt", 0)
                        load_x(xt, xk, xsrc, t0, n, [("xs", t0)])
                        if j == 0:
                            P.dma("sp", cosT[:], cos_d[:, t0 - NCTX:t0 - NCTX + 512], [], ["cs"], "cosT")
                            P.dma("sp", sinT[:], sin_d[:, t0 - NCTX:t0 - NCTX + 512], [], ["cs"], "sinT")
                        norm_mod((cm["sq"], "sq", cm["rt"], "rt", cm["tmp"], "tmp"), xt, xk, n, l, j, gsA, 0,
                                 hb, "hb")
                        for hh in range(10):
                            pq, pkq = pb[hh % 2], ("pb", hh % 2)
                            hp = hh % 2
                            sqh, rth, qn, t1, t2 = sqhs[hp], rths[hp], qns[hp], t1s[hp], t2s[hp]
                            sqk, rthk, qnk, t1k, t2k = ("sqh", hp), ("rth", hp), ("qn", hp), ("t1", hp), ("t2", hp)
                            pss, pks = pb[2 + hp], ("pb", 2 + hp)
                            prr, pkr = pb[4 + hp], ("pb", 4 + hp)
                            for kc in range(8):
                                P.pe(lambda e, pq=pq, kc=kc, hh=hh, n=n: e.matmul(
                                    pq[:, 0:n], wq[:, kc, hh * 128:(hh + 1) * 128], hb[:, kc, 0:n],
                                    start=(kc == 0), stop=(kc == 7)), ["wq", "hb"], [pkq])
                            P.act(lambda e, pq=pq, n=n, sqh=sqh: e.activation(sqh[:, 0:n], pq[:, 0:n], AF.Square),
                                  [pkq], [sqk])
                            P.pe(lambda e, n=n, sqh=sqh, pss=pss: e.matmul(pss[:, 0:n], ones_bf[:], sqh[:, 0:n],
                                                                         start=True, stop=True),
                                 [sqk, "ones"], [pks])
                            P.act(lambda e, n=n, rth=rth, pss=pss: e.activation(
                                rth[:, 0:n], pss[:, 0:n], AF.Sqrt, bias=epsc[:], scale=1.0 / 128),
                                [pks, "epsc"], [rthk])
                            P.dve(lambda e, n=n, rth=rth: e.reciprocal(rth[:, 0:n], rth[:, 0:n]), [rthk], [rthk])
                            gvec = V("qg") if hh < 8 else V("kg")
                            dst = QT[:, hh, t0:t0 + n] if hh < 8 else KT[:, hh - 8, t0:t0 + n]
                            dk = ("QT", hh) if hh < 8 else ("KT", hh - 8)
                            dkw = [dk, "QT" if hh < 8 else "KT"]
                            if j == 1:
                                P.dve(lambda e, pq=pq, n=n, gvec=gvec, dst=dst, rth=rth: e.scalar_tensor_tensor(
                                    dst, pq[:, 0:n], gvec, rth[:, 0:n], ALU.mult, ALU.mult),
                                    [pkq, rthk, "vecs"], dkw)
                            else:
                                P.dve(lambda e, pq=pq, n=n, gvec=gvec, rth=rth, qn=qn: e.scalar_tensor_tensor(
                                    qn[:, 0:n], pq[:, 0:n], gvec, rth[:, 0:n], ALU.mult, ALU.mult),
                                    [pkq, rthk, "vecs"], [qnk])
                                P.pe(lambda e, n=n, qn=qn, prr=prr: e.matmul(prr[:, 0:n], rotm[:], qn[:, 0:n],
                                                                           start=True, stop=True),
                                     [qnk, "rotm"], [pkr])
                                P.pool(lambda e, n=n, qn=qn, t1=t1: e.tensor_tensor(
                                    t1[:, 0:n], qn[:, 0:n], cosT[:, 0:n], ALU.mult), [qnk, "cs"], [t1k])
                                P.dve(lambda e, n=n, t2=t2, prr=prr: e.tensor_tensor(
                                    t2[:, 0:n], prr[:, 0:n], sinT[:, 0:n], ALU.mult), [pkr, "cs"], [t2k])
                                P.pool(lambda e, n=n, dst=dst, t1=t1, t2=t2: e.tensor_tensor(
                                    dst, t1[:, 0:n], t2[:, 0:n], ALU.add), [t1k, t2k], dkw)
                        for s in range(n // 128):
                            kt = t0 // 128 + s
                            pv, pkv = pb[6], ("pb", 6)
                            for kc in range(8):
                                P.pe(lambda e, pv=pv, kc=kc, s=s: e.matmul(
                                    pv[:, 0:256], hb[:, kc, s * 128:(s + 1) * 128], wq[:, kc, 1280:1536],
                                    start=(kc == 0), stop=(kc == 7)), ["wq", "hb"], [pkv])
                            P.act(lambda e, pv=pv, kt=kt: e.activation(Vt[:, kt, :], pv[:, 0:256], AF.Identity),
                                  [pkv], ["Vt"])
                    P.barrier()
                    P.emit()
                pT = [P.sb(st, "pT%d" % i, [128, 512], BF16) for i in range(4)]
                oT = [P.sb(st, "oT%d" % i, [128, 8, 512], BF16) for i in range(2)]
                rd = P.sb(st, "rd", [128, 512], F32)
                accs = [P.sb(st, "acc%d" % i, [128, 512], F32) for i in range(2)]
                accb = P.sb(st, "accb", [128, 512], F32)
                ones_f = P.sb(st, "ones_f", [128, 128], F32)
                P.pool(lambda e: e.memset(ones_f[:], 1.0), [], ["ones_f"])
                SC = 128.0 ** -0.5
                DEPTH = 3
                jobs = []
                for i, (t0, n, j) in enumerate(TILES):
                    for hq in range(8):
                        jobs.append(dict(i=i, t0=t0, n=n, j=j, hq=hq, kv=hq // 4, nkt=(2 if j == 1 else 34),
                                         ot=oT[i % 2], ok=("oT", i % 2),
                                         pO=pb[4 + hq % 2], pkO=("pb", 4 + hq % 2),
                                         pD=pb[6 + hq % 2], pkD=("pb", 6 + hq % 2)))

                def SE(J, kt):
                    pS, pkS = pb[kt % 4], ("pb", kt % 4)
                    ptt, ptk = pT[kt % 4], ("pT", kt % 4)
                    n, t0, kv, hq = J["n"], J["t0"], J["kv"], J["hq"]
                    P.pe(lambda e: e.matmul(
                        pS[:, 0:n], KT[:, kv, kt * 128:(kt + 1) * 128], QT[:, hq, t0:t0 + n],
                        start=True, stop=True), ["QT", "KT"], [pkS])
                    P.act(lambda e: e.activation(ptt[:, 0:n], pS[:, 0:n], AF.Exp, scale=SC), [pkS], [ptk])

                def VV(J, kt):
                    ptt, ptk = pT[kt % 4], ("pT", kt % 4)
                    n, kv, nkt, pO, pkO = J["n"], J["kv"], J["nkt"], J["pO"], J["pkO"]
                    P.pe(lambda e: e.matmul(
                        pO[:, 0:n], Vt[:, kt, kv * 128:(kv + 1) * 128], ptt[:, 0:n],
                        start=(kt == 0), stop=(kt == nkt - 1)), ["Vt", ptk], [pkO])
                    acc, acck = accs[kt % 2], ("acc", kt % 2)
                    if kt < 2:
                        P.dve(lambda e: e.tensor_copy(acc[:, 0:n], ptt[:, 0:n]), [ptk], [acck])
                    else:
                        P.dve(lambda e: e.tensor_tensor(acc[:, 0:n], acc[:, 0:n], ptt[:, 0:n], ALU.add),
                              [ptk, acck], [acck])

                def PRO(J):
                    for kt in range(min(DEPTH, J["nkt"])):
                        SE(J, kt)

                def BODY(J):
                    for kt in range(J["nkt"]):
                        if kt + DEPTH < J["nkt"]:
                            SE(J, kt + DEPTH)
                        VV(J, kt)
                    n = J["n"]
                    P.dve(lambda e: e.tensor_tensor(accb[:, 0:n], accs[0][:, 0:n], accs[1][:, 0:n], ALU.add),
                          [("acc", 0), ("acc", 1)], ["accb"])

                def EPI(J):
                    n, pD, pkD, pO, pkO, hq, ot, ok = (J["n"], J["pD"], J["pkD"], J["pO"], J["pkO"], J["hq"],
                                                       J["ot"], J["ok"])
                    P.pe(lambda e: e.matmul(pD[:, 0:n], ones_f[:], accb[:, 0:n], start=True, stop=True),
                         ["ones_f", "accb"], [pkD])
                    P.dve(lambda e: e.reciprocal(rd[:, 0:n], pD[:, 0:n]), [pkD], ["rd"])
                    P.dve(lambda e: e.tensor_tensor(ot[:, hq, 0:n], pO[:, 0:n], rd[:, 0:n], ALU.mult),
                          [pkO, "rd"], [ok])
                    if hq == 7:
                        t0 = J["t0"]
                        P.dma("sp", fm(uT_d)[:, 0:8, t0:t0 + n], ot[:, :, 0:n], [ok], ["uT_d"], ok)

                PRO(jobs[0])
                for k_, J in enumerate(jobs):
                    BODY(J)
                    if k_ + 1 < len(jobs):
                        PRO(jobs[k_ + 1])
                    EPI(J)
                P.barrier()
                P.emit()
            post_mixer(l, 0, at_w_o_d, 8, False)

        def gmlp(l):
            xsrc = xs_d
            with ExitStack() as st:
                cm = alloc_common(st, 1)
                hb = P.sb(st, "hb", [128, 8, 512], BF16)
                win = P.sb(st, "cwin", [128, 8, 4096], BF16)
                wsT = P.sb(st, "wsT", [128, 8, 128], BF16)
                lng = P.sb(st, "lng", [128, 2048], BF16)
                lnb = P.sb(st, "lnb", [128, 2048], BF16)
                bsr = P.sb(st, "bsr", [128, 16, 128], F32)
                uTs = [P.sb(st, "uTt%d" % i, [128, 16, 512], BF16) for i in range(2)]
                vv = P.sb(st, "vv", [128, 2048], F32)
                vnb = P.sb(st, "vnb", [128, 2048], BF16)
                junk = P.sb(st, "junk", [128, 512], BF16)
                sums = P.sb(st, "sums", [128, 8], F32)
                stt = P.sb(st, "stt", [128, 4], F32)
                mtmp = P.sb(st, "mtmp", [128, 4, 128], F32)
                cwv = cm_w_in_d.rearrange("(kc p) n -> p kc n", p=128)
                for q4 in range(4):
                    P.dma("pool", win[:, :, q4 * 1024:(q4 + 1) * 1024], cwv[:, :, q4 * 1024:(q4 + 1) * 1024],
                          [], ["cwin"], "cwin")
                P.dma("pool", wsT[:], cm_w_sT_d, [], ["wsT"], "wsT")
                P.dma("pool", lng[:], cm_lng_d, [], ["ln"], "lng")
                P.dma("pool", lnb[:], cm_lnb_d, [], ["ln"], "lnb")
                P.dma("sp", bsr[:], cm_bs_d, [], ["bsr"], "bsr")
                for i, (t0, n, j) in enumerate(TILES):
                    xt, xk = cm["xt"][0], ("xt", 0)
                    uTt, utk = uTs[i % 2], ("uTt", i % 2)
                    load_x(xt, xk, xsrc, t0, n, [("xs", t0)])
                    norm_mod((cm["sq"], "sq", cm["rt"], "rt", cm["tmp"], "tmp"), xt, xk, n, l, j, gsA, 0,
                             hb, "hb")
                    for uc in range(16):
                        pu, pku = pb[uc % 2], ("pb", uc % 2)
                        for kc in range(8):
                            P.pe(lambda e, pu=pu, kc=kc, uc=uc, n=n: e.matmul(
                                pu[:, 0:n], win[:, kc, uc * 128:(uc + 1) * 128], hb[:, kc, 0:n],
                                start=(kc == 0), stop=(kc == 7)), ["cwin", "hb"], [pku])
                        P.act(lambda e, pu=pu, uc=uc, n=n, uTt=uTt: e.activation(
                            uTt[:, uc, 0:n], pu[:, 0:n], AF.Gelu_apprx_tanh), [pku], [utk])
                    for s in range(n // 128):
                        for nb in range(4):
                            pv, pkv = pb[2 + nb % 2], ("pb", 2 + nb % 2)
                            for kc in range(8):
                                P.pe(lambda e, pv=pv, kc=kc, s=s, nb=nb: e.matmul(
                                    pv[:, :], hb[:, kc, s * 128:(s + 1) * 128],
                                    win[:, kc, 2048 + nb * 512:2048 + (nb + 1) * 512],
                                    start=(kc == 0), stop=(kc == 7)), ["cwin", "hb"], [pkv])
                            P.act(lambda e, pv=pv, nb=nb: e.activation(
                                vv[:, nb * 512:(nb + 1) * 512], pv[:, :], AF.Gelu_apprx_tanh,
                                accum_out=sums[:, nb:nb + 1]), [pkv], ["vv", "sums"])
                            P.act(lambda e, nb=nb: e.activation(
                                junk[:], vv[:, nb * 512:(nb + 1) * 512], AF.Square,
                                accum_out=sums[:, 4 + nb:5 + nb]), ["vv"], ["junk", "sums"])
                        P.dve(lambda e: e.tensor_reduce(stt[:, 0:1], sums[:, 0:4], AX.X, ALU.add), ["sums"], ["stt"])
                        P.dve(lambda e: e.tensor_reduce(stt[:, 1:2], sums[:, 4:8], AX.X, ALU.add), ["sums"], ["stt"])
                        P.dve(lambda e: e.tensor_scalar(stt[:, 0:2], stt[:, 0:2], 1.0 / 2048, None, ALU.mult),
                              ["stt"], ["stt"])
                        P.dve(lambda e: e.tensor_tensor(stt[:, 2:3], stt[:, 0:1], stt[:, 0:1], ALU.mult),
                              ["stt"], ["stt"])
                        P.dve(lambda e: e.tensor_tensor(stt[:, 1:2], stt[:, 1:2], stt[:, 2:3], ALU.subtract),
                              ["stt"], ["stt"])
                        P.act(lambda e: e.activation(stt[:, 1:2], stt[:, 1:2], AF.Sqrt, bias=epsc[:]),
                              ["stt", "epsc"], ["stt"])
                        P.dve(lambda e: e.reciprocal(stt[:, 1:2], stt[:, 1:2]), ["stt"], ["stt"])
                        P.dve(lambda e: e.tensor_scalar(vv[:], vv[:], stt[:, 0:1], stt[:, 1:2], ALU.subtract, ALU.mult),
                              ["vv", "stt"], ["vv"])
                        P.pool(lambda e: e.tensor_tensor(vv[:], vv[:], lng[:], ALU.mult), ["vv", "ln"], ["vv"])
                        P.dve(lambda e: e.tensor_tensor(vnb[:], vv[:], lnb[:], ALU.add), ["vv", "ln"], ["vnb"])
                        for q4 in range(4):
                            pm, pkm = pb[4 + q4 % 2], ("pb", 4 + q4 % 2)
                            for cc in range(4):
                                ch = q4 * 4 + cc
                                P.pe(lambda e, pm=pm, cc=cc, ch=ch: e.matmul(
                                    pm[:, cc * 128:(cc + 1) * 128], vnb[:, ch * 128:(ch + 1) * 128], wsT[:, ch // 2, :],
                                    start=True, stop=True), ["vnb", "wsT"], [pkm])
                            P.dve(lambda e, pm=pm, q4=q4: e.tensor_tensor(
                                mtmp[:], pm[:, :].rearrange("p (c t) -> p c t", c=4), bsr[:, q4 * 4:(q4 + 1) * 4, :],
                                ALU.add), [pkm, "bsr"], ["mtmp"])
                            P.pool(lambda e, q4=q4, s=s, uTt=uTt: e.tensor_tensor(
                                uTt[:, q4 * 4:(q4 + 1) * 4, s * 128:(s + 1) * 128], mtmp[:],
                                uTt[:, q4 * 4:(q4 + 1) * 4, s * 128:(s + 1) * 128], ALU.mult),
                                ["mtmp", utk], [utk])
                    P.dma("sp", fm(uT_d)[:, :, t0:t0 + n], uTt[:, :, 0:n], [utk], ["uT_d"], utk)
                P.barrier()
                P.emit()
            post_mixer(l, 0, cm_w_out_d, 16, False)

        def moe(l, last):
            ranges = [(256, 2304), (2304, 4352)] if last else [(0, 2176), (2176, 4352)]
            with ExitStack() as st:
                hfh = P.sb(st, "hfh", [128, 8, 2176], BF16)
                yacc = P.sb(st, "yacc", [128, 8, 2176], F32)
                wgs = [P.sb(st, "mwg%d" % i, [128, 8, 512], BF16) for i in range(2)]
                wus = [P.sb(st, "mwu%d" % i, [128, 8, 512], BF16) for i in range(2)]
                wds = [P.sb(st, "mwd%d" % i, [128, 4, 1024], BF16) for i in range(2)]
                wbs = [P.sb(st, "mwb0", [128, 2176], F32)] * 2
                sg = [P.sb(st, "msg%d" % i, [128, 512], F32) for i in range(2)]
                tt = [P.sb(st, "mtt%d" % i, [128, 512], F32) for i in range(2)]
                ab = [P.sb(st, "mab%d" % i, [128, 4, 512], BF16) for i in range(2)]
                xt2 = P.sb(st, "mxt", [128, 8, 256], F32)
                it = 0
                for (r0, r1) in ranges:
                    nt = r1 - r0
                    subt = []
                    o = 0
                    while o < nt:
                        subt.append((o, min(512, nt - o)))
                        o += 512
                    P.dma("sp", hfh[:, :, 0:nt], fm(hfT_d)[:, :, r0:r1], [("hfT", t0) for t0, _, _ in TILES],
                          ["hfh"], "hfh")
                    for c8 in range(8):
                        P.pool(lambda e, c8=c8: e.memset(yacc[:, c8, :], 0.0), [], ["yacc"])
                    for ex in range(32):
                        s2 = it % 2
                        wk = ("mw", s2)
                        wgt, wut, wdt, wbt = wgs[s2], wus[s2], wds[s2], wbs[s2]
                        P.dma("pool", wgt[:], moe_wg_d[l, ex].rearrange("(kc p) n -> p kc n", p=128), [], [wk], wk)
                        P.dma("pool", wut[:], moe_wu_d[l, ex].rearrange("(kc p) n -> p kc n", p=128), [], [wk], wk)
                        P.dma("pool", wdt[:], moe_wd_d[l, ex].rearrange("(kc p) n -> p kc n", p=128), [], [wk], wk)
                        wbk = ("mwb", 0)
                        P.dma("sp", wbt[:, 0:nt], wcT_d[ex, r0:r1].partition_broadcast(128),
                              ["wcT_d"], [wbk], wbk)
                        for ti, (o, n) in enumerate(subt):
                            abt, abk = ab[ti % 2], ("mab", ti % 2)
                            for jc in range(4):
                                pG, pkG = pb[jc % 2], ("pb", jc % 2)
                                pU, pkU = pb[2 + jc % 2], ("pb", 2 + jc % 2)
                                for kc in range(8):
                                    P.pe(lambda e, pG=pG, kc=kc, jc=jc, o=o, n=n, wgt=wgt: e.matmul(
                                        pG[:, 0:n], wgt[:, kc, jc * 128:(jc + 1) * 128], hfh[:, kc, o:o + n],
                                        start=(kc == 0), stop=(kc == 7)), [wk, "hfh"], [pkG])
                                for kc in range(8):
                                    P.pe(lambda e, pU=pU, kc=kc, jc=jc, o=o, n=n, wut=wut: e.matmul(
                                        pU[:, 0:n], wut[:, kc, jc * 128:(jc + 1) * 128], hfh[:, kc, o:o + n],
                                        start=(kc == 0), stop=(kc == 7)), [wk, "hfh"], [pkU])
                                sgt, sgk = sg[jc % 2], ("msg", jc % 2)
                                ttt, ttk = tt[jc % 2], ("mtt", jc % 2)
                                P.act(lambda e, pG=pG, sgt=sgt, n=n: e.activation(sgt[:, 0:n], pG[:, 0:n], AF.Silu),
                                      [pkG], [sgk])
                                P.dve(lambda e, pU=pU, sgt=sgt, ttt=ttt, n=n: e.tensor_tensor(
                                    ttt[:, 0:n], sgt[:, 0:n], pU[:, 0:n], ALU.mult), [sgk, pkU], [ttk])
                                P.pool(lambda e, ttt=ttt, abt=abt, jc=jc, o=o, n=n, wbt=wbt: e.tensor_tensor(
                                    abt[:, jc, 0:n], ttt[:, 0:n], wbt[:, o:o + n], ALU.mult), [ttk, wbk], [abk])
                            for co in range(8):
                                pD, pkD = pb[4 + co % 4], ("pb", 4 + co % 4)
                                for kc in range(4):
                                    P.pe(lambda e, pD=pD, kc=kc, co=co, n=n, wdt=wdt, abt=abt: e.matmul(
                                        pD[:, 0:n], wdt[:, kc, co * 128:(co + 1) * 128], abt[:, kc, 0:n],
                                        start=(kc == 0), stop=(kc == 3)), [wk, abk], [pkD])
                                P.dve(lambda e, pD=pD, co=co, o=o, n=n: e.tensor_tensor(
                                    yacc[:, co, o:o + n], yacc[:, co, o:o + n], pD[:, 0:n], ALU.add),
                                    [pkD, "yacc"], ["yacc"])
                        it += 1
                    if ydbg_d is not None and r0 == ranges[0][0] and l == nlayers - 1:
                        P.dma("sp", fm(ydbg_d)[:, :, 0:nt], yacc[:, :, 0:nt], ["yacc"], ["ydbg_d"], "yacc")
                    for ti, (o, n) in enumerate([(oo, min(256, nt - oo)) for oo in range(0, nt, 256)]):
                        a0 = r0 + o
                        P.dma("sp", xt2[:, :, 0:n], fm(xs_d)[:, :, a0:a0 + n], [("xs", t0) for t0, _, _ in TILES],
                              ["mxt"], "mxt")
                        segs = []
                        if a0 < NCTX:
                            segs.append((0, min(n, NCTX - a0), 1))
                            if a0 + n > NCTX:
                                segs.append((NCTX - a0, n, 0))
                        else:
                            segs.append((0, n, 0))
                        for (q0, q1, j) in segs:
                            for c in range(8):
                                P.dve(lambda e, c=c, q0=q0, q1=q1, j=j, o=o: e.scalar_tensor_tensor(
                                    xt2[:, c, q0:q1], yacc[:, c, o + q0:o + q1], modT[:, l, 40 + c, j:j + 1],
                                    xt2[:, c, q0:q1], ALU.mult, ALU.add), ["yacc", "mxt", "modT"], ["mxt"])
                        if last:
                            P.dma("pool", fm(outT_d)[:, :, a0 - NCTX:a0 - NCTX + n], xt2[:, :, 0:n], ["mxt"],
                                  ["out_d"], "mxt_st")
                        else:
                            P.dma("pool", fm(xs_d)[:, :, a0:a0 + n], xt2[:, :, 0:n], ["mxt"],
                                  [("xs", t0) for t0, _, _ in TILES], "mxt_st")
                P.barrier()
                P.emit()

        def moe_sparse(l, last):
            subtiles = list(range(2, 34)) if last else list(range(34))
            NB = 48 if last else NBLK
            with ExitStack() as st:
                hfts = [P.sb(st, "dhft%d" % i, [128, 1024], BF16) for i in range(2)]
                for ii, g in enumerate(subtiles):
                    ht, hk = hfts[ii % 2], ("dhft", ii % 2)
                    P.dma("sp", ht[:], hftok_d[g * 128:(g + 1) * 128, :], ["hftok_d"], [hk], hk)
                    for sl in range(2):
                        P.idma(xrows_d, desti[:, sl, g:g + 1], ht[:], None, [hk, "desti", "xrows_d"],
                               [("xrows_sc", ii % 2, sl)], ("dsc", ii % 2, sl))
                P.barrier()
                wgs = [P.sb(st, "mwg%d" % i, [128, 8, 512], BF16) for i in range(3)]
                wus = [P.sb(st, "mwu%d" % i, [128, 8, 512], BF16) for i in range(3)]
                wds = [P.sb(st, "mwd%d" % i, [128, 4, 1024], BF16) for i in range(3)]
                xbs = [P.sb(st, "mxb%d" % i, [128, 4, 1024], BF16) for i in range(2)]
                xbT = [P.sb(st, "mxbT%d" % i, [128, 8, 512], BF16) for i in range(2)]
                sg = [P.sb(st, "msg%d" % i, [128, 512], F32) for i in range(2)]
                ab = [P.sb(st, "mab%d" % i, [128, 4, 512], BF16) for i in range(2)]
                yb = [P.sb(st, "myb%d" % i, [128, 1024], F32) for i in range(2)]
                NOW = False

                def LOADS(bi):
                    s2 = bi % 2
                    s3 = bi % 3
                    wk = ("mw", s3)
                    wgt, wut, wdt = wgs[s3], wus[s3], wds[s3]
                    if not (NOW and bi >= 2):
                        for h in range(2):
                            ix = widx[:, bi, h:h + 1]
                            P.idma(wgt[:, 4 * h:4 * h + 4, :].rearrange("p a b -> p (a b)"), None, moe_wg_d, ix,
                                   ["widx"], [wk], wk)
                            P.idma(wut[:, 4 * h:4 * h + 4, :].rearrange("p a b -> p (a b)"), None, moe_wu_d, ix,
                                   ["widx"], [wk], wk)
                            P.idma(wdt[:, 2 * h:2 * h + 2, :].rearrange("p a b -> p (a b)"), None, moe_wd_d, ix,
                                   ["widx"], [wk], wk)
                    xb, xbk = xbs[s2], ("mxb", s2)
                    P.dma("sp", xb[:], xrows_d[bi * 512:(bi + 1) * 512, :].rearrange("(s p) d -> p s d", p=128),
                          ["xrows_d"], [xbk], xbk)

                def TR(bi):
                    s2 = bi % 2
                    xb, xbk = xbs[s2], ("mxb", s2)
                    xt_, xtk = xbT[s2], ("mxbT", s2)
                    for kc in range(8):
                        pX, pkX = (pb4b, ("pb", 4)) if kc % 2 == 0 else (pb5b, ("pb", 5))
                        for s_ in range(4):
                            P.pe(lambda e, pX=pX, s_=s_, kc=kc, xb=xb: e.transpose(
                                pX[:, s_ * 128:(s_ + 1) * 128], xb[:, s_, kc * 128:(kc + 1) * 128], identb[:]),
                                [xbk, "identb"], [pkX])
                        if kc % 2 == 0:
                            P.act(lambda e, pX=pX, kc=kc, xt_=xt_: e.activation(xt_[:, kc, :], pX[:, 0:512], AF.Identity),
                                  [pkX], [(xtk, kc)])
                        else:
                            P.dve(lambda e, pX=pX, kc=kc, xt_=xt_: e.tensor_copy(xt_[:, kc, :], pX[:, 0:512]),
                                  [pkX], [(xtk, kc)])

                def GU(bi):
                    s2 = bi % 2
                    s3 = bi % 3
                    wk = ("mw", s3)
                    wgt, wut = wgs[s3], wus[s3]
                    xt_, xtk = xbT[s2], ("mxbT", s2)
                    abt, abk = ab[s2], ("mab", s2)
                    for jc in range(4):
                        pG, pkG = pb[jc % 2], ("pb", jc % 2)
                        pU, pkU = pb[2 + jc % 2], ("pb", 2 + jc % 2)
                        for kc in range(8):
                            P.pe(lambda e, pG=pG, kc=kc, jc=jc, wgt=wgt, xt_=xt_: e.matmul(
                                pG[:, :], wgt[:, kc, jc * 128:(jc + 1) * 128], xt_[:, kc, :],
                                start=(kc == 0), stop=(kc == 7)), [wk, (xtk, kc)], [pkG])
                        for kc in range(8):
                            P.pe(lambda e, pU=pU, kc=kc, jc=jc, wut=wut, xt_=xt_: e.matmul(
                                pU[:, :], wut[:, kc, jc * 128:(jc + 1) * 128], xt_[:, kc, :],
                                start=(kc == 0), stop=(kc == 7)), [wk, (xtk, kc)], [pkU])
                        sgt, sgk = sg[jc % 2], ("msg", jc % 2)
                        P.act(lambda e, pG=pG, sgt=sgt: e.activation(sgt[:], pG[:, :], AF.Silu), [pkG], [sgk])
                        P.dve(lambda e, pU=pU, sgt=sgt, abt=abt, jc=jc: e.tensor_tensor(
                            abt[:, jc, :], sgt[:], pU[:, :], ALU.mult), [sgk, pkU], [(abk, jc)])

                def DN(bi):
                    s2 = bi % 2
                    s3 = bi % 3
                    wk = ("mw", s3)
                    wdt = wds[s3]
                    abt, abk = ab[s2], ("mab", s2)
                    for s_ in range(4):
                        ybt, ybk = yb[s_ % 2], ("myb", s_ % 2)
                        for hh in range(2):
                            pD, pkD = pb[6 + hh], ("pb", 6 + hh)
                            for kc in range(4):
                                P.pe(lambda e, pD=pD, kc=kc, s_=s_, hh=hh, wdt=wdt, abt=abt: e.matmul(
                                    pD[:, :], abt[:, kc, s_ * 128:(s_ + 1) * 128], wdt[:, kc, hh * 512:(hh + 1) * 512],
                                    start=(kc == 0), stop=(kc == 3)), [wk, (abk, kc)], [pkD])
                            if hh == 0:
                                P.act(lambda e, pD=pD, ybt=ybt: e.activation(ybt[:, 0:512], pD[:, :], AF.Identity),
                                      [pkD], [ybk])
                            else:
                                P.dve(lambda e, pD=pD, ybt=ybt: e.tensor_copy(ybt[:, 512:1024], pD[:, :]),
                                      [pkD], [ybk])
                        r0_ = bi * 512 + s_ * 128
                        P.dma("sp", yrows_d[r0_:r0_ + 128, :], ybt[:], [ybk], [("yrows_st", s_ % 2)], ybk)

                LOADS(0)
                LOADS(1)
                TR(0)
                for bi in range(NB):
                    if bi + 2 < NB:
                        LOADS(bi + 2)
                    GU(bi)
                    if bi + 1 < NB:
                        TR(bi + 1)
                    DN(bi)
                P.barrier()
                P.emit()
            with ExitStack() as st:
                y1 = [P.sb(st, "cy1_%d" % i, [128, 1024], F32) for i in range(4)]
                y2 = [P.sb(st, "cy2_%d" % i, [128, 1024], F32) for i in range(4)]
                yt4s = [P.sb(st, "cyt4_%d" % i, [128, 4, 1024], F32) for i in range(2)]
                xt2s = [P.sb(st, "cxt%d" % i, [128, 8, 512], F32) for i in range(2)]
                tiles = TILES[1:] if last else TILES
                ii = 0
                for ti_, (t0, n, j) in enumerate(tiles):
                    xt2, cxk = xt2s[ti_ % 2], "cxt%d" % (ti_ % 2)
                    yt4, cyk = yt4s[ti_ % 2], "cyt4_%d" % (ti_ % 2)
                    P.dma("sp", xt2[:, :, 0:n], fm(xs_d)[:, :, t0:t0 + n], [("xs", t0)], [cxk], cxk)
                    for s_ in range(n // 128):
                        g = t0 // 128 + s_
                        a1, k1 = y1[ii % 4], ("cy1", ii % 4)
                        a2, k2 = y2[ii % 4], ("cy2", ii % 4)
                        ii += 1
                        P.idma(a1[:], None, yrows_d, desti[:, 0, g:g + 1], ["desti"], [k1], k1)
                        P.idma(a2[:], None, yrows_d, desti[:, 1, g:g + 1], ["desti"], [k2], k2)
                        P.dve(lambda e, a1=a1, s_=s_, g=g, yt4=yt4: e.tensor_scalar(
                            yt4[:, s_, :], a1[:], rinfo[:, 4, g:g + 1], None, ALU.mult), [k1], [(cyk, s_)])
                        P.dve(lambda e, a2=a2, s_=s_, g=g, yt4=yt4: e.scalar_tensor_tensor(
                            yt4[:, s_, :], a2[:], rinfo[:, 5, g:g + 1], yt4[:, s_, :], ALU.mult, ALU.add),
                            [k2, (cyk, s_)], [(cyk, s_)])
                    for c in range(8):
                        pY, pkY = pb[c % 4], ("pb", c % 4)
                        for s_ in range(n // 128):
                            P.pe(lambda e, pY=pY, s_=s_, c=c, yt4=yt4: e.transpose(
                                pY[:, s_ * 128:(s_ + 1) * 128], yt4[:, s_, c * 128:(c + 1) * 128], ident[:]),
                                [(cyk, s_), "ident"], [pkY])
                        P.dve(lambda e, pY=pY, c=c, n=n, j=j, xt2=xt2: e.scalar_tensor_tensor(
                            xt2[:, c, 0:n], pY[:, 0:n], modT[:, l, 40 + c, j:j + 1], xt2[:, c, 0:n], ALU.mult, ALU.add),
                            [pkY, cxk, "modT"], [(cxk, c)] + ([cxk] if c in (0, 7) else []))
                    if last:
                        P.dma("sp", fm(outT_d)[:, :, t0 - NCTX:t0 - NCTX + n], xt2[:, :, 0:n], [cxk], ["out_d"],
                              cxk + "_st")
                    else:
                        P.dma("sp", fm(xs_d)[:, :, t0:t0 + n], xt2[:, :, 0:n], [cxk], [("xs", t0)], cxk + "_st")
                P.barrier()
                P.emit()

        zt = P.sb(gst, "zt", [128, 1024], BF16)
        P.pool(lambda e: e.memset(zt[:], 0.0), [], ["zt"])
        for bi in range(NBLK * 4):
            P.dma("sp", xrows_d[bi * 128:(bi + 1) * 128, :], zt[:], ["zt"], ["xrows_d"], "zt")
        for l in range(nlayers):
            if stop_after == ("ada", l):
                break
            kind = l % 3
            last = (l == 3)
            if kind == 0:
                rglru(l, l // 3, last)
            elif kind == 1:
                attention(l)
            else:
                gmlp(l)
            if stop_after in (("mix", l), ("r2", l)):
                break
            (moe_sparse if SPARSE else moe)(l, last)
        if dbg_d is not None:
            P.dma("sp", modT_d, modT[:].rearrange("p a b c -> p (a b c)"), ["modT"], ["modT_d"], "modTd")
            with ExitStack() as st:
                dt_ = P.sb(st, "dbgt", [128, 8, 512], F32)
                for (t0, n, j) in TILES:
                    P.dma("sp", dt_[:, :, 0:n], fm(xs_d)[:, :, t0:t0 + n], [("xs", t) for t, _, _ in TILES], ["dbgt"], "dbgt")
                    P.dma("sp", fm(dbg_d)[:, :, t0:t0 + n], dt_[:, :, 0:n], ["dbgt"], ["dbg_d"], "dbgt2")
                P.barrier()
                P.emit()
        P.barrier()
        P.emit()
        print("instructions:", P.n_ins, "dma sems:", len(P.dsem))
    return nc


def _host_prep(inp):
    f = np.float32
    x = np.asarray(inp["x"], f)
    ctx = np.asarray(inp["ctx"], f)
    vec = np.zeros((128, NV), f)

    def put(name, arr):
        a = np.asarray(arr, f).reshape(-1, 8, 128)
        o = VOFF[name]
        vec[:, o:o + a.shape[0] * 8] = a.transpose(2, 0, 1).reshape(128, -1)
    for l in range(4):
        put(("gmix", l), inp["norm_mix_g"][l])
        put(("gffn", l), inp["norm_ffn_g"][l])
        o = VOFF[("adab", l)]
        vec[:, o:o + 48] = np.asarray(inp["ada_b"][l], f).reshape(48, 128).T
    for j in range(2):
        put(("convw", j), inp["rg_conv_w"][j])
        put(("convb", j), inp["rg_conv_b"][j])
        put(("ba", j), inp["rg_ba"][j])
        put(("bi", j), inp["rg_bi"][j])
        put(("lam", j), inp["rg_lambda"][j])
    vec[:, VOFF["qg"]] = np.asarray(inp["at_q_g"], f)[0]
    vec[:, VOFF["kg"]] = np.asarray(inp["at_k_g"], f)[0]
    wr = np.concatenate([np.asarray(inp["moe_w_group"], f),
                         np.asarray(inp["moe_w_router"], f).reshape(4, 1024, 32)], axis=2)
    wr = np.ascontiguousarray(wr.reshape(4, 8, 128, 36).transpose(0, 2, 1, 3))
    br = np.concatenate([np.asarray(inp["moe_b_group"], f), np.asarray(inp["moe_b_router"], f).reshape(4, 32)], axis=1)
    br = np.ascontiguousarray(np.broadcast_to(br[:, None, :], (4, 128, 36)))
    p = np.arange(128)
    axis = p // 64
    fr = p % 32
    inv = (10000.0 ** (-(fr.astype(np.float64)) * 2.0 / 64)).astype(f)
    t = np.arange(4096)
    pos = np.where(axis[:, None] == 0, (t // 64)[None, :], (t % 64)[None, :]).astype(f)
    ang = pos * inv[:, None]
    cosT = np.cos(ang).astype(f)
    sinT = np.sin(ang).astype(f)
    rotm = np.zeros((128, 128), f)
    for q in range(128):
        half = (q // 32) % 2
        if half == 0:
            rotm[q + 32, q] = -1.0
        else:
            rotm[q - 32, q] = 1.0
    def relay(w, kc, n):
        w = np.asarray(w, f).reshape(4, 32, kc, 128, n).transpose(0, 1, 3, 2, 4)
        return np.ascontiguousarray(w).reshape(4 * 32 * 128 * 2, 2048)
    hconst = np.zeros((128, 193), f)
    hconst[:, 0:128] = np.triu(np.ones((128, 128), f), 1)
    hconst[:, 128:160] = np.arange(32, dtype=f)[None, :]
    hconst[:, 160] = 2.0 * np.arange(128, dtype=f)
    hconst[:, 161:193] = 1.0
    shared = {
        "vecs": vec,
        "ada_w": np.ascontiguousarray(inp["ada_w"], f),
        "rg_w_in": np.ascontiguousarray(inp["rg_w_in"], f),
        "rg_wa": np.ascontiguousarray(inp["rg_wa"], f),
        "rg_wi": np.ascontiguousarray(inp["rg_wi"], f),
        "rg_w_out": np.ascontiguousarray(inp["rg_w_out"], f),
        "at_w_qkv": np.ascontiguousarray(inp["at_w_qkv"][0], f),
        "at_w_o": np.ascontiguousarray(inp["at_w_o"][0], f),
        "cm_w_in": np.ascontiguousarray(inp["cm_w_in"][0], f),
        "cm_w_sT": np.ascontiguousarray(np.asarray(inp["cm_w_s"][0], f).transpose(2, 0, 1)),
        "cm_w_out": np.ascontiguousarray(inp["cm_w_out"][0], f),
        "cm_lng": np.ascontiguousarray(np.broadcast_to(np.asarray(inp["cm_ln_g"][0], f)[None, :], (128, 2048))),
        "cm_lnb": np.ascontiguousarray(np.broadcast_to(np.asarray(inp["cm_ln_b"][0], f)[None, :], (128, 2048))),
        "cm_bs": np.ascontiguousarray(np.broadcast_to(
            np.repeat(np.asarray(inp["cm_b_s"][0], f), 2, axis=0)[None, :, :], (128, 16, 128))),
        "wr": wr, "br": br,
        "moe_w_gate": relay(inp["moe_w_gate"], 8, 512),
        "moe_w_up": relay(inp["moe_w_up"], 8, 512),
        "moe_w_down": relay(inp["moe_w_down"], 4, 1024),
        "hconst": hconst,
        "cosT": cosT, "sinT": sinT, "rotm": rotm, "ident": np.eye(128, dtype=f),
    }
    in_maps = []
    for k in range(8):
        b = k % 4
        xT = np.ascontiguousarray(np.concatenate([ctx[b], x[b]], axis=0).T)
        cT = np.stack([np.asarray(inp["c"], f)[b], np.asarray(inp["c_ctx"], f)], axis=1)
        cT = np.ascontiguousarray(cT.reshape(8, 128, 2).transpose(1, 0, 2))
        m = dict(shared)
        m["xT"] = xT
        m["cT"] = cT
        in_maps.append(m)
    return in_maps


_NC_CACHE = {}


def kernel(**inputs):
    in_maps = _host_prep(inputs)
    if "nc" not in _NC_CACHE:
        _NC_CACHE["nc"] = build()
    nc = _NC_CACHE["nc"]
    res = run_bass_kernel_spmd(nc, in_maps, core_ids=list(range(8)))
    out = np.stack([np.ascontiguousarray(res.results[b]["outT"].T) for b in range(4)], axis=0)
    return out.astype(np.float32)
```
